# Optimizing a Trainium2 kernel written in Bass

```python
import math
import jax, jax.numpy as jnp
from jax import lax
import numpy as np

D_MODEL = 1024
BATCH = 32
SEQ = 2048
DEPTH = 2

CONV_CH = 512
CONV_K = 31
DA_HEADS = 4
DA_SUB = 64
DA_QK = 2 * DA_SUB
DA_V = 128
ROT_DIM = DA_SUB // 4
ROPE_THETA = 500000.0
Q_BLOCK = 128
HG_HEADS = 4
HG_DK = 128
HG_DV = 128
HG_CHUNK = 32
MIX_WIDTH = 512
N_BRANCH = 3
N_GROUPS = 4
EXPERTS_PER_GROUP = 8
N_EXPERTS = N_GROUPS * EXPERTS_PER_GROUP
TOP_K = 2
D_EXPERT = 512
DN_ALPHA = (2 * DEPTH) ** 0.25
DN_BETA = (8 * DEPTH) ** -0.25
LN_EPS = 1e-5

SPLIT_SIZES = (
    2 * CONV_CH,
    DA_HEADS * DA_QK,
    DA_HEADS * DA_QK,
    DA_HEADS * DA_V,
    HG_HEADS * HG_DK,
    HG_HEADS * HG_DK,
    HG_HEADS * HG_DV,
    HG_HEADS * HG_DV,
    N_BRANCH * D_MODEL,
)
IN_COLS = sum(SPLIT_SIZES)
SPLIT_IDX = tuple(int(v) for v in np.cumsum(SPLIT_SIZES)[:-1])

kernel_name = "hybrid_conv_diffattn_hgrn2_hiermoe_deepnorm"


def layer_norm(x, g, b):
    xf = x.astype(jnp.float32)
    mu = jnp.mean(xf, -1, keepdims=True)
    var = jnp.mean(jnp.square(xf - mu), -1, keepdims=True)
    return ((xf - mu) * lax.rsqrt(var + LN_EPS)).astype(x.dtype) * g + b


def rms_norm(x, g):
    xf = x.astype(jnp.float32)
    return (xf * lax.rsqrt(jnp.mean(xf * xf, -1, keepdims=True) + LN_EPS)).astype(x.dtype) * g


def rotary_tables(positions, dtype):
    inv_freq = ROPE_THETA ** (-jnp.arange(0, ROT_DIM, 2, dtype=jnp.float32) / ROT_DIM)
    ang = positions.astype(jnp.float32)[..., None] * inv_freq
    return jnp.cos(ang).astype(dtype), jnp.sin(ang).astype(dtype)


def partial_rotary(t, cos, sin):
    half = ROT_DIM // 2
    t1, t2 = t[..., :half], t[..., half:ROT_DIM]
    c, s = cos[:, :, None, :], sin[:, :, None, :]
    return jnp.concatenate([t1 * c - t2 * s, t2 * c + t1 * s, t[..., ROT_DIM:]], -1)


def conformer_conv(a, conv_w, conv_b, ln_g, ln_b):
    u = a[..., :CONV_CH] * jax.nn.sigmoid(a[..., CONV_CH:])
    y = lax.conv_general_dilated(
        u, conv_w[:, None, :].astype(u.dtype), window_strides=(1,),
        padding=[(CONV_K - 1, 0)], dimension_numbers=("NWC", "WIO", "NWC"),
        feature_group_count=CONV_CH) + conv_b
    return jax.nn.silu(layer_norm(y, ln_g, ln_b))


def diff_attention(q, k, v, cos, sin, lam_params, norm_g, layer_idx):
    B, S, _ = q.shape
    q = partial_rotary(q.reshape(B, S, 2 * DA_HEADS, DA_SUB), cos, sin) * (DA_SUB ** -0.5)
    k = partial_rotary(k.reshape(B, S, 2 * DA_HEADS, DA_SUB), cos, sin)
    q = q.reshape(B, S, DA_HEADS, 2, DA_SUB)
    k = k.reshape(B, S, DA_HEADS, 2, DA_SUB)
    v = v.reshape(B, S, DA_HEADS, DA_V)
    lam_init = 0.8 - 0.6 * math.exp(-0.3 * layer_idx)
    lp = lam_params.astype(jnp.float32)
    lam = jnp.exp(jnp.sum(lp[0] * lp[1])) - jnp.exp(jnp.sum(lp[2] * lp[3])) + lam_init
    nb = S // Q_BLOCK
    qb = q.reshape(B, nb, Q_BLOCK, DA_HEADS, 2, DA_SUB).transpose(1, 0, 2, 3, 4, 5)
    key_pos = jnp.arange(S)

    def one_block(args):
        qi, i = args
        s = jnp.einsum("bqhpd,bkhpd->bhpqk", qi, k, preferred_element_type=jnp.float32)
        qpos = i * Q_BLOCK + jnp.arange(Q_BLOCK)
        s = jnp.where(key_pos[None, :] <= qpos[:, None], s, -jnp.inf)
        p = jax.nn.softmax(s, axis=-1)
        a = p[:, :, 0] - lam * p[:, :, 1]
        return jnp.einsum("bhqk,bkhd->bqhd", a.astype(v.dtype), v)

    o = lax.map(one_block, (qb, jnp.arange(nb)))
    o = o.transpose(1, 0, 2, 3, 4).reshape(B, S, DA_HEADS, DA_V)
    o = rms_norm(o, norm_g) * (1.0 - lam_init)
    return o.reshape(B, S, DA_HEADS * DA_V)


def hgrn2(q, f_logit, i_in, g, lb, norm_g):
    B, S, _ = q.shape
    C = HG_CHUNK
    nc = S // C
    qf = jax.nn.silu(q).astype(jnp.float32).reshape(B, S, HG_HEADS, HG_DK)
    lbf = lb.astype(jnp.float32)
    log_f = jnp.logaddexp(jnp.log(lbf), jnp.log1p(-lbf) + jax.nn.log_sigmoid(f_logit.astype(jnp.float32)))
    kf = -jnp.expm1(log_f)
    log_f = log_f.reshape(B, S, HG_HEADS, HG_DK)
    kf = kf.reshape(B, S, HG_HEADS, HG_DK)
    vf = i_in.astype(jnp.float32).reshape(B, S, HG_HEADS, HG_DV)

    def to_chunks(t):
        return t.reshape(B, nc, C, HG_HEADS, t.shape[-1]).transpose(1, 0, 3, 2, 4)

    bcum = jnp.cumsum(to_chunks(log_f), axis=-2)
    causal = jnp.tril(jnp.ones((C, C), dtype=bool))

    def step(state, xs):
        q_, k_, v_, b_ = xs
        o_inter = jnp.einsum("bhtc,bhcv->bhtv", q_ * jnp.exp(b_), state)
        diff = b_[:, :, :, None, :] - b_[:, :, None, :, :]
        decay = jnp.exp(jnp.where(causal[:, :, None], diff, -jnp.inf))
        att = jnp.einsum("bhtc,bhsc,bhtsc->bhts", q_, k_, decay)
        o_intra = jnp.einsum("bhts,bhsv->bhtv", att, v_)
        b_last = b_[:, :, -1:, :]
        k_dec = k_ * jnp.exp(b_last - b_)
        state = jnp.exp(b_last[:, :, 0, :])[..., None] * state + jnp.einsum("bhsc,bhsv->bhcv", k_dec, v_)
        return state, o_inter + o_intra

    s0 = jnp.zeros((B, HG_HEADS, HG_DK, HG_DV), jnp.float32)
    _, o = lax.scan(step, s0, (to_chunks(qf), to_chunks(kf), to_chunks(vf), bcum))
    o = o.transpose(1, 0, 3, 2, 4).reshape(B, S, HG_HEADS, HG_DV).astype(q.dtype)
    o = rms_norm(o, norm_g) * jax.nn.silu(g.reshape(B, S, HG_HEADS, HG_DV))
    return o.reshape(B, S, HG_HEADS * HG_DV)


def hier_moe(x, wg, bg, we, be, w1, w3, w2):
    B, S, D = x.shape
    t = x.reshape(-1, D)
    glog = (t @ wg).astype(jnp.float32) + bg
    gprob = jax.nn.softmax(glog, axis=-1)
    g_idx = jnp.argmax(glog, axis=-1)
    p_group = jnp.take_along_axis(gprob, g_idx[:, None], axis=-1)
    elog_all = jnp.einsum("td,gde->tge", t, we).astype(jnp.float32) + be
    elog = jnp.take_along_axis(elog_all, g_idx[:, None, None], axis=1)[:, 0]
    top_v, top_i = lax.top_k(elog, TOP_K)
    w_top = jax.nn.softmax(top_v, axis=-1) * p_group
    expert_id = g_idx[:, None] * EXPERTS_PER_GROUP + top_i
    comb = jnp.sum(jax.nn.one_hot(expert_id, N_EXPERTS, dtype=jnp.float32) * w_top[..., None], axis=1)
    comb = comb.astype(t.dtype)
    out = jnp.zeros_like(t)
    for e in range(N_EXPERTS):
        h = jax.nn.silu(t @ w1[e]) * (t @ w3[e])
        out = out + comb[:, e:e + 1] * (h @ w2[e])
    return out.reshape(B, S, D)


def setup_inputs(seed: int = 0) -> dict:
    key = jax.random.key(seed)
    ks = jax.random.split(key, 26)
    f32 = jnp.float32
    nrm = lambda k, shape, scale: jax.random.normal(k, shape, f32) * scale
    L, D = DEPTH, D_MODEL
    start = jax.random.randint(ks[1], (BATCH,), 0, 4096, dtype=jnp.int32)
    positions = (start[:, None] + jnp.arange(SEQ, dtype=jnp.int32)[None, :]).astype(jnp.int32)
    return {
        "x": jax.random.normal(ks[0], (BATCH, SEQ, D), f32),
        "positions": positions,
        "w_in": nrm(ks[2], (L, D, IN_COLS), D ** -0.5),
        "conv_w": nrm(ks[3], (L, CONV_K, CONV_CH), CONV_K ** -0.5),
        "conv_b": nrm(ks[4], (L, CONV_CH), 0.02),
        "conv_ln_g": 1.0 + nrm(ks[5], (L, CONV_CH), 0.02),
        "conv_ln_b": nrm(ks[6], (L, CONV_CH), 0.02),
        "da_lambda": nrm(ks[7], (L, 4, DA_SUB), 0.1),
        "da_norm_g": 1.0 + nrm(ks[8], (L, DA_V), 0.02),
        "hg_lb": nrm(ks[9], (L, HG_HEADS * HG_DK), 0.5),
        "hg_norm_g": 1.0 + nrm(ks[10], (L, HG_DV), 0.02),
        "w_branch": nrm(ks[11], (L, N_BRANCH, MIX_WIDTH, D), MIX_WIDTH ** -0.5),
        "b_gate": nrm(ks[12], (L, N_BRANCH, D), 0.02),
        "w_out": nrm(ks[13], (L, D, D), D ** -0.5 * DN_BETA),
        "ln1_g": 1.0 + nrm(ks[14], (L, D), 0.02),
        "ln1_b": nrm(ks[15], (L, D), 0.02),
        "router_group": nrm(ks[16], (L, D, N_GROUPS), D ** -0.5),
        "router_group_b": nrm(ks[17], (L, N_GROUPS), 0.01),
        "router_expert": nrm(ks[18], (L, N_GROUPS, D, EXPERTS_PER_GROUP), D ** -0.5),
        "router_expert_b": nrm(ks[19], (L, N_GROUPS, EXPERTS_PER_GROUP), 0.01),
        "exp_w1": nrm(ks[20], (L, N_EXPERTS, D, D_EXPERT), D ** -0.5),
        "exp_w3": nrm(ks[21], (L, N_EXPERTS, D, D_EXPERT), D ** -0.5),
        "exp_w2": nrm(ks[22], (L, N_EXPERTS, D_EXPERT, D), D_EXPERT ** -0.5 * DN_BETA),
        "ln2_g": 1.0 + nrm(ks[23], (L, D), 0.02),
        "ln2_b": nrm(ks[24], (L, D), 0.02),
    }


def reference(x, positions, w_in, conv_w, conv_b, conv_ln_g, conv_ln_b, da_lambda, da_norm_g,
              hg_lb, hg_norm_g, w_branch, b_gate, w_out, ln1_g, ln1_b, router_group,
              router_group_b, router_expert, router_expert_b, exp_w1, exp_w3, exp_w2,
              ln2_g, ln2_b):
    B, S, D = x.shape
    cos, sin = rotary_tables(positions, x.dtype)
    lb_tab = jnp.cumsum(jax.nn.softmax(hg_lb.astype(jnp.float32), axis=0), axis=0)
    lb_tab = lb_tab - lb_tab[0:1]
    for l in range(DEPTH):
        h = x @ w_in[l]
        a_in, q_a, k_a, v_a, q_h, f_h, i_h, g_h, gate_logits = jnp.split(h, SPLIT_IDX, axis=-1)
        y_a = conformer_conv(a_in, conv_w[l], conv_b[l], conv_ln_g[l], conv_ln_b[l])
        y_b = diff_attention(q_a, k_a, v_a, cos, sin, da_lambda[l], da_norm_g[l], l)
        y_c = hgrn2(q_h, f_h, i_h, g_h, lb_tab[l], hg_norm_g[l])
        ys = jnp.stack([y_a, y_b, y_c], axis=2)
        proj = jnp.einsum("bsnc,ncd->bsnd", ys, w_branch[l])
        gates = jax.nn.sigmoid(gate_logits.reshape(B, S, N_BRANCH, D) + b_gate[l])
        mix = jnp.sum(gates * proj, axis=2) @ w_out[l]
        x = layer_norm(DN_ALPHA * x + mix, ln1_g[l], ln1_b[l])
        moe = hier_moe(x, router_group[l], router_group_b[l], router_expert[l], router_expert_b[l],
                       exp_w1[l], exp_w3[l], exp_w2[l])
        x = layer_norm(DN_ALPHA * x + moe, ln2_g[l], ln2_b[l])
    return x
```

```python
import math
from contextlib import ExitStack

import numpy as np
import concourse.bass as bass
import concourse.mybir as mybir
from concourse.bass_utils import run_bass_kernel_spmd

F32 = mybir.dt.float32
BF16 = mybir.dt.bfloat16
I32 = mybir.dt.int32
AF = mybir.ActivationFunctionType
ALU = mybir.AluOpType
AX = mybir.AxisListType

D = 1024
SEQ = 2048
NB = 32
NCORES = 8
SPC = NB // NCORES
T = 512
NBLK = SEQ // T
KC = D // 128
IN_COLS = 7680
NEXP = 32
DEXP = 512
CONV_K = 31
HALO = CONV_K - 1
DN_ALPHA = (2 * 2) ** 0.25
LN_EPS = 1e-5
ROPE_THETA = 500000.0
CLAMP = 40.0

_off = 0
def _fld(n):
    global _off
    o = _off
    _off += n
    return o
S_CONVW = _fld(4 * CONV_K)
S_CONVB = _fld(4)
S_CLNG = _fld(4)
S_CLNB = _fld(4)
S_DAG = _fld(1)
S_HGG = _fld(1)
S_BGATE = _fld(24)
S_LN1G = _fld(8)
S_LN1B = _fld(8)
S_LN2G = _fld(8)
S_LN2B = _fld(8)
S_WR = _fld(8 * 36)
S_RB = _fld(36)
S_LBP = _fld(8)
NS = _off
C_ID = 0
C_M1 = 128
C_M2 = 256
C_M3 = 384
C_CM = 512
C_INVF = 516
C_SGN = 517
C_ONES = 518
NCA = 646
CB_ID = 0
CB_PERM = 128
CB_M3 = 256
CB_ONES = 384
CB_MASK = 512
NCB = 512 + 2048


class Buf:
    __slots__ = ("name", "w", "r", "excl")

    def __init__(self, name, excl=False):
        self.name = name
        self.w = None
        self.r = {}
        self.excl = excl


class Eng:
    def __init__(self, name, handle, sem, is_pe=False):
        self.name = name
        self.h = handle
        self.sem = sem
        self.key = "s_" + name
        self.count = 0
        self.prog = []
        self.waited = {}
        self.is_pe = is_pe
        self.dma_i = 0
        self.pending_noinc = False


class FW:
    NDMA = 8

    def __init__(self, nc, stack):
        self.nc = nc
        self.sems = {}

        def mk(name):
            s = stack.enter_context(nc.semaphore(name))
            self.sems[name] = s
            return s
        self.pe = Eng("pe", nc.tensor, mk("s_pe"), is_pe=True)
        self.act = Eng("act", nc.scalar, mk("s_act"))
        self.dve = Eng("dve", nc.vector, mk("s_dve"))
        self.pool = Eng("pool", nc.gpsimd, mk("s_pool"))
        self.sp = Eng("sp", nc.sync, mk("s_sp"))
        self.engs = [self.pe, self.act, self.dve, self.pool, self.sp]
        self.dsem = {}
        for e in (self.sp, self.pool):
            self.dsem[e.name] = [mk(f"d_{e.name}{i}") for i in range(self.NDMA)]
        self.n_ops = 0

    def _deps(self, eng, reads, writes):
        need = {}
        for b in reads:
            if b.w is not None:
                k, v = b.w
                if need.get(k, 0) < v:
                    need[k] = v
            if b.excl:
                for k, v in b.r.items():
                    if k != eng.key and need.get(k, 0) < v:
                        need[k] = v
        for b in writes:
            if b.w is not None:
                k, v = b.w
                if need.get(k, 0) < v:
                    need[k] = v
            for k, v in b.r.items():
                if need.get(k, 0) < v:
                    need[k] = v
        waits = []
        for k, v in need.items():
            if eng.is_pe and k == eng.key:
                continue
            if eng.waited.get(k, 0) >= v:
                continue
            eng.waited[k] = v
            waits.append((k, v))
        return waits

    def op(self, eng, fn, reads=(), writes=(), inc=True):
        waits = self._deps(eng, reads, writes)
        if inc:
            eng.count += 1
            seq = eng.count
            eng.pending_noinc = False
        else:
            seq = eng.count + 1
            eng.pending_noinc = True
        key = eng.key
        eng.prog.append((waits, fn, 1 if inc else 0, None))
        for b in reads:
            if b.r.get(key, 0) < seq:
                b.r[key] = seq
        for b in writes:
            b.w = (key, seq)
            b.r = {}
        self.n_ops += 1

    def dma(self, eng, fn, reads=(), writes=()):
        ring = self.dsem[eng.name]
        i = eng.dma_i
        eng.dma_i += 1
        r = i % self.NDMA
        key = ("d", eng.name, r)
        waits = self._deps(eng, reads, writes)
        prev = i // self.NDMA
        if prev > 0 and eng.waited.get(key, 0) < prev * 16:
            eng.waited[key] = prev * 16
            waits.append((key, prev * 16))
        val = (prev + 1) * 16
        eng.prog.append((waits, fn, 16, ring[r]))
        for b in reads:
            if b.r.get(key, 0) < val:
                b.r[key] = val
        for b in writes:
            b.w = (key, val)
            b.r = {}
        self.n_ops += 1

    def sem_of(self, key):
        if isinstance(key, tuple):
            return self.dsem[key[1]][key[2]]
        return self.sems[key]

    def finish(self, final_bufs):
        nc = self.nc
        waits = self._deps(self.sp, final_bufs, [])
        self.sp.prog.append((waits, None, 0, None))
        for e in self.engs:
            if e.pending_noinc:
                raise RuntimeError(f"engine {e.name} ends with a non-inc instruction")
        fw = self

        def runner(e):
            def run(h):
                for waits, fn, inc, dsem in e.prog:
                    for k, v in waits:
                        h.wait_ge(fw.sem_of(k), v)
                    if fn is None:
                        continue
                    ins = fn()
                    if inc == 1:
                        ins.then_inc(e.sem, 1)
                    elif inc == 16:
                        ins.then_inc(dsem, 16)
            return run
        with nc.Block() as block:
            block.tensor(runner(self.pe))
            block.scalar(runner(self.act))
            block.vector(runner(self.dve))
            block.gpsimd(runner(self.pool))
            block.sync(runner(self.sp))


class Pool:
    def __init__(self, items):
        self.free = list(items)

    def get(self):
        if not self.free:
            raise RuntimeError("pool exhausted")
        return self.free.pop(0)

    def put(self, *xs):
        for x in xs:
            assert x not in self.free
            self.free.append(x)

    def get_run(self, n):
        fs = sorted(self.free)
        for a in fs:
            if all((a + i) in self.free for i in range(n)):
                for i in range(n):
                    self.free.remove(a + i)
                return a
        raise RuntimeError(f"no run of {n} free slots: {fs}")

    def put_run(self, a, n):
        self.put(*range(a, a + n))


def build_program(layers=(0, 1), n_seq=SPC, n_blk=NBLK, n_exp=NEXP, taps=None, stop=99):
    taps = taps or {}
    import os
    SKIP = os.environ.get('SKIP', '')
    nc = bass.Bass("TRN2", target_bir_lowering=False)
    dram = {}

    def din(name, shape, dt=F32):
        dram[name] = nc.dram_tensor(name, list(shape), dt, kind="ExternalInput").ap()
        return dram[name]

    x_d = din("x", [n_seq, SEQ, D])
    pos_d = din("positions", [n_seq, SEQ], I32)
    w_in_d = din("w_in", [2, D, IN_COLS])
    w_br_d = din("w_branch", [2, 3, 512, D])
    w_out_d = din("w_out", [2, D, D])
    w1_d = din("exp_w1", [2, NEXP, D, DEXP])
    w3_d = din("exp_w3", [2, NEXP, D, DEXP])
    w2_d = din("exp_w2", [2, NEXP, DEXP, D])
    small_d = din("small", [2, 128, NS])
    lbrow_d = din("lbrow", [2, 512])
    lam_d = din("lamrow", [2, 256])
    ca_d = din("constA", [128, NCA])
    cb_d = din("constB", [128, NCB])
    out_d = nc.dram_tensor("out", [n_seq, SEQ, D], F32, kind="ExternalOutput").ap()
    tap_d = {}
    for name, (shape, dt) in TAP_SHAPES.items():
        if name in taps:
            tap_d[name] = nc.dram_tensor("tap_" + name, list(shape), dt, kind="ExternalOutput").ap()

    with ExitStack() as st:
        fw = FW(nc, st)
        pe, act, dve, pool, sp = fw.pe, fw.act, fw.dve, fw.pool, fw.sp

        def sb(name, shape, dt=F32):
            return st.enter_context(nc.sbuf_tensor("sb_" + name, list(shape), dt))

        constA = sb("constA", [128, NCA]); b_constA = Buf("constA")
        cbf = sb("cbf", [128, NCB], BF16); b_cbf = Buf("cbf")
        small = sb("small", [128, 2, NS]); b_small = Buf("small")
        omlb_p = sb("omlb_p", [128, 2, 4]); omlb_row = sb("omlb_row", [128, 512]); b_omlb = Buf("omlb")
        lam_t = sb("lam_t", [128, 2, 4]); b_lam = Buf("lam")
        dagp = sb("dagp", [128, 2]);
        epst = sb("epst", [128, 1]); b_eps = Buf("eps")
        xres = sb("xres", [128, KC, T]); b_xres = [Buf(f"xres{i}") for i in range(KC)]
        xbf = sb("xbf", [128, KC, T], BF16); b_xbf = [Buf(f"xbf{i}") for i in range(KC)]
        KT = [sb(f"KT{l}", [128, 4, SEQ], BF16) for l in range(2)]
        b_KT = [[[Buf(f"KT{l}_{h}_{j}") for j in range(NBLK)] for h in range(4)] for l in range(2)]
        Vh = [sb(f"V{l}", [128, SEQ // 128, 512], BF16) for l in range(2)]
        b_V = [[Buf(f"V{l}_{t}") for t in range(SEQ // 128)] for l in range(2)]
        hst = [sb(f"hst{l}", [128, 4, 128]) for l in range(2)]; b_hst = [Buf(f"hst{l}") for l in range(2)]
        ubuf = [sb(f"ubuf{l}", [128, 4, HALO + T], BF16) for l in range(2)]
        b_ubuf = [[Buf(f"ubuf{l}_{c}") for c in range(4)] for l in range(2)]
        ropeC = sb("ropeC", [128, T]); ropeS = sb("ropeS", [128, T]); b_rope = Buf("rope")
        dec_t = sb("dec_t", [128, 4, 16]); b_dec = Buf("dec")
        comb = sb("comb", [128, 4, 32]); b_comb = Buf("comb")
        NW = 4
        wsl = [sb(f"wsl{i}", [128, 4096], BF16) for i in range(NW)]
        b_wsl = [Buf(f"wsl{i}") for i in range(NW)]
        print('sbuf remaining', nc.sbuf_bytes_remaining, flush=True)
        NSLOT = (nc.sbuf_bytes_remaining - 2048) // 2048
        print('NSLOT', NSLOT, flush=True)
        NSLOT = min(NSLOT, 40)
        arena = sb("arena", [128, NSLOT, 512])
        b_ar = [Buf(f"ar{i}") for i in range(NSLOT)]
        psum = [st.enter_context(nc.psum_tensor(f"ps{i}", [128, 512], F32)) for i in range(8)]
        b_ps = [Buf(f"ps{i}", excl=True) for i in range(8)]
        banks = Pool(range(8))
        slots = Pool(range(NSLOT))

        ident = constA[:, C_ID:C_ID + 128]
        M1 = constA[:, C_M1:C_M1 + 128]
        M2 = constA[:, C_M2:C_M2 + 128]
        M3 = constA[:, C_M3:C_M3 + 128]
        onesf = constA[:, C_ONES:C_ONES + 128]
        identb = cbf[:, CB_ID:CB_ID + 128]
        permb = cbf[:, CB_PERM:CB_PERM + 128]
        M3b = cbf[:, CB_M3:CB_M3 + 128]
        onesb = cbf[:, CB_ONES:CB_ONES + 128]

        def A(i):
            return arena[:, i, :]

        def AB(i):
            return arena[:, i, :].bitcast(BF16)

        def mm(out, lhsT, rhs, start, stop, R, W, inc=True):
            fw.op(pe, lambda: nc.tensor.matmul(out, lhsT, rhs, start=start, stop=stop), R, W, inc=inc)

        def actf(out, in_, func, R, W, bias=None, scale=None):
            kw = {}
            if bias is not None:
                kw["bias"] = bias
            if scale is not None:
                kw["scale"] = scale
            fw.op(act, lambda: nc.scalar.activation(out=out, in_=in_, func=func, **kw), R, W)

        def ttop(eng, out, a, b, op, R, W):
            fw.op(eng, lambda: eng.h.tensor_tensor(out, a, b, op), R, W)

        def tsop(eng, out, a, s1, s2, op0, op1, R, W):
            if s2 is None:
                fw.op(eng, lambda: eng.h.tensor_scalar(out, a, s1, None, op0), R, W)
            else:
                fw.op(eng, lambda: eng.h.tensor_scalar(out, a, s1, s2, op0, op1), R, W)

        def sttop(eng, out, a, s, b, op0, op1, R, W):
            fw.op(eng, lambda: eng.h.scalar_tensor_tensor(out, a, s, b, op0, op1), R, W)

        def cpy(eng, out, in_, R, W):
            if eng is act:
                fw.op(act, lambda: nc.scalar.activation(out=out, in_=in_, func=AF.Identity), R, W)
            else:
                fw.op(eng, lambda: eng.h.tensor_copy(out, in_), R, W)

        def tap(name, key, src_ap, R):
            if name in taps and taps[name] == key:
                b = Buf("tap_" + name)
                dst = tap_d[name]
                fw.dma(sp, (lambda dst=dst, src_ap=src_ap: nc.sync.dma_start(out=dst, in_=src_ap)), R, [b])
                final_bufs.append(b)

        final_bufs = []

        wlist = []
        for s in range(n_seq):
            for j in range(n_blk):
                for l in layers:
                    def inblk(b, l=l):
                        return (w_in_d[l][:, 512 * b:512 * b + 512].rearrange("(kc p) c -> p kc c", p=128), 8)
                    for b in range(9):
                        wlist.append(inblk(b))
                    for n in range(3):
                        wlist.append((w_br_d[l][n].rearrange("(kc p) c -> p kc c", p=128), 4))
                        wlist.append(inblk(9 + 2 * n))
                        wlist.append(inblk(10 + 2 * n))
                    for hf in range(2):
                        wlist.append((w_out_d[l][:, 512 * hf:512 * hf + 512].rearrange("(kc p) c -> p kc c", p=128), 8))
                    for e in range(n_exp):
                        wlist.append((w1_d[l][e].rearrange("(kc p) c -> p kc c", p=128), 8))
                        wlist.append((w3_d[l][e].rearrange("(kc p) c -> p kc c", p=128), 8))
                        wlist.append((w2_d[l][e].rearrange("(kc p) c -> p kc c", p=128), 4))
        wstate = {"issued": 0, "consumed": 0, "ready": []}
        wfree = Pool(range(NW))

        def w_issue():
            while wfree.free and wstate["issued"] < len(wlist):
                i = wfree.get()
                src, nk = wlist[wstate["issued"]]
                wstate["issued"] += 1
                dst = wsl[i][:].rearrange("p (k c) -> p k c", k=nk)
                fw.dma(pool, (lambda dst=dst, src=src: nc.gpsimd.dma_start(out=dst, in_=src)), [], [b_wsl[i]])
                wstate["ready"].append((i, nk))

        def w_get(nk_expect):
            w_issue()
            i, nk = wstate["ready"].pop(0)
            assert nk == nk_expect, (nk, nk_expect, wstate["consumed"])
            wstate["consumed"] += 1
            return i, wsl[i][:].rearrange("p (k c) -> p k c", k=nk)

        def w_put(i):
            wfree.put(i)
            w_issue()

        fw.dma(sp, lambda: nc.sync.dma_start(out=constA[:], in_=ca_d), [], [b_constA])
        fw.dma(sp, lambda: nc.sync.dma_start(out=small[:], in_=small_d.rearrange("l p n -> p l n")), [], [b_small])
        fw.dma(sp, lambda: nc.sync.dma_start(out=omlb_row[:], in_=lbrow_d[1:2, :].partition_broadcast(128)), [], [b_omlb])
        s0 = slots.get_run(5)
        stg = arena[:, s0:s0 + 5, :].rearrange("p a b -> p (a b)")
        stgb = [b_ar[s0 + i] for i in range(5)]
        fw.dma(sp, lambda: nc.sync.dma_start(out=stg[:, 0:NCB], in_=cb_d), [], stgb)
        cpy(dve, cbf[:], stg[:, 0:NCB], stgb, [b_cbf])
        slots.put_run(s0, 5)
        fw.op(dve, lambda: nc.vector.memset(epst[:], LN_EPS), [], [b_eps])
        s0 = slots.get()
        fw.dma(sp, lambda: nc.sync.dma_start(out=A(s0), in_=lbrow_d[0:1, :].partition_broadcast(128)), [], [b_ar[s0]])
        ttop(dve, omlb_row[:], A(s0), omlb_row[:], ALU.subtract, [b_omlb, b_ar[s0]], [b_omlb])
        actf(omlb_row[:], omlb_row[:], AF.Sigmoid, [b_omlb], [b_omlb])
        slots.put(s0)
        lbp = small[:, 0, S_LBP:S_LBP + 8].rearrange("p (l h) -> p l h", l=2)
        ttop(dve, omlb_p[:, 1, :], lbp[:, 0, :], lbp[:, 1, :], ALU.subtract, [b_small], [b_omlb])
        actf(omlb_p[:, 1, :], omlb_p[:, 1, :], AF.Sigmoid, [b_omlb], [b_omlb])
        fw.op(dve, lambda: nc.vector.memset(omlb_p[:, 0, :], 1.0), [], [b_omlb])
        sl_a = slots.get(); sl_b = slots.get(); sl_c = slots.get()
        rt = A(sl_a)[:, 0:256].rearrange("p (a b) -> p a b", a=4); b_rt = b_ar[sl_a]
        rt2 = A(sl_b)[:, 0:256].rearrange("p (a b) -> p a b", a=4); b_rt2 = b_ar[sl_b]
        for l in (range(2) if 'L' not in SKIP else []):
            lam_init = 0.8 - 0.6 * math.exp(-0.3 * l)
            lp = A(sl_c)[:, 0:256]
            lsrc = lam_d[l:l + 1, :].partition_broadcast(128)
            fw.dma(sp, (lambda lp=lp, lsrc=lsrc: nc.sync.dma_start(out=lp, in_=lsrc)), [], [b_ar[sl_c]])
            ttop(dve, rt[:, 0, :], lp[:, 0:64], lp[:, 64:128], ALU.mult, [b_ar[sl_c]], [b_rt])
            ttop(dve, rt[:, 1, :], lp[:, 128:192], lp[:, 192:256], ALU.mult, [b_ar[sl_c]], [b_rt])
            fw.op(dve, lambda: nc.vector.tensor_reduce(out=rt2[:, 0, 0:2], in_=rt[:, 0:2, :], axis=AX.X, op=ALU.add), [b_rt], [b_rt2])
            actf(rt2[:, 0, 2:4], rt2[:, 0, 0:2], AF.Exp, [b_rt2], [b_rt2])
            ttop(dve, lam_t[:, l, 0:1], rt2[:, 0, 3:4], rt2[:, 0, 2:3], ALU.subtract, [b_rt2], [b_lam])
            tsop(dve, lam_t[:, l, 0:1], lam_t[:, l, 0:1], -lam_init, None, ALU.add, None, [b_lam], [b_lam])
            tsop(dve, dagp[:, l:l + 1], small[:, l, S_DAG:S_DAG + 1], 1.0 - lam_init, None, ALU.mult, None, [b_small], [b_lam])
        slots.put(sl_a, sl_b, sl_c)

        for s in range(n_seq):
            for j in range(n_blk):
                t0 = j * T
                for tt in (range(4) if 'X' not in SKIP else []):
                    sa = slots.get_run(2); sb_ = sa + 1
                    xt = arena[:, sa:sa + 2, :].rearrange("p a b -> p (a b)")
                    xsrc = x_d[s, t0 + tt * 128:t0 + (tt + 1) * 128, :]
                    fw.dma(sp, (lambda xt=xt, xsrc=xsrc: nc.sync.dma_start(out=xt, in_=xsrc)),
                           [], [b_ar[sa], b_ar[sb_]])
                    for half in range(2):
                        pb = banks.get()
                        for q in range(4):
                            dc = half * 4 + q
                            mm(psum[pb][:, q * 128:(q + 1) * 128], xt[:, dc * 128:(dc + 1) * 128], ident, True, True,
                               [b_ar[sa], b_ar[sb_], b_constA], [b_ps[pb]], inc=(q == 3))
                        for q in range(4):
                            dc = half * 4 + q
                            cpy(dve if (q % 2 == 0 or 'v' in SKIP) else act, xres[:, dc, tt * 128:(tt + 1) * 128], psum[pb][:, q * 128:(q + 1) * 128],
                                [b_ps[pb]], [b_xres[dc]])
                        banks.put(pb)
                    slots.put(sa, sb_)
                for dc in (range(KC) if ('X' not in SKIP and 'b' not in SKIP) else []):
                    cpy(act if dc % 2 == 0 else dve, xbf[:, dc, :], xres[:, dc, :], [b_xres[dc]], [b_xbf[dc]])
                sa = slots.get(); sb_ = slots.get(); sc = slots.get()
                psrc = pos_d[s:s + 1, t0:t0 + T].partition_broadcast(128)
                fw.dma(sp, (lambda sa=sa, psrc=psrc: nc.sync.dma_start(out=A(sa).bitcast(I32), in_=psrc)),
                       [], [b_ar[sa]])
                cpy(dve, A(sb_), A(sa).bitcast(I32), [b_ar[sa]], [b_ar[sb_]])
                tsop(dve, A(sb_), A(sb_), constA[:, C_INVF:C_INVF + 1], None, ALU.mult, None, [b_ar[sb_], b_constA], [b_ar[sb_]])
                for which, dst in (((0, ropeS), (1, ropeC)) if 'R' not in SKIP else []):
                    if which == 1:
                        tsop(dve, A(sb_), A(sb_), float(np.pi / 2), None, ALU.add, None, [b_ar[sb_]], [b_ar[sb_]])
                    tsop(dve, A(sa).bitcast(I32), A(sb_), float(1.0 / (2 * np.pi)), None, ALU.mult, None, [b_ar[sb_]], [b_ar[sa]])
                    cpy(dve, A(sc), A(sa).bitcast(I32), [b_ar[sa]], [b_ar[sc]])
                    sttop(dve, A(sc), A(sc), float(-2 * np.pi), A(sb_), ALU.mult, ALU.add, [b_ar[sc], b_ar[sb_]], [b_ar[sc]])
                    if which == 0:
                        actf(dst[:], A(sc), AF.Sin, [b_ar[sc], b_constA], [b_rope], scale=constA[:, C_SGN:C_SGN + 1])
                    else:
                        actf(dst[:], A(sc), AF.Sin, [b_ar[sc]], [b_rope])
                slots.put(sa, sb_, sc)

                for l in layers:
                    key = (s, j, l)
                    sm = small[:, l, :]
                    emit_layer(locals())
                for tt in (range(4) if 'O' not in SKIP else []):
                    sa = slots.get_run(2); sb_ = sa + 1
                    ot = arena[:, sa:sa + 2, :].rearrange("p a b -> p (a b)")
                    for half in range(2):
                        pb = banks.get()
                        for q in range(4):
                            dc = half * 4 + q
                            mm(psum[pb][:, q * 128:(q + 1) * 128], xres[:, dc, tt * 128:(tt + 1) * 128], ident, True, True,
                               [b_xres[dc], b_constA], [b_ps[pb]], inc=(q == 3))
                        cpy(dve if half == 0 else act, ot[:, half * 512:(half + 1) * 512], psum[pb][:, :], [b_ps[pb]], [b_ar[sa + half]])
                        banks.put(pb)
                    bo = Buf("out")
                    odst = out_d[s, t0 + tt * 128:t0 + (tt + 1) * 128, :]
                    fw.dma(sp, (lambda ot=ot, odst=odst: nc.sync.dma_start(out=odst, in_=ot)),
                           [b_ar[sa], b_ar[sb_]], [bo])
                    final_bufs.append(bo)
                    slots.put(sa, sb_)
        assert stop < 99 or wstate["consumed"] == len(wlist), (wstate["consumed"], len(wlist))
        fw.finish(final_bufs)
    return nc


TAP_SHAPES = {
    "ya": ((128, 4, T), BF16), "yb": ((128, 4, T), BF16), "yc": ((128, 4, T), BF16),
    "x1": ((128, KC, T), F32), "x2": ((128, KC, T), F32), "mix": ((128, KC, T), F32),
    "q": ((128, 4, T), BF16), "acc": ((128, 4, D), F32), "comb": ((128, 4, 32), F32),
    "oh": ((128, 4, T), F32),
}


def emit_layer(E):
    g = E
    nc, fw = g["nc"], g["fw"]
    pe, act, dve, pool, sp = g["pe"], g["act"], g["dve"], g["pool"], g["sp"]
    mm, actf, ttop, tsop, sttop, cpy, tap = g["mm"], g["actf"], g["ttop"], g["tsop"], g["sttop"], g["cpy"], g["tap"]
    banks, slots, psum, b_ps, arena, b_ar = g["banks"], g["slots"], g["psum"], g["b_ps"], g["arena"], g["b_ar"]
    A, AB = g["A"], g["AB"]
    xres, b_xres, xbf, b_xbf = g["xres"], g["b_xres"], g["xbf"], g["b_xbf"]
    constA, b_constA, cbf, b_cbf, small, b_small = g["constA"], g["b_constA"], g["cbf"], g["b_cbf"], g["small"], g["b_small"]
    ident, M1, M2, M3, onesf, identb, permb, M3b, onesb = (g[k] for k in ("ident", "M1", "M2", "M3", "onesf", "identb", "permb", "M3b", "onesb"))
    epst, b_eps = g["epst"], g["b_eps"]
    w_get, w_put = g["w_get"], g["w_put"]
    l, j, s, key, sm = g["l"], g["j"], g["s"], g["key"], g["sm"]
    n_exp = g["n_exp"]
    t0 = j * T
    KTl, b_KTl, Vl, b_Vl = g["KT"][l], g["b_KT"][l], g["Vh"][l], g["b_V"][l]
    hstl, b_hstl = g["hst"][l], g["b_hst"][l]
    ub, b_ub = g["ubuf"][l], g["b_ubuf"][l]
    ropeC, ropeS, b_rope = g["ropeC"], g["ropeS"], g["b_rope"]
    dec_t, b_dec = g["dec_t"], g["b_dec"]
    comb, b_comb = g["comb"], g["b_comb"]
    omlb_p, omlb_row, b_omlb, lam_t, b_lam, dagp = g["omlb_p"], g["omlb_row"], g["b_omlb"], g["lam_t"], g["b_lam"], g["dagp"]
    XB = b_xbf
    stop = g["stop"]
    if stop <= 0:
        return

    def fm_proj(ws, oc, pb):
        for kc in range(KC):
            mm(psum[pb][:, :], ws[1][:, kc, oc * 128:(oc + 1) * 128], xbf[:, kc, :], kc == 0, kc == KC - 1,
               [g["b_wsl"][ws[0]], XB[kc]], [b_ps[pb]], inc=(kc == KC - 1))

    def tm_proj(ws, tt, pb):
        for kc in range(KC):
            mm(psum[pb][:, :], xbf[:, kc, tt * 128:(tt + 1) * 128], ws[1][:, kc, :], kc == 0, kc == KC - 1,
               [g["b_wsl"][ws[0]], XB[kc]], [b_ps[pb]], inc=(kc == KC - 1))

    def bc_stats(srcs, R, n_total):
        p1 = banks.get(); p2 = banks.get()
        n = len(srcs)
        sq = [slots.get(), slots.get()]
        for i, (ap, rb) in enumerate(zip(srcs, R)):
            mm(psum[p1][:, :], onesf, ap, i == 0, i == n - 1, rb + [b_constA], [b_ps[p1]], inc=(i == n - 1))
        for i, (ap, rb) in enumerate(zip(srcs, R)):
            q = sq[i % 2]
            fw.op(act, (lambda q=q, ap=ap: nc.scalar.activation(out=A(q), in_=ap, func=AF.Square)), rb, [b_ar[q]])
            mm(psum[p2][:, :], onesf, A(q), i == 0, i == n - 1, [b_ar[q], b_constA], [b_ps[p2]], inc=True)
        slots.put(*sq)
        m = slots.get(); r = slots.get()
        fw.op(act, lambda: nc.scalar.mul(A(m), psum[p1][:, :], 1.0 / n_total), [b_ps[p1]], [b_ar[m]])
        ttop(dve, A(r), A(m), A(m), ALU.mult, [b_ar[m]], [b_ar[r]])
        sttop(dve, A(r), psum[p2][:, :], 1.0 / n_total, A(r), ALU.mult, ALU.subtract, [b_ps[p2], b_ar[r]], [b_ar[r]])
        actf(A(r), A(r), AF.Sqrt, [b_ar[r], b_eps], [b_ar[r]], bias=epst[:, 0:1], scale=1.0)
        fw.op(dve, lambda: nc.vector.reciprocal(A(r), A(r)), [b_ar[r]], [b_ar[r]])
        banks.put(p1, p2)
        return m, r

    _y0 = slots.get_run(6)
    ysl = list(range(_y0, _y0 + 6))

    def ytile(n):
        return arena[:, ysl[2 * n]:ysl[2 * n] + 2, :].bitcast(BF16).rearrange("p a (h t) -> p (a h) t", h=2)

    def ybufs(n, c):
        return [b_ar[ysl[2 * n + c // 2]]]
    ya, yb, yc = ytile(0), ytile(1), ytile(2)

    wa1 = w_get(8); wa2 = w_get(8)
    if j == 0:
        for c in range(4):
            fw.op(dve, (lambda c=c: nc.vector.memset(ub[:, c, 0:HALO], 0.0)), [], [b_ub[c]])
    for c in range(4):
        pa = banks.get(); pg = banks.get()
        fm_proj(wa1, c, pa)
        fm_proj(wa2, c, pg)
        sg = slots.get()
        actf(A(sg), psum[pg][:, :], AF.Sigmoid, [b_ps[pg]], [b_ar[sg]])
        ttop(dve, ub[:, c, HALO:HALO + T], psum[pa][:, :], A(sg), ALU.mult, [b_ps[pa], b_ar[sg]], [b_ub[c]])
        slots.put(sg); banks.put(pa, pg)
    w_put(wa1[0]); w_put(wa2[0])
    ycs = [slots.get() for _ in range(4)]
    dgs = [list(range(a_, a_ + 4)) for a_ in (slots.get_run(4), slots.get_run(4))]
    for c in range(4):
        dsl = dgs[c % 2]
        assert dsl == list(range(dsl[0], dsl[0] + 4))
        dg = arena[:, dsl[0]:dsl[0] + 4, :].bitcast(BF16).rearrange("p a b -> p (a b)")[:, 0:CONV_K * 128].rearrange("p (k m) -> p k m", k=CONV_K)
        dgb = [b_ar[i] for i in dsl]
        wv = sm[:, S_CONVW + c * CONV_K:S_CONVW + (c + 1) * CONV_K]
        fw.op(dve, (lambda dg=dg, wv=wv: nc.vector.tensor_tensor(dg, identb.unsqueeze(1).to_broadcast([128, CONV_K, 128]),
                                                                   wv.unsqueeze(2).to_broadcast([128, CONV_K, 128]), ALU.mult)),
              [b_cbf, b_small], dgb)
        pc = banks.get()
        for k in range(CONV_K):
            mm(psum[pc][:, :], dg[:, k, :], ub[:, c, k:k + T], k == 0, k == CONV_K - 1, dgb + [b_ub[c]], [b_ps[pc]], inc=(k == CONV_K - 1))
        actf(A(ycs[c]), psum[pc][:, :], AF.Identity, [b_ps[pc], b_small], [b_ar[ycs[c]]], bias=sm[:, S_CONVB + c:S_CONVB + c + 1], scale=1.0)
        banks.put(pc)
        cpy(pool, ub[:, c, 0:HALO], ub[:, c, T:T + HALO], [b_ub[c]], [b_ub[c]])
    for d_ in dgs:
        slots.put(*d_)
    m_, r_ = bc_stats([A(i) for i in ycs], [[b_ar[i]] for i in ycs], 512.0)
    for c in range(4):
        ttop(dve, A(ycs[c]), A(ycs[c]), A(m_), ALU.subtract, [b_ar[ycs[c]], b_ar[m_]], [b_ar[ycs[c]]])
        ttop(dve, A(ycs[c]), A(ycs[c]), A(r_), ALU.mult, [b_ar[ycs[c]], b_ar[r_]], [b_ar[ycs[c]]])
        actf(ya[:, c, :], A(ycs[c]), AF.Silu, [b_ar[ycs[c]], b_small], ybufs(0, c),
             bias=sm[:, S_CLNB + c:S_CLNB + c + 1], scale=sm[:, S_CLNG + c:S_CLNG + c + 1])
    slots.put(m_, r_); slots.put(*ycs)
    tap("ya", key, ya, ybufs(0, 0) + ybufs(0, 2))
    if stop <= 1:
        return

    wq = w_get(8)
    _q0 = slots.get_run(2)
    qsl = [_q0, _q0 + 1]
    qT = arena[:, qsl[0]:qsl[0] + 2, :].bitcast(BF16).rearrange("p a (h t) -> p (a h) t", h=2)
    qTb = lambda h: [b_ar[qsl[h // 2]]]

    def rope_chunk(ws, h, dst_ap, dstR):
        pq = banks.get(); pp = banks.get()
        fm_proj(ws, h, pq)
        tb = slots.get(); t1 = slots.get(); t2 = slots.get()
        cpy(act, AB(tb)[:, 0:T], psum[pq][:, :], [b_ps[pq]], [b_ar[tb]])
        mm(psum[pp][:, :], permb, AB(tb)[:, 0:T], True, True, [b_cbf, b_ar[tb]], [b_ps[pp]])
        ttop(dve, A(t1), psum[pq][:, :], ropeC[:], ALU.mult, [b_ps[pq], b_rope], [b_ar[t1]])
        ttop(dve, A(t2), psum[pp][:, :], ropeS[:], ALU.mult, [b_ps[pp], b_rope], [b_ar[t2]])
        ttop(dve, dst_ap, A(t1), A(t2), ALU.add, [b_ar[t1], b_ar[t2]], dstR)
        slots.put(tb, t1, t2); banks.put(pq, pp)
    for h in range(4):
        rope_chunk(wq, h, qT[:, h, :], qTb(h))
    w_put(wq[0])
    wk = w_get(8)
    for h in range(4):
        rope_chunk(wk, h, KTl[:, h, t0:t0 + T], [b_KTl[h][j]])
    w_put(wk[0])
    wv = w_get(8)
    for tt in range(4):
        pv = banks.get()
        tm_proj(wv, tt, pv)
        cpy(act if tt % 2 == 0 else dve, Vl[:, 4 * j + tt, :], psum[pv][:, :], [b_ps[pv]], [b_Vl[4 * j + tt]])
        banks.put(pv)
    w_put(wv[0])
    tap("q", key, qT, qTb(0) + qTb(2))
    nkb = 4 * (j + 1)
    ohs = [slots.get() for _ in range(4)]
    for h in range(4):
        po = [banks.get(), banks.get()]
        pS = [banks.get(), banks.get()]
        items = [(p, kb) for kb in range(nkb) for p in range(2)]
        ptr = [slots.get(), slots.get()]
        pbufs = [(ptr[i // 2], i % 2) for i in range(4)]
        sc_banks = {}

        def score(idx):
            p, kb = items[idx]
            pb = banks.get()
            sc_banks[idx] = pb
            mm(psum[pb][:, :], KTl[64 * p:64 * p + 64, h, kb * 128:(kb + 1) * 128], qT[64 * p:64 * p + 64, h, :], True, True,
               [b_KTl[h][kb // 4]] + qTb(h), [b_ps[pb]])
        score(0)
        for idx in range(len(items)):
            p, kb = items[idx]
            if idx + 1 < len(items):
                score(idx + 1)
            pb = sc_banks.pop(idx)
            sl, hf = pbufs[idx % 4]
            PT = AB(sl)[:, hf * T:(hf + 1) * T]
            actf(PT, psum[pb][:, :], AF.Exp, [b_ps[pb]], [b_ar[sl]], scale=0.125)
            banks.put(pb)
            if kb >= 4 * j:
                mk = cbf[:, CB_MASK + (kb - 4 * j) * 512:CB_MASK + (kb - 4 * j + 1) * 512]
                ttop(pool, PT, PT, mk, ALU.mult, [b_ar[sl], b_cbf], [b_ar[sl]])
            mm(psum[po[p]][:, :], Vl[:, kb, h * 128:(h + 1) * 128], PT, kb == 0, kb == nkb - 1, [b_Vl[kb], b_ar[sl]], [b_ps[po[p]]], inc=False)
            mm(psum[pS[p]][:, :], onesb, PT, kb == 0, kb == nkb - 1, [b_cbf, b_ar[sl]], [b_ps[pS[p]]], inc=True)
        slots.put(*ptr)
        r1 = slots.get(); r2 = slots.get()
        fw.op(dve, (lambda r1=r1, pS=pS: nc.vector.reciprocal(A(r1), psum[pS[0]][:, :])), [b_ps[pS[0]]], [b_ar[r1]])
        fw.op(dve, (lambda r2=r2, pS=pS: nc.vector.reciprocal(A(r2), psum[pS[1]][:, :])), [b_ps[pS[1]]], [b_ar[r2]])
        ttop(dve, A(r1), psum[po[0]][:, :], A(r1), ALU.mult, [b_ps[po[0]], b_ar[r1]], [b_ar[r1]])
        ttop(dve, A(r2), psum[po[1]][:, :], A(r2), ALU.mult, [b_ps[po[1]], b_ar[r2]], [b_ar[r2]])
        sttop(dve, A(ohs[h]), A(r2), lam_t[:, l, 0:1], A(r1), ALU.mult, ALU.add, [b_ar[r1], b_ar[r2], b_lam], [b_ar[ohs[h]]])
        slots.put(r1, r2); banks.put(*po); banks.put(*pS)
    for h in range(4):
        sq = slots.get(); pm = banks.get()
        fw.op(act, (lambda sq=sq, h=h: nc.scalar.activation(out=A(sq), in_=A(ohs[h]), func=AF.Square)), [b_ar[ohs[h]]], [b_ar[sq]])
        mm(psum[pm][:, :], onesf, A(sq), True, True, [b_ar[sq], b_constA], [b_ps[pm]])
        actf(A(sq), psum[pm][:, :], AF.Sqrt, [b_ps[pm], b_eps], [b_ar[sq]], bias=epst[:, 0:1], scale=1.0 / 128)
        fw.op(dve, (lambda sq=sq: nc.vector.reciprocal(A(sq), A(sq))), [b_ar[sq]], [b_ar[sq]])
        sttop(dve, yb[:, h, :], A(ohs[h]), dagp[:, l:l + 1], A(sq), ALU.mult, ALU.mult, [b_ar[ohs[h]], b_ar[sq], b_lam], ybufs(1, h))
        slots.put(sq); banks.put(pm)
    slots.put(*ohs); slots.put(*qsl)
    tap("yb", key, yb, ybufs(1, 0) + ybufs(1, 2))
    if stop <= 2:
        return

    def bf4(n0):
        return arena[:, n0:n0 + 2, :].bitcast(BF16).rearrange("p a (h t) -> p (a h) t", h=2)

    def two():
        return slots.get_run(2)
    s_qs, s_kT, s_qb, s_kd = (two() for _ in range(4))
    qs, kTt, qb, kdec = (bf4(x) for x in (s_qs, s_kT, s_qb, s_kd))
    B2 = lambda s0: [b_ar[s0], b_ar[s0 + 1]]
    Bh = lambda s0, h: [b_ar[s0 + h // 2]]
    attm = [slots.get(), slots.get()]
    wqh = w_get(8)
    for h in range(4):
        pb = banks.get()
        fm_proj(wqh, h, pb)
        actf(qs[:, h, :], psum[pb][:, :], AF.Silu, [b_ps[pb]], Bh(s_qs, h))
        banks.put(pb)
    w_put(wqh[0])
    wfh = w_get(8)
    for h in range(4):
        pb = banks.get(); tmp = slots.get()
        fm_proj(wfh, h, pb)
        actf(A(tmp), psum[pb][:, :], AF.Sigmoid, [b_ps[pb]], [b_ar[tmp]], scale=-1.0)
        tsop(dve, kTt[:, h, :], A(tmp), omlb_p[:, l, h:h + 1], None, ALU.mult, None, [b_ar[tmp], b_omlb], Bh(s_kT, h))
        banks.put(pb); slots.put(tmp)
    if j == 0:
        fw.op(dve, lambda: nc.vector.memset(hstl[:], 0.0), [], [b_hstl])
    v3 = lambda ap: ap.rearrange("p (h t) -> p h t", h=4)
    for tt in range(4):
        tsl = slice(tt * 128, (tt + 1) * 128)
        kt_ = slots.get(); lf_ = slots.get()
        pb = banks.get()
        tm_proj(wfh, tt, pb)
        actf(A(kt_), psum[pb][:, :], AF.Sigmoid, [b_ps[pb]], [b_ar[kt_]], scale=-1.0)
        banks.put(pb)
        if l == 1:
            ttop(dve, A(kt_), A(kt_), omlb_row[:], ALU.mult, [b_ar[kt_], b_omlb], [b_ar[kt_]])
        actf(A(lf_), A(kt_), AF.Ln, [b_ar[kt_]], [b_ar[lf_]], scale=-1.0, bias=1.0)
        pb = banks.get(); e2 = slots.get()
        mm(psum[pb][:, :], M2, A(lf_), True, True, [b_constA, b_ar[lf_]], [b_ps[pb]])
        actf(A(e2), psum[pb][:, :], AF.Exp, [b_ps[pb]], [b_ar[e2]])
        ttop(dve, kdec[:, tt, :], A(kt_), A(e2), ALU.mult, [b_ar[kt_], b_ar[e2]], Bh(s_kd, tt))
        banks.put(pb); slots.put(e2); slots.put(kt_)
        p1 = banks.get(); p3 = banks.get()
        for h in range(4):
            mm(psum[p1][:, h * 128:(h + 1) * 128], A(lf_)[:, h * 128:(h + 1) * 128], M1, True, True,
               [b_ar[lf_], b_constA], [b_ps[p1]], inc=(h == 3))
        for h in range(4):
            mm(psum[p3][:, h * 128:(h + 1) * 128], A(lf_)[:, h * 128:(h + 1) * 128], M3, True, True,
               [b_ar[lf_], b_constA], [b_ps[p3]], inc=(h == 3))
        slots.put(lf_)
        c1 = slots.get(); e1 = slots.get(); e3 = slots.get(); qk = slots.get()
        tsop(dve, A(c1), psum[p1][:, :], CLAMP, -CLAMP, ALU.min, ALU.max, [b_ps[p1]], [b_ar[c1]])
        actf(A(e1), A(c1), AF.Exp, [b_ar[c1]], [b_ar[e1]])
        actf(A(c1), A(c1), AF.Exp, [b_ar[c1]], [b_ar[c1]], scale=-1.0)
        actf(A(e3), psum[p3][:, :], AF.Exp, [b_ps[p3]], [b_ar[e3]])
        banks.put(p1, p3)
        qt = v3(AB(qk)[:, 0:512]); kt = v3(AB(qk)[:, 512:1024])
        ttop(dve, qt, qs[:, :, tsl], v3(A(e1)), ALU.mult, B2(s_qs) + [b_ar[e1]], [b_ar[qk]])
        ttop(dve, kt, kTt[:, :, tsl], v3(A(c1)), ALU.mult, B2(s_kT) + [b_ar[c1]], [b_ar[qk]])
        ttop(dve, qb[:, :, tsl], qs[:, :, tsl], v3(A(e3)), ALU.mult, B2(s_qs) + [b_ar[e3]], B2(s_qb))
        e3v = A(e3).rearrange("p (h c t) -> p h c t", h=4, c=4)[:, :, :, 31]
        cpy(dve, dec_t[:, :, 4 * tt:4 * tt + 4], e3v, [b_ar[e3]], [b_dec])
        slots.put(c1, e1, e3)
        pa = banks.get()
        for h in range(4):
            mm(psum[pa][:, h * 128:(h + 1) * 128], kt[:, h, :], qt[:, h, :], True, True, [b_ar[qk]], [b_ps[pa]], inc=(h == 3))
        slots.put(qk)
        asl = attm[tt // 2]
        am = AB(asl)[:, (tt % 2) * 512:(tt % 2) * 512 + 512].rearrange("p (h t) -> p h t", h=4)
        fw.op(dve, (lambda am=am, pa=pa: nc.vector.tensor_tensor(am, psum[pa][:, :].rearrange("p (h t) -> p h t", h=4),
                                                                  M3.unsqueeze(1).to_broadcast([128, 4, 128]), ALU.mult)),
              [b_ps[pa], b_constA], [b_ar[asl]])
        banks.put(pa)
    w_put(wfh[0])
    slots.put_run(s_qs, 2); slots.put_run(s_kT, 2)
    s_it, s_gs = two(), two()
    itok, gsil = bf4(s_it), bf4(s_gs)
    wih = w_get(8)
    for tt in range(4):
        pb = banks.get()
        tm_proj(wih, tt, pb)
        cpy(act if tt % 2 else dve, itok[:, tt, :], psum[pb][:, :], [b_ps[pb]], Bh(s_it, tt))
        banks.put(pb)
    w_put(wih[0])
    wgh = w_get(8)
    for h in range(4):
        pb = banks.get()
        fm_proj(wgh, h, pb)
        actf(gsil[:, h, :], psum[pb][:, :], AF.Silu, [b_ps[pb]], Bh(s_gs, h))
        banks.put(pb)
    w_put(wgh[0])
    _o0 = slots.get_run(4)
    osl = list(range(_o0, _o0 + 4))
    sbf = [slots.get(), slots.get()]
    kdm = [slots.get(), slots.get()]
    cur = 0
    cpy(act, AB(sbf[cur])[:, 0:512], hstl[:].rearrange("p h v -> p (h v)"), [b_hstl], [b_ar[sbf[cur]]])
    for tt in range(4):
        po_ = banks.get()
        asl = attm[tt // 2]
        am = AB(asl)[:, (tt % 2) * 512:(tt % 2) * 512 + 512].rearrange("p (h t) -> p h t", h=4)
        for c in range(4):
            cc = 4 * tt + c
            km = kdm[cc % 2]
            kmv = AB(km)[:, 0:512]
            tsop(pool, kmv, kdec[:, tt, :], constA[:, C_CM + c:C_CM + c + 1], None, ALU.mult, None, Bh(s_kd, tt) + [b_constA], [b_ar[km]])
            pu = banks.get()
            for h in range(4):
                mm(psum[pu][:, h * 128:(h + 1) * 128], kmv[:, h * 128:(h + 1) * 128], itok[:, tt, h * 128:(h + 1) * 128], True, True,
                   [b_ar[km]] + Bh(s_it, tt), [b_ps[pu]], inc=(h == 3))
            for h in range(4):
                oc_ = psum[po_][:, h * 128 + 32 * c:h * 128 + 32 * c + 32]
                mm(oc_, AB(sbf[cur])[:, h * 128:(h + 1) * 128], qb[:, h, tt * 128 + 32 * c:tt * 128 + 32 * c + 32], True, False,
                   [b_ar[sbf[cur]]] + B2(s_qb), [b_ps[po_]], inc=False)
                mm(oc_, itok[:, tt, h * 128:(h + 1) * 128], am[:, h, 32 * c:32 * c + 32], False, True,
                   Bh(s_it, tt) + [b_ar[asl]], [b_ps[po_]], inc=(h == 3))
            for h in range(4):
                sttop(dve, hstl[:, h, :], hstl[:, h, :], dec_t[:, h, cc:cc + 1], psum[pu][:, h * 128:(h + 1) * 128], ALU.mult, ALU.add,
                      [b_hstl, b_dec, b_ps[pu]], [b_hstl])
            banks.put(pu)
            cur ^= 1
            cpy(act, AB(sbf[cur])[:, 0:512], hstl[:].rearrange("p h v -> p (h v)"), [b_hstl], [b_ar[sbf[cur]]])
        for h in range(4):
            cpy(act if h % 2 else dve, A(osl[h])[:, tt * 128:(tt + 1) * 128], psum[po_][:, h * 128:(h + 1) * 128], [b_ps[po_]], [b_ar[osl[h]]])
        banks.put(po_)
    slots.put(*sbf); slots.put(*kdm); slots.put(*attm)
    tap("oh", key, arena[:, osl[0]:osl[0] + 4, :], [b_ar[i] for i in osl])
    for h in range(4):
        sq = slots.get(); pm = banks.get()
        fw.op(act, (lambda sq=sq, h=h: nc.scalar.activation(out=A(sq), in_=A(osl[h]), func=AF.Square)), [b_ar[osl[h]]], [b_ar[sq]])
        mm(psum[pm][:, :], onesf, A(sq), True, True, [b_ar[sq], b_constA], [b_ps[pm]])
        actf(A(sq), psum[pm][:, :], AF.Sqrt, [b_ps[pm], b_eps], [b_ar[sq]], bias=epst[:, 0:1], scale=1.0 / 128)
        fw.op(dve, (lambda sq=sq: nc.vector.reciprocal(A(sq), A(sq))), [b_ar[sq]], [b_ar[sq]])
        sttop(dve, A(sq), A(osl[h]), sm[:, S_HGG:S_HGG + 1], A(sq), ALU.mult, ALU.mult, [b_ar[osl[h]], b_ar[sq], b_small], [b_ar[sq]])
        ttop(dve, yc[:, h, :], A(sq), gsil[:, h, :], ALU.mult, [b_ar[sq]] + Bh(s_gs, h), ybufs(2, h))
        slots.put(sq); banks.put(pm)
    slots.put(*osl)
    for s0 in (s_it, s_gs, s_qb, s_kd):
        slots.put(s0, s0 + 1)
    tap("yc", key, yc, ybufs(2, 0) + ybufs(2, 2))
    if stop <= 3:
        return

    _m0 = slots.get_run(8)
    mixs = list(range(_m0, _m0 + 8))
    ys = [ya, yb, yc]
    for n in range(3):
        wb = w_get(4); wg0 = w_get(8); wg1 = w_get(8)
        for dc in range(KC):
            wgx = wg0 if dc < 4 else wg1
            pp = banks.get(); pg = banks.get()
            for kc in range(4):
                mm(psum[pp][:, :], wb[1][:, kc, dc * 128:(dc + 1) * 128], ys[n][:, kc, :], kc == 0, kc == 3,
                   [g["b_wsl"][wb[0]]] + ybufs(n, kc), [b_ps[pp]], inc=(kc == 3))
            fm_proj(wgx, dc % 4, pg)
            sg = slots.get()
            actf(A(sg), psum[pg][:, :], AF.Sigmoid, [b_ps[pg], b_small], [b_ar[sg]],
                 bias=sm[:, S_BGATE + n * 8 + dc:S_BGATE + n * 8 + dc + 1], scale=1.0)
            if n == 0:
                ttop(dve, A(mixs[dc]), psum[pp][:, :], A(sg), ALU.mult, [b_ps[pp], b_ar[sg]], [b_ar[mixs[dc]]])
            else:
                ttop(dve, A(sg), psum[pp][:, :], A(sg), ALU.mult, [b_ps[pp], b_ar[sg]], [b_ar[sg]])
                ttop(dve, A(mixs[dc]), A(mixs[dc]), A(sg), ALU.add, [b_ar[mixs[dc]], b_ar[sg]], [b_ar[mixs[dc]]])
            slots.put(sg); banks.put(pp, pg)
        w_put(wb[0]); w_put(wg0[0]); w_put(wg1[0])
    slots.put(*ysl)
    tap("mix", key, arena[:, mixs[0]:mixs[0] + 8, :], [b_ar[i] for i in mixs])
    _mb = slots.get_run(4)
    mb = [_mb, _mb + 2]
    mixb = arena[:, mb[0]:mb[0] + 4, :].bitcast(BF16).rearrange("p a (h t) -> p (a h) t", h=2)
    mixbB = lambda dc: [b_ar[mb[0] + dc // 2]]
    for dc in range(KC):
        cpy(act if dc % 2 else dve, mixb[:, dc, :], A(mixs[dc]), [b_ar[mixs[dc]]], mixbB(dc))
    slots.put(*mixs)
    for hf in range(2):
        wo = w_get(8)
        for oc in range(4):
            dc = hf * 4 + oc
            pb = banks.get()
            for kc in range(KC):
                mm(psum[pb][:, :], wo[1][:, kc, oc * 128:(oc + 1) * 128], mixb[:, kc, :], kc == 0, kc == KC - 1,
                   [g["b_wsl"][wo[0]]] + mixbB(kc), [b_ps[pb]], inc=(kc == KC - 1))
            sttop(dve, xres[:, dc, :], xres[:, dc, :], float(DN_ALPHA), psum[pb][:, :], ALU.mult, ALU.add, [b_xres[dc], b_ps[pb]], [b_xres[dc]])
            banks.put(pb)
        w_put(wo[0])
    for m0 in mb:
        slots.put(m0, m0 + 1)

    def layer_norm_res(goff, boff):
        m_, r_ = bc_stats([xres[:, dc, :] for dc in range(KC)], [[b_xres[dc]] for dc in range(KC)], float(D))
        for dc in range(KC):
            ttop(dve, xres[:, dc, :], xres[:, dc, :], A(m_), ALU.subtract, [b_xres[dc], b_ar[m_]], [b_xres[dc]])
            ttop(dve, xres[:, dc, :], xres[:, dc, :], A(r_), ALU.mult, [b_xres[dc], b_ar[r_]], [b_xres[dc]])
            actf(xres[:, dc, :], xres[:, dc, :], AF.Identity, [b_xres[dc], b_small], [b_xres[dc]],
                 bias=sm[:, boff + dc:boff + dc + 1], scale=sm[:, goff + dc:goff + dc + 1])
            cpy(dve, xbf[:, dc, :], xres[:, dc, :], [b_xres[dc]], [b_xbf[dc]])
        slots.put(m_, r_)
    layer_norm_res(S_LN1G, S_LN1B)
    tap("x1", key, xres[:], b_xres)
    if stop <= 4:
        return

    sl_a = slots.get(); sl_b = slots.get(); sl_c = slots.get()
    rt = A(sl_a)[:, 0:256].rearrange("p (a b) -> p a b", a=4); b_rt = b_ar[sl_a]
    rt2 = A(sl_b)[:, 0:256].rearrange("p (a b) -> p a b", a=4); b_rt2 = b_ar[sl_b]
    rt3 = A(sl_c)[:, 0:256].rearrange("p (a b) -> p a b", a=4); b_rt3 = b_ar[sl_c]
    pr = banks.get()
    wr = sm[:, S_WR:S_WR + 288].rearrange("p (k c) -> p k c", k=8)
    for tt in range(4):
        for kc in range(KC):
            mm(psum[pr][:, tt * 64:tt * 64 + 36], xres[:, kc, tt * 128:(tt + 1) * 128], wr[:, kc, :], kc == 0, kc == KC - 1,
               [b_xres[kc], b_small], [b_ps[pr]], inc=(kc == KC - 1))
    L = rt[:, :, 0:36]
    prv = psum[pr][:, 0:256].rearrange("p (t c) -> p t c", c=64)[:, :, 0:36]
    rbv = sm[:, S_RB:S_RB + 36].unsqueeze(1).to_broadcast([128, 4, 36])
    ttop(dve, L, prv, rbv, ALU.add, [b_ps[pr], b_small], [b_rt])
    banks.put(pr)
    gl = rt[:, :, 0:4]
    El = rt[:, :, 4:36]
    gmax = rt2[:, :, 0:1]
    fw.op(dve, lambda: nc.vector.tensor_reduce(out=rt2[:, :, 0], in_=gl, axis=AX.X, op=ALU.max), [b_rt], [b_rt2])
    ge = rt2[:, :, 4:8]
    ttop(dve, ge, gl, gmax.to_broadcast([128, 4, 4]), ALU.subtract, [b_rt, b_rt2], [b_rt2])
    oneh = rt2[:, :, 8:12]
    tsop(dve, oneh, ge, 0.0, None, ALU.is_ge, None, [b_rt2], [b_rt2])
    actf(ge, ge, AF.Exp, [b_rt2], [b_rt2])
    fw.op(dve, lambda: nc.vector.tensor_reduce(out=rt2[:, :, 1], in_=ge, axis=AX.X, op=ALU.add), [b_rt2], [b_rt2])
    fw.op(dve, lambda: nc.vector.reciprocal(rt2[:, :, 2], rt2[:, :, 1]), [b_rt2], [b_rt2])
    pen = rt2[:, :, 12:16]
    tsop(dve, pen, oneh, 1e30, -1e30, ALU.mult, ALU.add, [b_rt2], [b_rt2])
    Em = rt3[:, :, 0:32]
    ttop(dve, Em.rearrange("p t (g e) -> p t g e", g=4), El.rearrange("p t (g e) -> p t g e", g=4),
         pen.unsqueeze(3).to_broadcast([128, 4, 4, 8]), ALU.add, [b_rt, b_rt2], [b_rt3])
    top = rt3[:, :, 32:40]
    for tt in range(4):
        fw.op(dve, (lambda tt=tt: nc.vector.max(out=rt3[:, tt, 32:40], in_=rt3[:, tt, 0:32])), [b_rt3], [b_rt3])
    wts = rt2[:, :, 16:18]
    ttop(dve, rt2[:, :, 18:19], rt3[:, :, 32:33], rt3[:, :, 33:34], ALU.subtract, [b_rt3], [b_rt2])
    actf(rt2[:, :, 18:19], rt2[:, :, 18:19], AF.Sigmoid, [b_rt2], [b_rt2])
    ttop(dve, rt2[:, :, 16:17], rt2[:, :, 18:19], rt2[:, :, 2:3], ALU.mult, [b_rt2], [b_rt2])
    ttop(dve, rt2[:, :, 17:18], rt2[:, :, 2:3], rt2[:, :, 16:17], ALU.subtract, [b_rt2], [b_rt2])
    eq = rt[:, :, 0:32]
    ttop(dve, eq, Em, rt3[:, :, 32:33].to_broadcast([128, 4, 32]), ALU.is_equal, [b_rt3], [b_rt])
    ttop(dve, comb[:], eq, rt2[:, :, 16:17].to_broadcast([128, 4, 32]), ALU.mult, [b_rt, b_rt2], [b_comb])
    ttop(dve, eq, Em, rt3[:, :, 33:34].to_broadcast([128, 4, 32]), ALU.is_equal, [b_rt3], [b_rt])
    ttop(dve, eq, eq, rt2[:, :, 17:18].to_broadcast([128, 4, 32]), ALU.mult, [b_rt, b_rt2], [b_rt])
    ttop(dve, comb[:], comb[:], eq, ALU.add, [b_comb, b_rt], [b_comb])
    tap("comb", key, comb[:], [b_comb])
    slots.put(sl_a, sl_b, sl_c)

    _a0 = slots.get_run(8)
    accs = list(range(_a0, _a0 + 8))
    hts = [two(), two()]
    for e in range(n_exp):
        w1 = w_get(8); w3 = w_get(8)
        h0 = hts[e % 2]
        hT = bf4(h0)
        for oc in range(4):
            p1 = banks.get(); p3 = banks.get()
            fm_proj(w1, oc, p1)
            fm_proj(w3, oc, p3)
            sl = slots.get()
            actf(A(sl), psum[p1][:, :], AF.Silu, [b_ps[p1]], [b_ar[sl]])
            ttop(dve, hT[:, oc, :], A(sl), psum[p3][:, :], ALU.mult, [b_ar[sl], b_ps[p3]], Bh(h0, oc))
            slots.put(sl); banks.put(p1, p3)
        w_put(w1[0]); w_put(w3[0])
        w2 = w_get(4)
        for tt in range(4):
            for hf in range(2):
                pb = banks.get()
                for oc in range(4):
                    mm(psum[pb][:, :], hT[:, oc, tt * 128:(tt + 1) * 128], w2[1][:, oc, hf * 512:(hf + 1) * 512], oc == 0, oc == 3,
                       Bh(h0, oc) + [g["b_wsl"][w2[0]]], [b_ps[pb]], inc=(oc == 3))
                a_ = accs[tt * 2 + hf]
                if e == 0:
                    tsop(dve, A(a_), psum[pb][:, :], comb[:, tt, e:e + 1], None, ALU.mult, None, [b_ps[pb], b_comb], [b_ar[a_]])
                else:
                    sttop(dve, A(a_), psum[pb][:, :], comb[:, tt, e:e + 1], A(a_), ALU.mult, ALU.add, [b_ps[pb], b_comb, b_ar[a_]], [b_ar[a_]])
                banks.put(pb)
        w_put(w2[0])
    for h0 in hts:
        slots.put(h0, h0 + 1)
    tap("acc", key, arena[:, accs[0]:accs[0] + 8, :].rearrange("p (t h) c -> p t (h c)", h=2), [b_ar[i] for i in accs])
    for dc in range(KC):
        pb = banks.get()
        hf = dc // 4
        cs = (dc % 4) * 128
        for tt in range(4):
            a_ = accs[tt * 2 + hf]
            mm(psum[pb][:, tt * 128:(tt + 1) * 128], A(a_)[:, cs:cs + 128], ident, True, True, [b_ar[a_], b_constA], [b_ps[pb]], inc=(tt == 3))
        sttop(dve, xres[:, dc, :], xres[:, dc, :], float(DN_ALPHA), psum[pb][:, :], ALU.mult, ALU.add, [b_xres[dc], b_ps[pb]], [b_xres[dc]])
        banks.put(pb)
    slots.put(*accs)
    layer_norm_res(S_LN2G, S_LN2B)
    tap("x2", key, xres[:], b_xres)


def _const_packs():
    ca = np.zeros((128, NCA), np.float32)
    ca[:, C_ID:C_ID + 128] = np.eye(128)
    idx = np.arange(128)
    ch = idx // 32
    same = ch[:, None] == ch[None, :]
    s_le_t = idx[:, None] <= idx[None, :]
    mid = ch * 32 + 15
    s_le_mid = idx[:, None] <= mid[None, :]
    ca[:, C_M1:C_M1 + 128] = same * (s_le_t.astype(np.float32) - s_le_mid.astype(np.float32))
    ca[:, C_M2:C_M2 + 128] = same * (idx[:, None] > idx[None, :])
    ca[:, C_M3:C_M3 + 128] = same * s_le_t
    for c in range(4):
        ca[:, C_CM + c] = (ch == c)
    d = idx % 64
    jj = d % 8
    inv = ROPE_THETA ** (-(2.0 * jj) / 16.0)
    ca[:, C_INVF] = np.where(d < 16, inv, 0.0).astype(np.float32)
    ca[:, C_SGN] = np.where(d < 8, -1.0, np.where(d < 16, 1.0, 0.0))
    ca[:, C_ONES:C_ONES + 128] = 1.0
    cb = np.zeros((128, NCB), np.float32)
    cb[:, CB_ID:CB_ID + 128] = np.eye(128)
    perm = np.zeros((128, 128), np.float32)
    for m in range(128):
        dm = m % 64
        if dm < 8:
            perm[m + 8, m] = 1.0
        elif dm < 16:
            perm[m - 8, m] = 1.0
    cb[:, CB_PERM:CB_PERM + 128] = perm
    cb[:, CB_M3:CB_M3 + 128] = same * s_le_t
    cb[:, CB_ONES:CB_ONES + 128] = 1.0
    q = np.arange(512)
    for jm in range(4):
        cb[:, CB_MASK + jm * 512:CB_MASK + (jm + 1) * 512] = (q[None, :] >= (jm * 128 + idx[:, None]))
    return ca, cb


def _small_pack(inp):
    sp_ = np.zeros((2, 128, NS), np.float32)
    f = lambda a: np.asarray(a, np.float32)
    for l in range(2):
        sp_[l, :, S_CONVW:S_CONVW + 124] = f(inp["conv_w"][l]).reshape(CONV_K, 4, 128).transpose(2, 1, 0).reshape(128, 124)
        sp_[l, :, S_CONVB:S_CONVB + 4] = f(inp["conv_b"][l]).reshape(4, 128).T
        sp_[l, :, S_CLNG:S_CLNG + 4] = f(inp["conv_ln_g"][l]).reshape(4, 128).T
        sp_[l, :, S_CLNB:S_CLNB + 4] = f(inp["conv_ln_b"][l]).reshape(4, 128).T
        sp_[l, :, S_DAG] = f(inp["da_norm_g"][l])
        sp_[l, :, S_HGG] = f(inp["hg_norm_g"][l])
        sp_[l, :, S_BGATE:S_BGATE + 24] = f(inp["b_gate"][l]).reshape(3, 8, 128).transpose(2, 0, 1).reshape(128, 24)
        for nm, off in (("ln1_g", S_LN1G), ("ln1_b", S_LN1B), ("ln2_g", S_LN2G), ("ln2_b", S_LN2B)):
            sp_[l, :, off:off + 8] = f(inp[nm][l]).reshape(8, 128).T
        wr = np.concatenate([f(inp["router_group"][l]), f(inp["router_expert"][l]).transpose(1, 0, 2).reshape(D, 32)], axis=1)
        sp_[l, :, S_WR:S_WR + 288] = wr.reshape(8, 128, 36).transpose(1, 0, 2).reshape(128, 288)
        rb = np.concatenate([f(inp["router_group_b"][l]), f(inp["router_expert_b"][l]).reshape(32)])
        sp_[l, :, S_RB:S_RB + 36] = rb[None, :]
        sp_[l, :, S_LBP:S_LBP + 8] = f(inp["hg_lb"]).reshape(2, 4, 128).transpose(2, 0, 1).reshape(128, 8)
    return sp_


_PROG_CACHE = {}


def _run(inp, layers, x_in):
    keyp = tuple(layers)
    if keyp not in _PROG_CACHE:
        _PROG_CACHE[keyp] = build_program(layers=layers)
    nc = _PROG_CACHE[keyp]
    ca, cb = _const_packs()
    smallp = _small_pack(inp)
    f = lambda a: np.ascontiguousarray(np.asarray(a, np.float32))
    shared = {
        "w_in": f(inp["w_in"]), "w_branch": f(inp["w_branch"]), "w_out": f(inp["w_out"]),
        "exp_w1": f(inp["exp_w1"]), "exp_w3": f(inp["exp_w3"]), "exp_w2": f(inp["exp_w2"]),
        "small": smallp, "lbrow": f(inp["hg_lb"]), "lamrow": f(inp["da_lambda"]).reshape(2, 256), "constA": ca, "constB": cb,
    }
    pos = np.ascontiguousarray(np.asarray(inp["positions"], np.int32))
    in_maps = []
    for c in range(NCORES):
        m = dict(shared)
        m["x"] = np.ascontiguousarray(x_in[c * SPC:(c + 1) * SPC])
        m["positions"] = np.ascontiguousarray(pos[c * SPC:(c + 1) * SPC])
        in_maps.append(m)
    res = run_bass_kernel_spmd(nc, in_maps, core_ids=list(range(NCORES)))
    return np.concatenate([np.asarray(r["out"], np.float32) for r in res.results], axis=0)


def kernel(**inputs):
    x = np.asarray(inputs["x"], np.float32)
    return _run(inputs, (0, 1), x)
```

```python
import math
from contextlib import ExitStack

import numpy as np
import concourse.bass as bass
import concourse.mybir as mybir
from concourse.bass_utils import run_bass_kernel_spmd

F32 = mybir.dt.float32
BF16 = mybir.dt.bfloat16
I32 = mybir.dt.int32
AF = mybir.ActivationFunctionType
ALU = mybir.AluOpType
AX = mybir.AxisListType

D = 1024
SEQ = 2048
NB = 32
NCORES = 8
SPC = NB // NCORES
T = 512
NBLK = SEQ // T
KC = D // 128
IN_COLS = 7680
NEXP = 32
DEXP = 512
CONV_K = 31
HALO = CONV_K - 1
DN_ALPHA = (2 * 2) ** 0.25
LN_EPS = 1e-5
ROPE_THETA = 500000.0
CLAMP = 40.0

_off = 0
def _fld(n):
    global _off
    o = _off
    _off += n
    return o
S_CONVW = _fld(4 * CONV_K)
S_CONVB = _fld(4)
S_CLNG = _fld(4)
S_CLNB = _fld(4)
S_DAG = _fld(1)
S_HGG = _fld(1)
S_BGATE = _fld(24)
S_LN1G = _fld(8)
S_LN1B = _fld(8)
S_LN2G = _fld(8)
S_LN2B = _fld(8)
S_WR = _fld(8 * 36)
S_RB = _fld(36)
S_LBP = _fld(8)
NS = _off
C_ID = 0
C_M1 = 128
C_M2 = 256
C_M3 = 384
C_CM = 512
C_INVF = 516
C_SGN = 517
C_ONES = 518
NCA = 646
CB_ID = 0
CB_PERM = 128
CB_M3 = 256
CB_ONES = 384
CB_MASK = 512
NCB = 512 + 2048


class Buf:
    __slots__ = ("name", "w", "r", "excl")

    def __init__(self, name, excl=False):
        self.name = name
        self.w = None
        self.r = {}
        self.excl = excl


class Eng:
    def __init__(self, name, handle, sem, is_pe=False):
        self.name = name
        self.h = handle
        self.sem = sem
        self.key = "s_" + name
        self.count = 0
        self.prog = []
        self.waited = {}
        self.is_pe = is_pe
        self.dma_i = 0
        self.pending_noinc = False


class FW:
    NDMA = 8

    def __init__(self, nc, stack):
        self.nc = nc
        self.sems = {}

        def mk(name):
            s = stack.enter_context(nc.semaphore(name))
            self.sems[name] = s
            return s
        self.pe = Eng("pe", nc.tensor, mk("s_pe"), is_pe=True)
        self.act = Eng("act", nc.scalar, mk("s_act"))
        self.dve = Eng("dve", nc.vector, mk("s_dve"))
        self.pool = Eng("pool", nc.gpsimd, mk("s_pool"))
        self.sp = Eng("sp", nc.sync, mk("s_sp"))
        self.engs = [self.pe, self.act, self.dve, self.pool, self.sp]
        self.dsem = {}
        for e in (self.sp, self.pool):
            self.dsem[e.name] = [mk(f"d_{e.name}{i}") for i in range(self.NDMA)]
        self.n_ops = 0
        self.tag = ''
        self.pe_tags = []
        self.pe_names = []

    def _deps(self, eng, reads, writes):
        need = {}
        for b in reads:
            if b.w is not None:
                k, v = b.w
                if need.get(k, 0) < v:
                    need[k] = v
            if b.excl:
                for k, v in b.r.items():
                    if k != eng.key and need.get(k, 0) < v:
                        need[k] = v
        for b in writes:
            if b.w is not None:
                k, v = b.w
                if need.get(k, 0) < v:
                    need[k] = v
            for k, v in b.r.items():
                if need.get(k, 0) < v:
                    need[k] = v
        waits = []
        for k, v in need.items():
            if eng.is_pe and k == eng.key:
                continue
            if eng.waited.get(k, 0) >= v:
                continue
            eng.waited[k] = v
            waits.append((k, v))
        return waits

    def op(self, eng, fn, reads=(), writes=(), inc=True):
        waits = self._deps(eng, reads, writes)
        if eng.is_pe:
            self.pe_tags.append(self.tag)
        if inc:
            eng.count += 1
            seq = eng.count
            eng.pending_noinc = False
        else:
            seq = eng.count + 1
            eng.pending_noinc = True
        key = eng.key
        eng.prog.append((waits, fn, 1 if inc else 0, None))
        for b in reads:
            if b.r.get(key, 0) < seq:
                b.r[key] = seq
        for b in writes:
            b.w = (key, seq)
            b.r = {}
        self.n_ops += 1

    def dma(self, eng, fn, reads=(), writes=()):
        ring = self.dsem[eng.name]
        i = eng.dma_i
        eng.dma_i += 1
        r = i % self.NDMA
        key = ("d", eng.name, r)
        waits = self._deps(eng, reads, writes)
        prev = i // self.NDMA
        if prev > 0 and eng.waited.get(key, 0) < prev * 16:
            eng.waited[key] = prev * 16
            waits.append((key, prev * 16))
        val = (prev + 1) * 16
        eng.prog.append((waits, fn, 16, ring[r]))
        for b in reads:
            if b.r.get(key, 0) < val:
                b.r[key] = val
        for b in writes:
            b.w = (key, val)
            b.r = {}
        self.n_ops += 1

    def sem_of(self, key):
        if isinstance(key, tuple):
            return self.dsem[key[1]][key[2]]
        return self.sems[key]

    def finish(self, final_bufs):
        nc = self.nc
        waits = self._deps(self.sp, final_bufs, [])
        self.sp.prog.append((waits, None, 0, None))
        for e in self.engs:
            if e.pending_noinc:
                raise RuntimeError(f"engine {e.name} ends with a non-inc instruction")
        fw = self

        def runner(e):
            def run(h):
                for waits, fn, inc, dsem in e.prog:
                    for k, v in waits:
                        h.wait_ge(fw.sem_of(k), v)
                    if fn is None:
                        continue
                    ins = fn()
                    if e.is_pe:
                        fw.pe_names.append(getattr(getattr(ins, 'ins', ins), 'name', None))
                    if inc == 1:
                        ins.then_inc(e.sem, 1)
                    elif inc == 16:
                        ins.then_inc(dsem, 16)
            return run
        with nc.Block() as block:
            block.tensor(runner(self.pe))
            block.scalar(runner(self.act))
            block.vector(runner(self.dve))
            block.gpsimd(runner(self.pool))
            block.sync(runner(self.sp))


class Pool:
    def __init__(self, items):
        self.free = list(items)

    def get(self):
        if not self.free:
            raise RuntimeError("pool exhausted")
        return self.free.pop(0)

    def put(self, *xs):
        for x in xs:
            assert x not in self.free
            self.free.append(x)

    def get_run(self, n):
        fs = sorted(self.free)
        for a in fs:
            if all((a + i) in self.free for i in range(n)):
                for i in range(n):
                    self.free.remove(a + i)
                return a
        raise RuntimeError(f"no run of {n} free slots: {fs}")

    def put_run(self, a, n):
        self.put(*range(a, a + n))


def build_program(layers=(0, 1), n_seq=SPC, n_blk=NBLK, n_exp=NEXP, taps=None, stop=99):
    taps = taps or {}
    import os
    SKIP = os.environ.get('SKIP', '')
    nc = bass.Bass("TRN2", target_bir_lowering=False)
    dram = {}

    def din(name, shape, dt=F32):
        dram[name] = nc.dram_tensor(name, list(shape), dt, kind="ExternalInput").ap()
        return dram[name]

    x_d = din("x", [n_seq, SEQ, D])
    pos_d = din("positions", [n_seq, SEQ], I32)
    w_in_d = din("w_in", [2, D, IN_COLS])
    w_br_d = din("w_branch", [2, 3, 512, D])
    w_out_d = din("w_out", [2, D, D])
    w1_d = din("exp_w1", [2, NEXP, D, DEXP])
    w3_d = din("exp_w3", [2, NEXP, D, DEXP])
    w2_d = din("exp_w2", [2, NEXP, DEXP, D])
    small_d = din("small", [2, 128, NS])
    lbrow_d = din("lbrow", [2, 512])
    lam_d = din("lamrow", [2, 256])
    ca_d = din("constA", [128, NCA])
    cb_d = din("constB", [128, NCB])
    out_d = nc.dram_tensor("out", [n_seq, SEQ, D], F32, kind="ExternalOutput").ap()
    tap_d = {}
    for name, (shape, dt) in TAP_SHAPES.items():
        if name in taps:
            tap_d[name] = nc.dram_tensor("tap_" + name, list(shape), dt, kind="ExternalOutput").ap()

    with ExitStack() as st:
        fw = FW(nc, st)
        pe, act, dve, pool, sp = fw.pe, fw.act, fw.dve, fw.pool, fw.sp

        def sb(name, shape, dt=F32):
            return st.enter_context(nc.sbuf_tensor("sb_" + name, list(shape), dt))

        constA = sb("constA", [128, NCA]); b_constA = Buf("constA")
        cbf = sb("cbf", [128, NCB], BF16); b_cbf = Buf("cbf")
        small = sb("small", [128, 2, NS]); b_small = Buf("small")
        omlb_p = sb("omlb_p", [128, 2, 4]); omlb_row = sb("omlb_row", [128, 512]); b_omlb = Buf("omlb")
        lam_t = sb("lam_t", [128, 2, 4]); b_lam = Buf("lam")
        dagp = sb("dagp", [128, 2]);
        epst = sb("epst", [128, 1]); b_eps = Buf("eps")
        xres = sb("xres", [128, KC, T]); b_xres = [Buf(f"xres{i}") for i in range(KC)]
        xbf = sb("xbf", [128, KC, T], BF16); b_xbf = [Buf(f"xbf{i}") for i in range(KC)]
        KT = [sb(f"KT{l}", [128, 4, SEQ], BF16) for l in range(2)]
        b_KT = [[[Buf(f"KT{l}_{h}_{j}") for j in range(NBLK)] for h in range(4)] for l in range(2)]
        Vh = [sb(f"V{l}", [128, SEQ // 128, 512], BF16) for l in range(2)]
        b_V = [[Buf(f"V{l}_{t}") for t in range(SEQ // 128)] for l in range(2)]
        hst = [sb(f"hst{l}", [128, 4, 128]) for l in range(2)]; b_hst = [Buf(f"hst{l}") for l in range(2)]
        ubuf = [sb(f"ubuf{l}", [128, 4, HALO + T], BF16) for l in range(2)]
        b_ubuf = [[Buf(f"ubuf{l}_{c}") for c in range(4)] for l in range(2)]
        ropeC = sb("ropeC", [128, T]); ropeS = sb("ropeS", [128, T]); b_rope = Buf("rope")
        dec_t = sb("dec_t", [128, 4, 16]); b_dec = Buf("dec")
        comb = sb("comb", [128, 4, 32]); b_comb = Buf("comb")
        NW = 4
        wsl = [sb(f"wsl{i}", [128, 4096], BF16) for i in range(NW)]
        b_wsl = [Buf(f"wsl{i}") for i in range(NW)]
        print('sbuf remaining', nc.sbuf_bytes_remaining, flush=True)
        NSLOT = (nc.sbuf_bytes_remaining - 2048) // 2048
        print('NSLOT', NSLOT, flush=True)
        NSLOT = min(NSLOT, 40)
        arena = sb("arena", [128, NSLOT, 512])
        b_ar = [Buf(f"ar{i}") for i in range(NSLOT)]
        psum = [st.enter_context(nc.psum_tensor(f"ps{i}", [128, 512], F32)) for i in range(8)]
        b_ps = [Buf(f"ps{i}", excl=True) for i in range(8)]
        banks = Pool(range(8))
        slots = Pool(range(NSLOT))

        ident = constA[:, C_ID:C_ID + 128]
        M1 = constA[:, C_M1:C_M1 + 128]
        M2 = constA[:, C_M2:C_M2 + 128]
        M3 = constA[:, C_M3:C_M3 + 128]
        onesf = constA[:, C_ONES:C_ONES + 128]
        identb = cbf[:, CB_ID:CB_ID + 128]
        permb = cbf[:, CB_PERM:CB_PERM + 128]
        M3b = cbf[:, CB_M3:CB_M3 + 128]
        onesb = cbf[:, CB_ONES:CB_ONES + 128]

        def A(i):
            return arena[:, i, :]

        def AB(i):
            return arena[:, i, :].bitcast(BF16)

        def mm(out, lhsT, rhs, start, stop, R, W, inc=True):
            fw.op(pe, lambda: nc.tensor.matmul(out, lhsT, rhs, start=start, stop=stop), R, W, inc=inc)

        def actf(out, in_, func, R, W, bias=None, scale=None):
            kw = {}
            if bias is not None:
                kw["bias"] = bias
            if scale is not None:
                kw["scale"] = scale
            fw.op(act, lambda: nc.scalar.activation(out=out, in_=in_, func=func, **kw), R, W)

        def ttop(eng, out, a, b, op, R, W):
            fw.op(eng, lambda: eng.h.tensor_tensor(out, a, b, op), R, W)

        def tsop(eng, out, a, s1, s2, op0, op1, R, W):
            if s2 is None:
                fw.op(eng, lambda: eng.h.tensor_scalar(out, a, s1, None, op0), R, W)
            else:
                fw.op(eng, lambda: eng.h.tensor_scalar(out, a, s1, s2, op0, op1), R, W)

        def sttop(eng, out, a, s, b, op0, op1, R, W):
            fw.op(eng, lambda: eng.h.scalar_tensor_tensor(out, a, s, b, op0, op1), R, W)

        def cpy(eng, out, in_, R, W):
            if eng is act:
                fw.op(act, lambda: nc.scalar.activation(out=out, in_=in_, func=AF.Identity), R, W)
            else:
                fw.op(eng, lambda: eng.h.tensor_copy(out, in_), R, W)

        def tap(name, key, src_ap, R):
            if name in taps and taps[name] == key:
                b = Buf("tap_" + name)
                dst = tap_d[name]
                fw.dma(sp, (lambda dst=dst, src_ap=src_ap: nc.sync.dma_start(out=dst, in_=src_ap)), R, [b])
                final_bufs.append(b)

        final_bufs = []

        wlist = []
        for s in range(n_seq):
            for j in range(n_blk):
                for l in layers:
                    def inblk(b, l=l):
                        return (w_in_d[l][:, 512 * b:512 * b + 512].rearrange("(kc p) c -> p kc c", p=128), 8)
                    for b in range(9):
                        wlist.append(inblk(b))
                    for n in range(3):
                        wlist.append((w_br_d[l][n].rearrange("(kc p) c -> p kc c", p=128), 4))
                        wlist.append(inblk(9 + 2 * n))
                        wlist.append(inblk(10 + 2 * n))
                    for hf in range(2):
                        wlist.append((w_out_d[l][:, 512 * hf:512 * hf + 512].rearrange("(kc p) c -> p kc c", p=128), 8))
                    for e in range(n_exp):
                        wlist.append((w1_d[l][e].rearrange("(kc p) c -> p kc c", p=128), 8))
                        wlist.append((w3_d[l][e].rearrange("(kc p) c -> p kc c", p=128), 8))
                        wlist.append((w2_d[l][e].rearrange("(kc p) c -> p kc c", p=128), 4))
        wstate = {"issued": 0, "consumed": 0, "ready": []}
        wfree = Pool(range(NW))

        def w_issue():
            while wfree.free and wstate["issued"] < len(wlist):
                i = wfree.get()
                src, nk = wlist[wstate["issued"]]
                wstate["issued"] += 1
                dst = wsl[i][:].rearrange("p (k c) -> p k c", k=nk)
                fw.dma(pool, (lambda dst=dst, src=src: nc.gpsimd.dma_start(out=dst, in_=src)), [], [b_wsl[i]])
                wstate["ready"].append((i, nk))

        def w_get(nk_expect):
            w_issue()
            i, nk = wstate["ready"].pop(0)
            assert nk == nk_expect, (nk, nk_expect, wstate["consumed"])
            wstate["consumed"] += 1
            return i, wsl[i][:].rearrange("p (k c) -> p k c", k=nk)

        def w_put(i):
            wfree.put(i)
            w_issue()

        fw.dma(sp, lambda: nc.sync.dma_start(out=constA[:], in_=ca_d), [], [b_constA])
        fw.dma(sp, lambda: nc.sync.dma_start(out=small[:], in_=small_d.rearrange("l p n -> p l n")), [], [b_small])
        fw.dma(sp, lambda: nc.sync.dma_start(out=omlb_row[:], in_=lbrow_d[1:2, :].partition_broadcast(128)), [], [b_omlb])
        s0 = slots.get_run(5)
        stg = arena[:, s0:s0 + 5, :].rearrange("p a b -> p (a b)")
        stgb = [b_ar[s0 + i] for i in range(5)]
        fw.dma(sp, lambda: nc.sync.dma_start(out=stg[:, 0:NCB], in_=cb_d), [], stgb)
        cpy(dve, cbf[:], stg[:, 0:NCB], stgb, [b_cbf])
        slots.put_run(s0, 5)
        fw.op(dve, lambda: nc.vector.memset(epst[:], LN_EPS), [], [b_eps])
        s0 = slots.get()
        fw.dma(sp, lambda: nc.sync.dma_start(out=A(s0), in_=lbrow_d[0:1, :].partition_broadcast(128)), [], [b_ar[s0]])
        ttop(dve, omlb_row[:], A(s0), omlb_row[:], ALU.subtract, [b_omlb, b_ar[s0]], [b_omlb])
        actf(omlb_row[:], omlb_row[:], AF.Sigmoid, [b_omlb], [b_omlb])
        slots.put(s0)
        lbp = small[:, 0, S_LBP:S_LBP + 8].rearrange("p (l h) -> p l h", l=2)
        ttop(dve, omlb_p[:, 1, :], lbp[:, 0, :], lbp[:, 1, :], ALU.subtract, [b_small], [b_omlb])
        actf(omlb_p[:, 1, :], omlb_p[:, 1, :], AF.Sigmoid, [b_omlb], [b_omlb])
        fw.op(dve, lambda: nc.vector.memset(omlb_p[:, 0, :], 1.0), [], [b_omlb])
        sl_a = slots.get(); sl_b = slots.get(); sl_c = slots.get()
        rt = A(sl_a)[:, 0:256].rearrange("p (a b) -> p a b", a=4); b_rt = b_ar[sl_a]
        rt2 = A(sl_b)[:, 0:256].rearrange("p (a b) -> p a b", a=4); b_rt2 = b_ar[sl_b]
        for l in (range(2) if 'L' not in SKIP else []):
            lam_init = 0.8 - 0.6 * math.exp(-0.3 * l)
            lp = A(sl_c)[:, 0:256]
            lsrc = lam_d[l:l + 1, :].partition_broadcast(128)
            fw.dma(sp, (lambda lp=lp, lsrc=lsrc: nc.sync.dma_start(out=lp, in_=lsrc)), [], [b_ar[sl_c]])
            ttop(dve, rt[:, 0, :], lp[:, 0:64], lp[:, 64:128], ALU.mult, [b_ar[sl_c]], [b_rt])
            ttop(dve, rt[:, 1, :], lp[:, 128:192], lp[:, 192:256], ALU.mult, [b_ar[sl_c]], [b_rt])
            fw.op(dve, lambda: nc.vector.tensor_reduce(out=rt2[:, 0, 0:2], in_=rt[:, 0:2, :], axis=AX.X, op=ALU.add), [b_rt], [b_rt2])
            actf(rt2[:, 0, 2:4], rt2[:, 0, 0:2], AF.Exp, [b_rt2], [b_rt2])
            ttop(dve, lam_t[:, l, 0:1], rt2[:, 0, 3:4], rt2[:, 0, 2:3], ALU.subtract, [b_rt2], [b_lam])
            tsop(dve, lam_t[:, l, 0:1], lam_t[:, l, 0:1], -lam_init, None, ALU.add, None, [b_lam], [b_lam])
            tsop(dve, dagp[:, l:l + 1], small[:, l, S_DAG:S_DAG + 1], 1.0 - lam_init, None, ALU.mult, None, [b_small], [b_lam])
        slots.put(sl_a, sl_b, sl_c)

        for s in range(n_seq):
            for j in range(n_blk):
                t0 = j * T
                fw.tag = 'xload'
                for tt in (range(4) if 'X' not in SKIP else []):
                    sa = slots.get_run(2); sb_ = sa + 1
                    xt = arena[:, sa:sa + 2, :].rearrange("p a b -> p (a b)")
                    xsrc = x_d[s, t0 + tt * 128:t0 + (tt + 1) * 128, :]
                    fw.dma(sp, (lambda xt=xt, xsrc=xsrc: nc.sync.dma_start(out=xt, in_=xsrc)),
                           [], [b_ar[sa], b_ar[sb_]])
                    for half in range(2):
                        pb = banks.get()
                        for q in range(4):
                            dc = half * 4 + q
                            mm(psum[pb][:, q * 128:(q + 1) * 128], xt[:, dc * 128:(dc + 1) * 128], ident, True, True,
                               [b_ar[sa], b_ar[sb_], b_constA], [b_ps[pb]], inc=(q == 3))
                        for q in range(4):
                            dc = half * 4 + q
                            cpy(dve if (q % 2 == 0 or 'v' in SKIP) else act, xres[:, dc, tt * 128:(tt + 1) * 128], psum[pb][:, q * 128:(q + 1) * 128],
                                [b_ps[pb]], [b_xres[dc]])
                        banks.put(pb)
                    slots.put(sa, sb_)
                for dc in (range(KC) if ('X' not in SKIP and 'b' not in SKIP) else []):
                    cpy(act if dc % 2 == 0 else dve, xbf[:, dc, :], xres[:, dc, :], [b_xres[dc]], [b_xbf[dc]])
                fw.tag = 'rope'
                sa = slots.get(); sb_ = slots.get(); sc = slots.get()
                psrc = pos_d[s:s + 1, t0:t0 + T].partition_broadcast(128)
                fw.dma(sp, (lambda sa=sa, psrc=psrc: nc.sync.dma_start(out=A(sa).bitcast(I32), in_=psrc)),
                       [], [b_ar[sa]])
                cpy(dve, A(sb_), A(sa).bitcast(I32), [b_ar[sa]], [b_ar[sb_]])
                tsop(dve, A(sb_), A(sb_), constA[:, C_INVF:C_INVF + 1], None, ALU.mult, None, [b_ar[sb_], b_constA], [b_ar[sb_]])
                for which, dst in (((0, ropeS), (1, ropeC)) if 'R' not in SKIP else []):
                    if which == 1:
                        tsop(dve, A(sb_), A(sb_), float(np.pi / 2), None, ALU.add, None, [b_ar[sb_]], [b_ar[sb_]])
                    tsop(dve, A(sa).bitcast(I32), A(sb_), float(1.0 / (2 * np.pi)), None, ALU.mult, None, [b_ar[sb_]], [b_ar[sa]])
                    cpy(dve, A(sc), A(sa).bitcast(I32), [b_ar[sa]], [b_ar[sc]])
                    sttop(dve, A(sc), A(sc), float(-2 * np.pi), A(sb_), ALU.mult, ALU.add, [b_ar[sc], b_ar[sb_]], [b_ar[sc]])
                    if which == 0:
                        actf(dst[:], A(sc), AF.Sin, [b_ar[sc], b_constA], [b_rope], scale=constA[:, C_SGN:C_SGN + 1])
                    else:
                        actf(dst[:], A(sc), AF.Sin, [b_ar[sc]], [b_rope])
                slots.put(sa, sb_, sc)

                for l in layers:
                    key = (s, j, l)
                    sm = small[:, l, :]
                    emit_layer(locals())
                fw.tag = 'outstore'
                for tt in (range(4) if 'O' not in SKIP else []):
                    sa = slots.get_run(2); sb_ = sa + 1
                    ot = arena[:, sa:sa + 2, :].rearrange("p a b -> p (a b)")
                    for half in range(2):
                        pb = banks.get()
                        for q in range(4):
                            dc = half * 4 + q
                            mm(psum[pb][:, q * 128:(q + 1) * 128], xres[:, dc, tt * 128:(tt + 1) * 128], ident, True, True,
                               [b_xres[dc], b_constA], [b_ps[pb]], inc=(q == 3))
                        cpy(dve if half == 0 else act, ot[:, half * 512:(half + 1) * 512], psum[pb][:, :], [b_ps[pb]], [b_ar[sa + half]])
                        banks.put(pb)
                    bo = Buf("out")
                    odst = out_d[s, t0 + tt * 128:t0 + (tt + 1) * 128, :]
                    fw.dma(sp, (lambda ot=ot, odst=odst: nc.sync.dma_start(out=odst, in_=ot)),
                           [b_ar[sa], b_ar[sb_]], [bo])
                    final_bufs.append(bo)
                    slots.put(sa, sb_)
        assert stop < 99 or wstate["consumed"] == len(wlist), (wstate["consumed"], len(wlist))
        fw.finish(final_bufs)
    nc._pe_tags = fw.pe_tags
    nc._pe_names = fw.pe_names
    return nc


TAP_SHAPES = {
    "ya": ((128, 4, T), BF16), "yb": ((128, 4, T), BF16), "yc": ((128, 4, T), BF16),
    "x1": ((128, KC, T), F32), "x2": ((128, KC, T), F32), "mix": ((128, KC, T), F32),
    "q": ((128, 4, T), BF16), "acc": ((128, 4, D), F32), "comb": ((128, 4, 32), F32),
    "oh": ((128, 4, T), F32),
}


def emit_layer(E):
    g = E
    nc, fw = g["nc"], g["fw"]
    pe, act, dve, pool, sp = g["pe"], g["act"], g["dve"], g["pool"], g["sp"]
    mm, actf, ttop, tsop, sttop, cpy, tap = g["mm"], g["actf"], g["ttop"], g["tsop"], g["sttop"], g["cpy"], g["tap"]
    banks, slots, psum, b_ps, arena, b_ar = g["banks"], g["slots"], g["psum"], g["b_ps"], g["arena"], g["b_ar"]
    A, AB = g["A"], g["AB"]
    xres, b_xres, xbf, b_xbf = g["xres"], g["b_xres"], g["xbf"], g["b_xbf"]
    constA, b_constA, cbf, b_cbf, small, b_small = g["constA"], g["b_constA"], g["cbf"], g["b_cbf"], g["small"], g["b_small"]
    ident, M1, M2, M3, onesf, identb, permb, M3b, onesb = (g[k] for k in ("ident", "M1", "M2", "M3", "onesf", "identb", "permb", "M3b", "onesb"))
    epst, b_eps = g["epst"], g["b_eps"]
    w_get, w_put = g["w_get"], g["w_put"]
    l, j, s, key, sm = g["l"], g["j"], g["s"], g["key"], g["sm"]
    n_exp = g["n_exp"]
    t0 = j * T
    KTl, b_KTl, Vl, b_Vl = g["KT"][l], g["b_KT"][l], g["Vh"][l], g["b_V"][l]
    hstl, b_hstl = g["hst"][l], g["b_hst"][l]
    ub, b_ub = g["ubuf"][l], g["b_ubuf"][l]
    ropeC, ropeS, b_rope = g["ropeC"], g["ropeS"], g["b_rope"]
    dec_t, b_dec = g["dec_t"], g["b_dec"]
    comb, b_comb = g["comb"], g["b_comb"]
    omlb_p, omlb_row, b_omlb, lam_t, b_lam, dagp = g["omlb_p"], g["omlb_row"], g["b_omlb"], g["lam_t"], g["b_lam"], g["dagp"]
    XB = b_xbf
    stop = g["stop"]
    if stop <= 0:
        return

    def fm_proj(ws, oc, pb):
        for kc in range(KC):
            mm(psum[pb][:, :], ws[1][:, kc, oc * 128:(oc + 1) * 128], xbf[:, kc, :], kc == 0, kc == KC - 1,
               [g["b_wsl"][ws[0]], XB[kc]], [b_ps[pb]], inc=(kc == KC - 1))

    def tm_proj(ws, tt, pb):
        for kc in range(KC):
            mm(psum[pb][:, :], xbf[:, kc, tt * 128:(tt + 1) * 128], ws[1][:, kc, :], kc == 0, kc == KC - 1,
               [g["b_wsl"][ws[0]], XB[kc]], [b_ps[pb]], inc=(kc == KC - 1))

    def bc_stats(srcs, R, n_total):
        p1 = banks.get(); p2 = banks.get()
        n = len(srcs)
        sq = [slots.get(), slots.get()]
        for i, (ap, rb) in enumerate(zip(srcs, R)):
            mm(psum[p1][:, :], onesf, ap, i == 0, i == n - 1, rb + [b_constA], [b_ps[p1]], inc=(i == n - 1))
        for i, (ap, rb) in enumerate(zip(srcs, R)):
            q = sq[i % 2]
            fw.op(act, (lambda q=q, ap=ap: nc.scalar.activation(out=A(q), in_=ap, func=AF.Square)), rb, [b_ar[q]])
            mm(psum[p2][:, :], onesf, A(q), i == 0, i == n - 1, [b_ar[q], b_constA], [b_ps[p2]], inc=True)
        slots.put(*sq)
        m = slots.get(); r = slots.get()
        fw.op(act, lambda: nc.scalar.mul(A(m), psum[p1][:, :], 1.0 / n_total), [b_ps[p1]], [b_ar[m]])
        ttop(dve, A(r), A(m), A(m), ALU.mult, [b_ar[m]], [b_ar[r]])
        sttop(dve, A(r), psum[p2][:, :], 1.0 / n_total, A(r), ALU.mult, ALU.subtract, [b_ps[p2], b_ar[r]], [b_ar[r]])
        actf(A(r), A(r), AF.Sqrt, [b_ar[r], b_eps], [b_ar[r]], bias=epst[:, 0:1], scale=1.0)
        fw.op(dve, lambda: nc.vector.reciprocal(A(r), A(r)), [b_ar[r]], [b_ar[r]])
        banks.put(p1, p2)
        return m, r

    _y0 = slots.get_run(6)
    ysl = list(range(_y0, _y0 + 6))

    def ytile(n):
        return arena[:, ysl[2 * n]:ysl[2 * n] + 2, :].bitcast(BF16).rearrange("p a (h t) -> p (a h) t", h=2)

    def ybufs(n, c):
        return [b_ar[ysl[2 * n + c // 2]]]
    ya, yb, yc = ytile(0), ytile(1), ytile(2)

    fw.tag = 'conv_proj'
    wa1 = w_get(8); wa2 = w_get(8)
    if j == 0:
        for c in range(4):
            fw.op(dve, (lambda c=c: nc.vector.memset(ub[:, c, 0:HALO], 0.0)), [], [b_ub[c]])
    for c in range(4):
        pa = banks.get(); pg = banks.get()
        fm_proj(wa1, c, pa)
        fm_proj(wa2, c, pg)
        sg = slots.get()
        actf(A(sg), psum[pg][:, :], AF.Sigmoid, [b_ps[pg]], [b_ar[sg]])
        ttop(dve, ub[:, c, HALO:HALO + T], psum[pa][:, :], A(sg), ALU.mult, [b_ps[pa], b_ar[sg]], [b_ub[c]])
        slots.put(sg); banks.put(pa, pg)
    w_put(wa1[0]); w_put(wa2[0])
    fw.tag = 'conv'
    ycs = [slots.get() for _ in range(4)]
    dgs = [list(range(a_, a_ + 4)) for a_ in (slots.get_run(4), slots.get_run(4))]
    for c in range(4):
        dsl = dgs[c % 2]
        assert dsl == list(range(dsl[0], dsl[0] + 4))
        dg = arena[:, dsl[0]:dsl[0] + 4, :].bitcast(BF16).rearrange("p a b -> p (a b)")[:, 0:CONV_K * 128].rearrange("p (k m) -> p k m", k=CONV_K)
        dgb = [b_ar[i] for i in dsl]
        wv = sm[:, S_CONVW + c * CONV_K:S_CONVW + (c + 1) * CONV_K]
        fw.op(dve, (lambda dg=dg, wv=wv: nc.vector.tensor_tensor(dg, identb.unsqueeze(1).to_broadcast([128, CONV_K, 128]),
                                                                   wv.unsqueeze(2).to_broadcast([128, CONV_K, 128]), ALU.mult)),
              [b_cbf, b_small], dgb)
        pc = banks.get()
        for k in range(CONV_K):
            mm(psum[pc][:, :], dg[:, k, :], ub[:, c, k:k + T], k == 0, k == CONV_K - 1, dgb + [b_ub[c]], [b_ps[pc]], inc=(k == CONV_K - 1))
        actf(A(ycs[c]), psum[pc][:, :], AF.Identity, [b_ps[pc], b_small], [b_ar[ycs[c]]], bias=sm[:, S_CONVB + c:S_CONVB + c + 1], scale=1.0)
        banks.put(pc)
        cpy(pool, ub[:, c, 0:HALO], ub[:, c, T:T + HALO], [b_ub[c]], [b_ub[c]])
    for d_ in dgs:
        slots.put(*d_)
    fw.tag = 'conv_ln'
    m_, r_ = bc_stats([A(i) for i in ycs], [[b_ar[i]] for i in ycs], 512.0)
    for c in range(4):
        ttop(dve, A(ycs[c]), A(ycs[c]), A(m_), ALU.subtract, [b_ar[ycs[c]], b_ar[m_]], [b_ar[ycs[c]]])
        ttop(dve, A(ycs[c]), A(ycs[c]), A(r_), ALU.mult, [b_ar[ycs[c]], b_ar[r_]], [b_ar[ycs[c]]])
        actf(ya[:, c, :], A(ycs[c]), AF.Silu, [b_ar[ycs[c]], b_small], ybufs(0, c),
             bias=sm[:, S_CLNB + c:S_CLNB + c + 1], scale=sm[:, S_CLNG + c:S_CLNG + c + 1])
    slots.put(m_, r_); slots.put(*ycs)
    tap("ya", key, ya, ybufs(0, 0) + ybufs(0, 2))
    if stop <= 1:
        return

    fw.tag = 'att_qkv'
    wq = w_get(8); wk = w_get(8)
    _q0 = slots.get_run(4)
    qsl = list(range(_q0, _q0 + 4))
    qz = arena[:, _q0:_q0 + 4, :].bitcast(BF16).rearrange("p a (s t) -> p a s t", s=2)
    qTb = lambda h: [b_ar[qsl[h]]]
    for h in range(4):
        fw.op(pool, (lambda h=h: nc.gpsimd.memset(arena[:, qsl[h], :], 0.0)), [], qTb(h))
    jobs = [(wq, h, 0) for h in range(4)] + [(wk, h, 1) for h in range(4)]
    rst = {}

    def rope_s1(i):
        ws, h, kind = jobs[i]
        pq = banks.get()
        fm_proj(ws, h, pq)
        tb = slots.get()
        cpy(act, AB(tb)[:, 0:T], psum[pq][:, :], [b_ps[pq]], [b_ar[tb]])
        rst[i] = (pq, tb)
        if i == 3:
            w_put(wq[0])
        if i == 7:
            w_put(wk[0])

    def rope_s2(i):
        ws, h, kind = jobs[i]
        pq, tb = rst.pop(i)
        pp = banks.get()
        t1 = slots.get(); t2 = slots.get()
        mm(psum[pp][:, :], permb, AB(tb)[:, 0:T], True, True, [b_cbf, b_ar[tb]], [b_ps[pp]])
        ttop(dve, A(t1), psum[pq][:, :], ropeC[:], ALU.mult, [b_ps[pq], b_rope], [b_ar[t1]])
        ttop(dve, A(t2), psum[pp][:, :], ropeS[:], ALU.mult, [b_ps[pp], b_rope], [b_ar[t2]])
        if kind == 0:
            ttop(dve, qz[0:64, h, 0, :], A(t1)[0:64, :], A(t2)[0:64, :], ALU.add, [b_ar[t1], b_ar[t2]], qTb(h))
            ttop(dve, qz[64:128, h, 1, :], A(t1)[64:128, :], A(t2)[64:128, :], ALU.add, [b_ar[t1], b_ar[t2]], qTb(h))
        else:
            ttop(dve, KTl[:, h, t0:t0 + T], A(t1), A(t2), ALU.add, [b_ar[t1], b_ar[t2]], [b_KTl[h][j]])
        slots.put(tb, t1, t2); banks.put(pq, pp)
    rope_s1(0)
    for i in range(8):
        if i + 1 < 8:
            rope_s1(i + 1)
        rope_s2(i)
    wv = w_get(8)
    for tt in range(4):
        pv = banks.get()
        tm_proj(wv, tt, pv)
        cpy(act if tt % 2 == 0 else dve, Vl[:, 4 * j + tt, :], psum[pv][:, :], [b_ps[pv]], [b_Vl[4 * j + tt]])
        banks.put(pv)
    w_put(wv[0])
    fw.tag = 'att_core'
    nkb = 4 * (j + 1)
    ohs = [slots.get() for _ in range(4)]
    for h in range(4):
        po = [banks.get(), banks.get()]
        pS = [banks.get(), banks.get()]
        items = [(p, kb) for kb in range(nkb) for p in range(2)]
        ptr = [slots.get(), slots.get()]
        pbufs = [(ptr[i // 2], i % 2) for i in range(4)]
        sc_banks = {}

        def score(idx):
            p, kb = items[idx]
            pb = banks.get()
            sc_banks[idx] = pb
            diag = kb >= 4 * j
            mm(psum[pb][:, :], KTl[:, h, kb * 128:(kb + 1) * 128], qz[:, h, p, :], True, not diag,
               [b_KTl[h][kb // 4]] + qTb(h), [b_ps[pb]], inc=not diag)
            if diag:
                mk = cbf[:, CB_MASK + (kb - 4 * j) * 512:CB_MASK + (kb - 4 * j + 1) * 512]
                mm(psum[pb][:, :], identb, mk, False, True, [b_cbf], [b_ps[pb]])
        score(0)
        for idx in range(len(items)):
            p, kb = items[idx]
            if idx + 1 < len(items):
                score(idx + 1)
            pb = sc_banks.pop(idx)
            sl, hf = pbufs[idx % 4]
            PT = AB(sl)[:, hf * T:(hf + 1) * T]
            actf(PT, psum[pb][:, :], AF.Exp, [b_ps[pb]], [b_ar[sl]], scale=0.125)
            banks.put(pb)
            mm(psum[po[p]][:, :], Vl[:, kb, h * 128:(h + 1) * 128], PT, kb == 0, kb == nkb - 1, [b_Vl[kb], b_ar[sl]], [b_ps[po[p]]], inc=False)
            mm(psum[pS[p]][:, :], onesb, PT, kb == 0, kb == nkb - 1, [b_cbf, b_ar[sl]], [b_ps[pS[p]]], inc=True)
        slots.put(*ptr)
        r1 = slots.get(); r2 = slots.get()
        fw.op(dve, (lambda r1=r1, pS=pS: nc.vector.reciprocal(A(r1), psum[pS[0]][:, :])), [b_ps[pS[0]]], [b_ar[r1]])
        fw.op(dve, (lambda r2=r2, pS=pS: nc.vector.reciprocal(A(r2), psum[pS[1]][:, :])), [b_ps[pS[1]]], [b_ar[r2]])
        ttop(dve, A(r1), psum[po[0]][:, :], A(r1), ALU.mult, [b_ps[po[0]], b_ar[r1]], [b_ar[r1]])
        ttop(dve, A(r2), psum[po[1]][:, :], A(r2), ALU.mult, [b_ps[po[1]], b_ar[r2]], [b_ar[r2]])
        sttop(dve, A(ohs[h]), A(r2), lam_t[:, l, 0:1], A(r1), ALU.mult, ALU.add, [b_ar[r1], b_ar[r2], b_lam], [b_ar[ohs[h]]])
        slots.put(r1, r2); banks.put(*po); banks.put(*pS)
    fw.tag = 'att_norm'
    for h in range(4):
        sq = slots.get(); pm = banks.get()
        fw.op(act, (lambda sq=sq, h=h: nc.scalar.activation(out=A(sq), in_=A(ohs[h]), func=AF.Square)), [b_ar[ohs[h]]], [b_ar[sq]])
        mm(psum[pm][:, :], onesf, A(sq), True, True, [b_ar[sq], b_constA], [b_ps[pm]])
        actf(A(sq), psum[pm][:, :], AF.Sqrt, [b_ps[pm], b_eps], [b_ar[sq]], bias=epst[:, 0:1], scale=1.0 / 128)
        fw.op(dve, (lambda sq=sq: nc.vector.reciprocal(A(sq), A(sq))), [b_ar[sq]], [b_ar[sq]])
        sttop(dve, yb[:, h, :], A(ohs[h]), dagp[:, l:l + 1], A(sq), ALU.mult, ALU.mult, [b_ar[ohs[h]], b_ar[sq], b_lam], ybufs(1, h))
        slots.put(sq); banks.put(pm)
    slots.put(*ohs); slots.put(*qsl)
    tap("yb", key, yb, ybufs(1, 0) + ybufs(1, 2))
    if stop <= 2:
        return

    fw.tag = 'hg_proj'
    def bf4(n0):
        return arena[:, n0:n0 + 2, :].bitcast(BF16).rearrange("p a (h t) -> p (a h) t", h=2)

    def two():
        return slots.get_run(2)
    s_qs, s_kT, s_qb, s_kd = (two() for _ in range(4))
    qs, kTt, qb, kdec = (bf4(x) for x in (s_qs, s_kT, s_qb, s_kd))
    B2 = lambda s0: [b_ar[s0], b_ar[s0 + 1]]
    Bh = lambda s0, h: [b_ar[s0 + h // 2]]
    attm = [slots.get(), slots.get()]
    wqh = w_get(8)
    for h in range(4):
        pb = banks.get()
        fm_proj(wqh, h, pb)
        actf(qs[:, h, :], psum[pb][:, :], AF.Silu, [b_ps[pb]], Bh(s_qs, h))
        banks.put(pb)
    w_put(wqh[0])
    wfh = w_get(8)
    for h in range(4):
        pb = banks.get(); tmp = slots.get()
        fm_proj(wfh, h, pb)
        actf(A(tmp), psum[pb][:, :], AF.Sigmoid, [b_ps[pb]], [b_ar[tmp]], scale=-1.0)
        tsop(dve, kTt[:, h, :], A(tmp), omlb_p[:, l, h:h + 1], None, ALU.mult, None, [b_ar[tmp], b_omlb], Bh(s_kT, h))
        banks.put(pb); slots.put(tmp)
    if j == 0:
        fw.op(dve, lambda: nc.vector.memset(hstl[:], 0.0), [], [b_hstl])
    fw.tag = 'hg_prep'
    v3 = lambda ap: ap.rearrange("p (h t) -> p h t", h=4)
    for tt in range(4):
        tsl = slice(tt * 128, (tt + 1) * 128)
        kt_ = slots.get(); lf_ = slots.get()
        pb = banks.get()
        tm_proj(wfh, tt, pb)
        actf(A(kt_), psum[pb][:, :], AF.Sigmoid, [b_ps[pb]], [b_ar[kt_]], scale=-1.0)
        banks.put(pb)
        if l == 1:
            ttop(dve, A(kt_), A(kt_), omlb_row[:], ALU.mult, [b_ar[kt_], b_omlb], [b_ar[kt_]])
        actf(A(lf_), A(kt_), AF.Ln, [b_ar[kt_]], [b_ar[lf_]], scale=-1.0, bias=1.0)
        pb = banks.get(); e2 = slots.get()
        mm(psum[pb][:, :], M2, A(lf_), True, True, [b_constA, b_ar[lf_]], [b_ps[pb]])
        actf(A(e2), psum[pb][:, :], AF.Exp, [b_ps[pb]], [b_ar[e2]])
        ttop(dve, kdec[:, tt, :], A(kt_), A(e2), ALU.mult, [b_ar[kt_], b_ar[e2]], Bh(s_kd, tt))
        banks.put(pb); slots.put(e2); slots.put(kt_)
        p1 = banks.get(); p3 = banks.get()
        for h in range(4):
            mm(psum[p1][:, h * 128:(h + 1) * 128], A(lf_)[:, h * 128:(h + 1) * 128], M1, True, True,
               [b_ar[lf_], b_constA], [b_ps[p1]], inc=(h == 3))
        for h in range(4):
            mm(psum[p3][:, h * 128:(h + 1) * 128], A(lf_)[:, h * 128:(h + 1) * 128], M3, True, True,
               [b_ar[lf_], b_constA], [b_ps[p3]], inc=(h == 3))
        slots.put(lf_)
        c1 = slots.get(); e1 = slots.get(); e3 = slots.get(); qk = slots.get()
        tsop(dve, A(c1), psum[p1][:, :], CLAMP, -CLAMP, ALU.min, ALU.max, [b_ps[p1]], [b_ar[c1]])
        actf(A(e1), A(c1), AF.Exp, [b_ar[c1]], [b_ar[e1]])
        actf(A(c1), A(c1), AF.Exp, [b_ar[c1]], [b_ar[c1]], scale=-1.0)
        actf(A(e3), psum[p3][:, :], AF.Exp, [b_ps[p3]], [b_ar[e3]])
        banks.put(p1, p3)
        qt = v3(AB(qk)[:, 0:512]); kt = v3(AB(qk)[:, 512:1024])
        ttop(dve, qt, qs[:, :, tsl], v3(A(e1)), ALU.mult, B2(s_qs) + [b_ar[e1]], [b_ar[qk]])
        ttop(dve, kt, kTt[:, :, tsl], v3(A(c1)), ALU.mult, B2(s_kT) + [b_ar[c1]], [b_ar[qk]])
        ttop(dve, qb[:, :, tsl], qs[:, :, tsl], v3(A(e3)), ALU.mult, B2(s_qs) + [b_ar[e3]], B2(s_qb))
        e3v = A(e3).rearrange("p (h c t) -> p h c t", h=4, c=4)[:, :, :, 31]
        cpy(dve, dec_t[:, :, 4 * tt:4 * tt + 4], e3v, [b_ar[e3]], [b_dec])
        slots.put(c1, e1, e3)
        pa = banks.get()
        for h in range(4):
            mm(psum[pa][:, h * 128:(h + 1) * 128], kt[:, h, :], qt[:, h, :], True, True, [b_ar[qk]], [b_ps[pa]], inc=(h == 3))
        slots.put(qk)
        asl = attm[tt // 2]
        am = AB(asl)[:, (tt % 2) * 512:(tt % 2) * 512 + 512].rearrange("p (h t) -> p h t", h=4)
        fw.op(dve, (lambda am=am, pa=pa: nc.vector.tensor_tensor(am, psum[pa][:, :].rearrange("p (h t) -> p h t", h=4),
                                                                  M3.unsqueeze(1).to_broadcast([128, 4, 128]), ALU.mult)),
              [b_ps[pa], b_constA], [b_ar[asl]])
        banks.put(pa)
    w_put(wfh[0])
    slots.put_run(s_qs, 2); slots.put_run(s_kT, 2)
    fw.tag = 'hg_proj2'
    s_it, s_gs = two(), two()
    itok, gsil = bf4(s_it), bf4(s_gs)
    wih = w_get(8)
    for tt in range(4):
        pb = banks.get()
        tm_proj(wih, tt, pb)
        cpy(act if tt % 2 else dve, itok[:, tt, :], psum[pb][:, :], [b_ps[pb]], Bh(s_it, tt))
        banks.put(pb)
    w_put(wih[0])
    wgh = w_get(8)
    for h in range(4):
        pb = banks.get()
        fm_proj(wgh, h, pb)
        actf(gsil[:, h, :], psum[pb][:, :], AF.Silu, [b_ps[pb]], Bh(s_gs, h))
        banks.put(pb)
    w_put(wgh[0])
    fw.tag = 'hg_rec'
    _o0 = slots.get_run(4)
    osl = list(range(_o0, _o0 + 4))
    sbf = [slots.get(), slots.get()]
    kdm = [slots.get() for _ in range(4)]
    cur = 0
    cpy(act, AB(sbf[cur])[:, 0:512], hstl[:].rearrange("p h v -> p (h v)"), [b_hstl], [b_ar[sbf[cur]]])
    for tt in range(4):
        po_ = banks.get()
        asl = attm[tt // 2]
        am = AB(asl)[:, (tt % 2) * 512:(tt % 2) * 512 + 512].rearrange("p (h t) -> p h t", h=4)
        pus = []
        for c in range(4):
            km = kdm[c]
            kmv = AB(km)[:, 0:512]
            actf(kmv, kdec[:, tt, :], AF.Identity, Bh(s_kd, tt) + [b_constA], [b_ar[km]], scale=constA[:, C_CM + c:C_CM + c + 1])
            pu = banks.get()
            pus.append(pu)
            for h in range(4):
                mm(psum[pu][:, h * 128:(h + 1) * 128], kmv[:, h * 128:(h + 1) * 128], itok[:, tt, h * 128:(h + 1) * 128], True, True,
                   [b_ar[km]] + Bh(s_it, tt), [b_ps[pu]], inc=(h == 3))
        for c in range(4):
            cc = 4 * tt + c
            pu = pus[c]
            for h in range(4):
                oc_ = psum[po_][:, h * 128 + 32 * c:h * 128 + 32 * c + 32]
                mm(oc_, AB(sbf[cur])[:, h * 128:(h + 1) * 128], qb[:, h, tt * 128 + 32 * c:tt * 128 + 32 * c + 32], True, False,
                   [b_ar[sbf[cur]]] + B2(s_qb), [b_ps[po_]], inc=False)
                mm(oc_, itok[:, tt, h * 128:(h + 1) * 128], am[:, h, 32 * c:32 * c + 32], False, True,
                   Bh(s_it, tt) + [b_ar[asl]], [b_ps[po_]], inc=(h == 3))
            for h in range(4):
                sttop(dve, hstl[:, h, :], hstl[:, h, :], dec_t[:, h, cc:cc + 1], psum[pu][:, h * 128:(h + 1) * 128], ALU.mult, ALU.add,
                      [b_hstl, b_dec, b_ps[pu]], [b_hstl])
            banks.put(pu)
            cur ^= 1
            cpy(act, AB(sbf[cur])[:, 0:512], hstl[:].rearrange("p h v -> p (h v)"), [b_hstl], [b_ar[sbf[cur]]])
        for h in range(4):
            cpy(act if h % 2 else dve, A(osl[h])[:, tt * 128:(tt + 1) * 128], psum[po_][:, h * 128:(h + 1) * 128], [b_ps[po_]], [b_ar[osl[h]]])
        banks.put(po_)
    slots.put(*sbf); slots.put(*kdm); slots.put(*attm)
    fw.tag = 'hg_norm'
    tap("oh", key, arena[:, osl[0]:osl[0] + 4, :], [b_ar[i] for i in osl])
    for h in range(4):
        sq = slots.get(); pm = banks.get()
        fw.op(act, (lambda sq=sq, h=h: nc.scalar.activation(out=A(sq), in_=A(osl[h]), func=AF.Square)), [b_ar[osl[h]]], [b_ar[sq]])
        mm(psum[pm][:, :], onesf, A(sq), True, True, [b_ar[sq], b_constA], [b_ps[pm]])
        actf(A(sq), psum[pm][:, :], AF.Sqrt, [b_ps[pm], b_eps], [b_ar[sq]], bias=epst[:, 0:1], scale=1.0 / 128)
        fw.op(dve, (lambda sq=sq: nc.vector.reciprocal(A(sq), A(sq))), [b_ar[sq]], [b_ar[sq]])
        sttop(dve, A(sq), A(osl[h]), sm[:, S_HGG:S_HGG + 1], A(sq), ALU.mult, ALU.mult, [b_ar[osl[h]], b_ar[sq], b_small], [b_ar[sq]])
        ttop(dve, yc[:, h, :], A(sq), gsil[:, h, :], ALU.mult, [b_ar[sq]] + Bh(s_gs, h), ybufs(2, h))
        slots.put(sq); banks.put(pm)
    slots.put(*osl)
    for s0 in (s_it, s_gs, s_qb, s_kd):
        slots.put(s0, s0 + 1)
    tap("yc", key, yc, ybufs(2, 0) + ybufs(2, 2))
    if stop <= 3:
        return

    fw.tag = 'merge'
    _m0 = slots.get_run(8)
    mixs = list(range(_m0, _m0 + 8))
    ys = [ya, yb, yc]
    for n in range(3):
        wb = w_get(4); wg0 = w_get(8); wg1 = w_get(8)
        for dc in range(KC):
            wgx = wg0 if dc < 4 else wg1
            pp = banks.get(); pg = banks.get()
            for kc in range(4):
                mm(psum[pp][:, :], wb[1][:, kc, dc * 128:(dc + 1) * 128], ys[n][:, kc, :], kc == 0, kc == 3,
                   [g["b_wsl"][wb[0]]] + ybufs(n, kc), [b_ps[pp]], inc=(kc == 3))
            fm_proj(wgx, dc % 4, pg)
            sg = slots.get()
            actf(A(sg), psum[pg][:, :], AF.Sigmoid, [b_ps[pg], b_small], [b_ar[sg]],
                 bias=sm[:, S_BGATE + n * 8 + dc:S_BGATE + n * 8 + dc + 1], scale=1.0)
            if n == 0:
                ttop(dve, A(mixs[dc]), psum[pp][:, :], A(sg), ALU.mult, [b_ps[pp], b_ar[sg]], [b_ar[mixs[dc]]])
            else:
                ttop(dve, A(sg), psum[pp][:, :], A(sg), ALU.mult, [b_ps[pp], b_ar[sg]], [b_ar[sg]])
                ttop(dve, A(mixs[dc]), A(mixs[dc]), A(sg), ALU.add, [b_ar[mixs[dc]], b_ar[sg]], [b_ar[mixs[dc]]])
            slots.put(sg); banks.put(pp, pg)
        w_put(wb[0]); w_put(wg0[0]); w_put(wg1[0])
    slots.put(*ysl)
    tap("mix", key, arena[:, mixs[0]:mixs[0] + 8, :], [b_ar[i] for i in mixs])
    _mb = slots.get_run(4)
    mb = [_mb, _mb + 2]
    mixb = arena[:, mb[0]:mb[0] + 4, :].bitcast(BF16).rearrange("p a (h t) -> p (a h) t", h=2)
    mixbB = lambda dc: [b_ar[mb[0] + dc // 2]]
    for dc in range(KC):
        cpy(act if dc % 2 else dve, mixb[:, dc, :], A(mixs[dc]), [b_ar[mixs[dc]]], mixbB(dc))
    slots.put(*mixs)
    fw.tag = 'outproj'
    for hf in range(2):
        wo = w_get(8)
        for oc in range(4):
            dc = hf * 4 + oc
            pb = banks.get()
            for kc in range(KC):
                mm(psum[pb][:, :], wo[1][:, kc, oc * 128:(oc + 1) * 128], mixb[:, kc, :], kc == 0, kc == KC - 1,
                   [g["b_wsl"][wo[0]]] + mixbB(kc), [b_ps[pb]], inc=(kc == KC - 1))
            sttop(dve, xres[:, dc, :], xres[:, dc, :], float(DN_ALPHA), psum[pb][:, :], ALU.mult, ALU.add, [b_xres[dc], b_ps[pb]], [b_xres[dc]])
            banks.put(pb)
        w_put(wo[0])
    for m0 in mb:
        slots.put(m0, m0 + 1)

    def layer_norm_res(goff, boff):
        m_, r_ = bc_stats([xres[:, dc, :] for dc in range(KC)], [[b_xres[dc]] for dc in range(KC)], float(D))
        for dc in range(KC):
            ttop(dve, xres[:, dc, :], xres[:, dc, :], A(m_), ALU.subtract, [b_xres[dc], b_ar[m_]], [b_xres[dc]])
            ttop(dve, xres[:, dc, :], xres[:, dc, :], A(r_), ALU.mult, [b_xres[dc], b_ar[r_]], [b_xres[dc]])
            actf(xbf[:, dc, :], xres[:, dc, :], AF.Identity, [b_xres[dc], b_small], [b_xbf[dc]],
                 bias=sm[:, boff + dc:boff + dc + 1], scale=sm[:, goff + dc:goff + dc + 1])
            actf(xres[:, dc, :], xres[:, dc, :], AF.Identity, [b_xres[dc], b_small], [b_xres[dc]],
                 bias=sm[:, boff + dc:boff + dc + 1], scale=sm[:, goff + dc:goff + dc + 1])
        slots.put(m_, r_)
    fw.tag = 'ln1'
    layer_norm_res(S_LN1G, S_LN1B)
    tap("x1", key, xres[:], b_xres)
    if stop <= 4:
        return

    fw.tag = 'router'
    sl_a = slots.get(); sl_b = slots.get(); sl_c = slots.get()
    rt = A(sl_a)[:, 0:256].rearrange("p (a b) -> p a b", a=4); b_rt = b_ar[sl_a]
    rt2 = A(sl_b)[:, 0:256].rearrange("p (a b) -> p a b", a=4); b_rt2 = b_ar[sl_b]
    rt3 = A(sl_c)[:, 0:256].rearrange("p (a b) -> p a b", a=4); b_rt3 = b_ar[sl_c]
    pr = banks.get()
    wr = sm[:, S_WR:S_WR + 288].rearrange("p (k c) -> p k c", k=8)
    for tt in range(4):
        for kc in range(KC):
            mm(psum[pr][:, tt * 64:tt * 64 + 36], xres[:, kc, tt * 128:(tt + 1) * 128], wr[:, kc, :], kc == 0, kc == KC - 1,
               [b_xres[kc], b_small], [b_ps[pr]], inc=(kc == KC - 1))
    L = rt[:, :, 0:36]
    prv = psum[pr][:, 0:256].rearrange("p (t c) -> p t c", c=64)[:, :, 0:36]
    rbv = sm[:, S_RB:S_RB + 36].unsqueeze(1).to_broadcast([128, 4, 36])
    ttop(dve, L, prv, rbv, ALU.add, [b_ps[pr], b_small], [b_rt])
    banks.put(pr)
    gl = rt[:, :, 0:4]
    El = rt[:, :, 4:36]
    gmax = rt2[:, :, 0:1]
    fw.op(dve, lambda: nc.vector.tensor_reduce(out=rt2[:, :, 0], in_=gl, axis=AX.X, op=ALU.max), [b_rt], [b_rt2])
    ge = rt2[:, :, 4:8]
    ttop(dve, ge, gl, gmax.to_broadcast([128, 4, 4]), ALU.subtract, [b_rt, b_rt2], [b_rt2])
    oneh = rt2[:, :, 8:12]
    tsop(dve, oneh, ge, 0.0, None, ALU.is_ge, None, [b_rt2], [b_rt2])
    actf(ge, ge, AF.Exp, [b_rt2], [b_rt2])
    fw.op(dve, lambda: nc.vector.tensor_reduce(out=rt2[:, :, 1], in_=ge, axis=AX.X, op=ALU.add), [b_rt2], [b_rt2])
    fw.op(dve, lambda: nc.vector.reciprocal(rt2[:, :, 2], rt2[:, :, 1]), [b_rt2], [b_rt2])
    pen = rt2[:, :, 12:16]
    tsop(dve, pen, oneh, 1e30, -1e30, ALU.mult, ALU.add, [b_rt2], [b_rt2])
    Em = rt3[:, :, 0:32]
    ttop(dve, Em.rearrange("p t (g e) -> p t g e", g=4), El.rearrange("p t (g e) -> p t g e", g=4),
         pen.unsqueeze(3).to_broadcast([128, 4, 4, 8]), ALU.add, [b_rt, b_rt2], [b_rt3])
    top = rt3[:, :, 32:40]
    for tt in range(4):
        fw.op(dve, (lambda tt=tt: nc.vector.max(out=rt3[:, tt, 32:40], in_=rt3[:, tt, 0:32])), [b_rt3], [b_rt3])
    wts = rt2[:, :, 16:18]
    ttop(dve, rt2[:, :, 18:19], rt3[:, :, 32:33], rt3[:, :, 33:34], ALU.subtract, [b_rt3], [b_rt2])
    actf(rt2[:, :, 18:19], rt2[:, :, 18:19], AF.Sigmoid, [b_rt2], [b_rt2])
    ttop(dve, rt2[:, :, 16:17], rt2[:, :, 18:19], rt2[:, :, 2:3], ALU.mult, [b_rt2], [b_rt2])
    ttop(dve, rt2[:, :, 17:18], rt2[:, :, 2:3], rt2[:, :, 16:17], ALU.subtract, [b_rt2], [b_rt2])
    eq = rt[:, :, 0:32]
    ttop(dve, eq, Em, rt3[:, :, 32:33].to_broadcast([128, 4, 32]), ALU.is_equal, [b_rt3], [b_rt])
    ttop(dve, comb[:], eq, rt2[:, :, 16:17].to_broadcast([128, 4, 32]), ALU.mult, [b_rt, b_rt2], [b_comb])
    ttop(dve, eq, Em, rt3[:, :, 33:34].to_broadcast([128, 4, 32]), ALU.is_equal, [b_rt3], [b_rt])
    ttop(dve, eq, eq, rt2[:, :, 17:18].to_broadcast([128, 4, 32]), ALU.mult, [b_rt, b_rt2], [b_rt])
    ttop(dve, comb[:], comb[:], eq, ALU.add, [b_comb, b_rt], [b_comb])
    tap("comb", key, comb[:], [b_comb])
    slots.put(sl_a, sl_b, sl_c)

    fw.tag = 'moe'
    _a0 = slots.get_run(8)
    accs = list(range(_a0, _a0 + 8))
    hts = [two(), two()]
    for e in range(n_exp):
        w1 = w_get(8); w3 = w_get(8)
        h0 = hts[e % 2]
        hT = bf4(h0)
        for oc in range(4):
            p1 = banks.get(); p3 = banks.get()
            fm_proj(w1, oc, p1)
            fm_proj(w3, oc, p3)
            sl = slots.get()
            actf(A(sl), psum[p1][:, :], AF.Silu, [b_ps[p1]], [b_ar[sl]])
            ttop(dve, hT[:, oc, :], A(sl), psum[p3][:, :], ALU.mult, [b_ar[sl], b_ps[p3]], Bh(h0, oc))
            slots.put(sl); banks.put(p1, p3)
        w_put(w1[0]); w_put(w3[0])
        w2 = w_get(4)
        for tt in range(4):
            for hf in range(2):
                pb = banks.get()
                for oc in range(4):
                    mm(psum[pb][:, :], hT[:, oc, tt * 128:(tt + 1) * 128], w2[1][:, oc, hf * 512:(hf + 1) * 512], oc == 0, oc == 3,
                       Bh(h0, oc) + [g["b_wsl"][w2[0]]], [b_ps[pb]], inc=(oc == 3))
                a_ = accs[tt * 2 + hf]
                if e == 0:
                    tsop(dve, A(a_), psum[pb][:, :], comb[:, tt, e:e + 1], None, ALU.mult, None, [b_ps[pb], b_comb], [b_ar[a_]])
                else:
                    sttop(dve, A(a_), psum[pb][:, :], comb[:, tt, e:e + 1], A(a_), ALU.mult, ALU.add, [b_ps[pb], b_comb, b_ar[a_]], [b_ar[a_]])
                banks.put(pb)
        w_put(w2[0])
    for h0 in hts:
        slots.put(h0, h0 + 1)
    tap("acc", key, arena[:, accs[0]:accs[0] + 8, :].rearrange("p (t h) c -> p t (h c)", h=2), [b_ar[i] for i in accs])
    fw.tag = 'moe_T'
    for dc in range(KC):
        pb = banks.get()
        hf = dc // 4
        cs = (dc % 4) * 128
        for tt in range(4):
            a_ = accs[tt * 2 + hf]
            mm(psum[pb][:, tt * 128:(tt + 1) * 128], A(a_)[:, cs:cs + 128], ident, True, True, [b_ar[a_], b_constA], [b_ps[pb]], inc=(tt == 3))
        sttop(dve, xres[:, dc, :], xres[:, dc, :], float(DN_ALPHA), psum[pb][:, :], ALU.mult, ALU.add, [b_xres[dc], b_ps[pb]], [b_xres[dc]])
        banks.put(pb)
    slots.put(*accs)
    fw.tag = 'ln2'
    layer_norm_res(S_LN2G, S_LN2B)
    tap("x2", key, xres[:], b_xres)


def _const_packs():
    ca = np.zeros((128, NCA), np.float32)
    ca[:, C_ID:C_ID + 128] = np.eye(128)
    idx = np.arange(128)
    ch = idx // 32
    same = ch[:, None] == ch[None, :]
    s_le_t = idx[:, None] <= idx[None, :]
    mid = ch * 32 + 15
    s_le_mid = idx[:, None] <= mid[None, :]
    ca[:, C_M1:C_M1 + 128] = same * (s_le_t.astype(np.float32) - s_le_mid.astype(np.float32))
    ca[:, C_M2:C_M2 + 128] = same * (idx[:, None] > idx[None, :])
    ca[:, C_M3:C_M3 + 128] = same * s_le_t
    for c in range(4):
        ca[:, C_CM + c] = (ch == c)
    d = idx % 64
    jj = d % 8
    inv = ROPE_THETA ** (-(2.0 * jj) / 16.0)
    ca[:, C_INVF] = np.where(d < 16, inv, 0.0).astype(np.float32)
    ca[:, C_SGN] = np.where(d < 8, -1.0, np.where(d < 16, 1.0, 0.0))
    ca[:, C_ONES:C_ONES + 128] = 1.0
    cb = np.zeros((128, NCB), np.float32)
    cb[:, CB_ID:CB_ID + 128] = np.eye(128)
    perm = np.zeros((128, 128), np.float32)
    for m in range(128):
        dm = m % 64
        if dm < 8:
            perm[m + 8, m] = 1.0
        elif dm < 16:
            perm[m - 8, m] = 1.0
    cb[:, CB_PERM:CB_PERM + 128] = perm
    cb[:, CB_M3:CB_M3 + 128] = same * s_le_t
    cb[:, CB_ONES:CB_ONES + 128] = 1.0
    q = np.arange(512)
    for jm in range(4):
        cb[:, CB_MASK + jm * 512:CB_MASK + (jm + 1) * 512] = np.where(q[None, :] >= (jm * 128 + idx[:, None]), 0.0, -30000.0)
    return ca, cb


def _small_pack(inp):
    sp_ = np.zeros((2, 128, NS), np.float32)
    f = lambda a: np.asarray(a, np.float32)
    for l in range(2):
        sp_[l, :, S_CONVW:S_CONVW + 124] = f(inp["conv_w"][l]).reshape(CONV_K, 4, 128).transpose(2, 1, 0).reshape(128, 124)
        sp_[l, :, S_CONVB:S_CONVB + 4] = f(inp["conv_b"][l]).reshape(4, 128).T
        sp_[l, :, S_CLNG:S_CLNG + 4] = f(inp["conv_ln_g"][l]).reshape(4, 128).T
        sp_[l, :, S_CLNB:S_CLNB + 4] = f(inp["conv_ln_b"][l]).reshape(4, 128).T
        sp_[l, :, S_DAG] = f(inp["da_norm_g"][l])
        sp_[l, :, S_HGG] = f(inp["hg_norm_g"][l])
        sp_[l, :, S_BGATE:S_BGATE + 24] = f(inp["b_gate"][l]).reshape(3, 8, 128).transpose(2, 0, 1).reshape(128, 24)
        for nm, off in (("ln1_g", S_LN1G), ("ln1_b", S_LN1B), ("ln2_g", S_LN2G), ("ln2_b", S_LN2B)):
            sp_[l, :, off:off + 8] = f(inp[nm][l]).reshape(8, 128).T
        wr = np.concatenate([f(inp["router_group"][l]), f(inp["router_expert"][l]).transpose(1, 0, 2).reshape(D, 32)], axis=1)
        sp_[l, :, S_WR:S_WR + 288] = wr.reshape(8, 128, 36).transpose(1, 0, 2).reshape(128, 288)
        rb = np.concatenate([f(inp["router_group_b"][l]), f(inp["router_expert_b"][l]).reshape(32)])
        sp_[l, :, S_RB:S_RB + 36] = rb[None, :]
        sp_[l, :, S_LBP:S_LBP + 8] = f(inp["hg_lb"]).reshape(2, 4, 128).transpose(2, 0, 1).reshape(128, 8)
    return sp_


_PROG_CACHE = {}


def _run(inp, layers, x_in):
    keyp = tuple(layers)
    if keyp not in _PROG_CACHE:
        _PROG_CACHE[keyp] = build_program(layers=layers)
    nc = _PROG_CACHE[keyp]
    ca, cb = _const_packs()
    smallp = _small_pack(inp)
    f = lambda a: np.ascontiguousarray(np.asarray(a, np.float32))
    shared = {
        "w_in": f(inp["w_in"]), "w_branch": f(inp["w_branch"]), "w_out": f(inp["w_out"]),
        "exp_w1": f(inp["exp_w1"]), "exp_w3": f(inp["exp_w3"]), "exp_w2": f(inp["exp_w2"]),
        "small": smallp, "lbrow": f(inp["hg_lb"]), "lamrow": f(inp["da_lambda"]).reshape(2, 256), "constA": ca, "constB": cb,
    }
    pos = np.ascontiguousarray(np.asarray(inp["positions"], np.int32))
    in_maps = []
    for c in range(NCORES):
        m = dict(shared)
        m["x"] = np.ascontiguousarray(x_in[c * SPC:(c + 1) * SPC])
        m["positions"] = np.ascontiguousarray(pos[c * SPC:(c + 1) * SPC])
        in_maps.append(m)
    res = run_bass_kernel_spmd(nc, in_maps, core_ids=list(range(NCORES)))
    return np.concatenate([np.asarray(r["out"], np.float32) for r in res.results], axis=0)


def kernel(**inputs):
    x = np.asarray(inputs["x"], np.float32)
    return _run(inputs, (0, 1), x)
```

```python
import math
from contextlib import ExitStack

import numpy as np
import concourse.bass as bass
import concourse.mybir as mybir
from concourse.bass_utils import run_bass_kernel_spmd

F32 = mybir.dt.float32
BF16 = mybir.dt.bfloat16
I32 = mybir.dt.int32
AF = mybir.ActivationFunctionType
ALU = mybir.AluOpType
AX = mybir.AxisListType

D = 1024
SEQ = 2048
NB = 32
NCORES = 8
SPC = NB // NCORES
T = 512
NBLK = SEQ // T
KC = D // 128
IN_COLS = 7680
NEXP = 32
DEXP = 512
CONV_K = 31
HALO = CONV_K - 1
DN_ALPHA = (2 * 2) ** 0.25
LN_EPS = 1e-5
ROPE_THETA = 500000.0
CLAMP = 40.0

_off = 0
def _fld(n):
    global _off
    o = _off
    _off += n
    return o
S_CONVW = _fld(4 * CONV_K)
S_CONVB = _fld(4)
S_CLNG = _fld(4)
S_CLNB = _fld(4)
S_DAG = _fld(1)
S_HGG = _fld(1)
S_BGATE = _fld(24)
S_LN1G = _fld(8)
S_LN1B = _fld(8)
S_LN2G = _fld(8)
S_LN2B = _fld(8)
S_WR = _fld(8 * 36)
S_RB = _fld(36)
S_LBP = _fld(8)
NS = _off
C_ID = 0
C_M1 = 128
C_M2 = 256
C_M3 = 384
C_CM = 512
C_INVF = 516
C_SGN = 517
C_ONES = 518
NCA = 646
CB_ID = 0
CB_PERM = 128
CB_M3 = 256
CB_ONES = 384
CB_MASK = 512
NCB = 512 + 2048


class Buf:
    __slots__ = ("name", "w", "r", "excl")

    def __init__(self, name, excl=False):
        self.name = name
        self.w = None
        self.r = {}
        self.excl = excl


class Eng:
    def __init__(self, name, handle, sem, is_pe=False):
        self.name = name
        self.h = handle
        self.sem = sem
        self.key = "s_" + name
        self.count = 0
        self.prog = []
        self.waited = {}
        self.is_pe = is_pe
        self.dma_i = 0
        self.pending_noinc = False


class FW:
    NDMA = 8

    def __init__(self, nc, stack):
        self.nc = nc
        self.sems = {}

        def mk(name):
            s = stack.enter_context(nc.semaphore(name))
            self.sems[name] = s
            return s
        self.pe = Eng("pe", nc.tensor, mk("s_pe"), is_pe=True)
        self.act = Eng("act", nc.scalar, mk("s_act"))
        self.dve = Eng("dve", nc.vector, mk("s_dve"))
        self.pool = Eng("pool", nc.gpsimd, mk("s_pool"))
        self.sp = Eng("sp", nc.sync, mk("s_sp"))
        self.engs = [self.pe, self.act, self.dve, self.pool, self.sp]
        self.dsem = {}
        for e in (self.sp, self.pool):
            self.dsem[e.name] = [mk(f"d_{e.name}{i}") for i in range(self.NDMA)]
        self.n_ops = 0
        self.tag = ''
        self.pe_tags = []
        self.pe_names = []

    def _deps(self, eng, reads, writes):
        need = {}
        for b in reads:
            if b.w is not None:
                k, v = b.w
                if need.get(k, 0) < v:
                    need[k] = v
            if b.excl:
                for k, v in b.r.items():
                    if k != eng.key and need.get(k, 0) < v:
                        need[k] = v
        for b in writes:
            if b.w is not None:
                k, v = b.w
                if need.get(k, 0) < v:
                    need[k] = v
            for k, v in b.r.items():
                if need.get(k, 0) < v:
                    need[k] = v
        waits = []
        for k, v in need.items():
            if eng.is_pe and k == eng.key:
                continue
            if eng.waited.get(k, 0) >= v:
                continue
            eng.waited[k] = v
            waits.append((k, v))
        return waits

    def op(self, eng, fn, reads=(), writes=(), inc=True):
        waits = self._deps(eng, reads, writes)
        if eng.is_pe:
            self.pe_tags.append(self.tag)
        if inc:
            eng.count += 1
            seq = eng.count
            eng.pending_noinc = False
        else:
            seq = eng.count + 1
            eng.pending_noinc = True
        key = eng.key
        eng.prog.append((waits, fn, 1 if inc else 0, None))
        for b in reads:
            if b.r.get(key, 0) < seq:
                b.r[key] = seq
        for b in writes:
            b.w = (key, seq)
            b.r = {}
        self.n_ops += 1

    def dma(self, eng, fn, reads=(), writes=()):
        ring = self.dsem[eng.name]
        i = eng.dma_i
        eng.dma_i += 1
        r = i % self.NDMA
        key = ("d", eng.name, r)
        waits = self._deps(eng, reads, writes)
        prev = i // self.NDMA
        if prev > 0 and eng.waited.get(key, 0) < prev * 16:
            eng.waited[key] = prev * 16
            waits.append((key, prev * 16))
        val = (prev + 1) * 16
        eng.prog.append((waits, fn, 16, ring[r]))
        for b in reads:
            if b.r.get(key, 0) < val:
                b.r[key] = val
        for b in writes:
            b.w = (key, val)
            b.r = {}
        self.n_ops += 1

    def sem_of(self, key):
        if isinstance(key, tuple):
            return self.dsem[key[1]][key[2]]
        return self.sems[key]

    def finish(self, final_bufs):
        nc = self.nc
        waits = self._deps(self.sp, final_bufs, [])
        self.sp.prog.append((waits, None, 0, None))
        for e in self.engs:
            if e.pending_noinc:
                raise RuntimeError(f"engine {e.name} ends with a non-inc instruction")
        fw = self

        def runner(e):
            def run(h):
                for waits, fn, inc, dsem in e.prog:
                    for k, v in waits:
                        h.wait_ge(fw.sem_of(k), v)
                    if fn is None:
                        continue
                    ins = fn()
                    if e.is_pe:
                        fw.pe_names.append(getattr(getattr(ins, 'ins', ins), 'name', None))
                    if inc == 1:
                        ins.then_inc(e.sem, 1)
                    elif inc == 16:
                        ins.then_inc(dsem, 16)
            return run
        with nc.Block() as block:
            block.tensor(runner(self.pe))
            block.scalar(runner(self.act))
            block.vector(runner(self.dve))
            block.gpsimd(runner(self.pool))
            block.sync(runner(self.sp))


class Pool:
    def __init__(self, items):
        self.free = list(items)

    def get(self):
        if not self.free:
            raise RuntimeError("pool exhausted")
        return self.free.pop(0)

    def put(self, *xs):
        for x in xs:
            assert x not in self.free
            self.free.append(x)

    def get_run(self, n):
        fs = sorted(self.free)
        for a in fs:
            if all((a + i) in self.free for i in range(n)):
                for i in range(n):
                    self.free.remove(a + i)
                return a
        raise RuntimeError(f"no run of {n} free slots: {fs}")

    def put_run(self, a, n):
        self.put(*range(a, a + n))


def build_program(layers=(0, 1), n_seq=SPC, n_blk=NBLK, n_exp=NEXP, taps=None, stop=99):
    taps = taps or {}
    import os
    SKIP = os.environ.get('SKIP', '')
    nc = bass.Bass("TRN2", target_bir_lowering=False)
    dram = {}

    def din(name, shape, dt=F32):
        dram[name] = nc.dram_tensor(name, list(shape), dt, kind="ExternalInput").ap()
        return dram[name]

    x_d = din("x", [n_seq, SEQ, D])
    pos_d = din("positions", [n_seq, SEQ], I32)
    w_in_d = din("w_in", [2, D, IN_COLS])
    w_br_d = din("w_branch", [2, 3, 512, D])
    w_out_d = din("w_out", [2, D, D])
    w1_d = din("exp_w1", [2, NEXP, D, DEXP])
    w3_d = din("exp_w3", [2, NEXP, D, DEXP])
    w2_d = din("exp_w2", [2, NEXP, DEXP, D])
    small_d = din("small", [2, 128, NS])
    lbrow_d = din("lbrow", [2, 512])
    lam_d = din("lamrow", [2, 256])
    ca_d = din("constA", [128, NCA])
    cb_d = din("constB", [128, NCB])
    out_d = nc.dram_tensor("out", [n_seq, SEQ, D], F32, kind="ExternalOutput").ap()
    tap_d = {}
    for name, (shape, dt) in TAP_SHAPES.items():
        if name in taps:
            tap_d[name] = nc.dram_tensor("tap_" + name, list(shape), dt, kind="ExternalOutput").ap()

    with ExitStack() as st:
        fw = FW(nc, st)
        pe, act, dve, pool, sp = fw.pe, fw.act, fw.dve, fw.pool, fw.sp

        def sb(name, shape, dt=F32):
            return st.enter_context(nc.sbuf_tensor("sb_" + name, list(shape), dt))

        constA = sb("constA", [128, NCA]); b_constA = Buf("constA")
        cbf = sb("cbf", [128, NCB], BF16); b_cbf = Buf("cbf")
        small = sb("small", [128, 2, NS]); b_small = Buf("small")
        omlb_p = sb("omlb_p", [128, 2, 4]); omlb_row = sb("omlb_row", [128, 512]); b_omlb = Buf("omlb")
        lam_t = sb("lam_t", [128, 2, 4]); b_lam = Buf("lam")
        dagp = sb("dagp", [128, 2]);
        epst = sb("epst", [128, 1]); b_eps = Buf("eps")
        xres = sb("xres", [128, KC, T]); b_xres = [Buf(f"xres{i}") for i in range(KC)]
        xbf = sb("xbf", [128, KC, T], BF16); b_xbf = [Buf(f"xbf{i}") for i in range(KC)]
        KT = [sb(f"KT{l}", [128, 4, SEQ], BF16) for l in range(2)]
        b_KT = [[[Buf(f"KT{l}_{h}_{j}") for j in range(NBLK)] for h in range(4)] for l in range(2)]
        Vh = [sb(f"V{l}", [128, SEQ // 128, 512], BF16) for l in range(2)]
        b_V = [[Buf(f"V{l}_{t}") for t in range(SEQ // 128)] for l in range(2)]
        hst = [sb(f"hst{l}", [128, 4, 128]) for l in range(2)]; b_hst = [Buf(f"hst{l}") for l in range(2)]
        ubuf = [sb(f"ubuf{l}", [128, 4, HALO + T], BF16) for l in range(2)]
        b_ubuf = [[Buf(f"ubuf{l}_{c}") for c in range(4)] for l in range(2)]
        ropeC = sb("ropeC", [128, T]); ropeS = sb("ropeS", [128, T]); b_rope = Buf("rope")
        dec_t = sb("dec_t", [128, 4, 16]); b_dec = Buf("dec")
        comb = sb("comb", [128, 4, 32]); b_comb = Buf("comb")
        NW = 4
        wsl = [sb(f"wsl{i}", [128, 4096], BF16) for i in range(NW)]
        b_wsl = [Buf(f"wsl{i}") for i in range(NW)]
        print('sbuf remaining', nc.sbuf_bytes_remaining, flush=True)
        NSLOT = (nc.sbuf_bytes_remaining - 2048) // 2048
        print('NSLOT', NSLOT, flush=True)
        NSLOT = min(NSLOT, 40)
        arena = sb("arena", [128, NSLOT, 512])
        b_ar = [Buf(f"ar{i}") for i in range(NSLOT)]
        psum = [st.enter_context(nc.psum_tensor(f"ps{i}", [128, 512], F32)) for i in range(8)]
        b_ps = [Buf(f"ps{i}", excl=True) for i in range(8)]
        banks = Pool(range(8))
        slots = Pool(range(NSLOT))

        ident = constA[:, C_ID:C_ID + 128]
        M1 = constA[:, C_M1:C_M1 + 128]
        M2 = constA[:, C_M2:C_M2 + 128]
        M3 = constA[:, C_M3:C_M3 + 128]
        onesf = constA[:, C_ONES:C_ONES + 128]
        identb = cbf[:, CB_ID:CB_ID + 128]
        permb = cbf[:, CB_PERM:CB_PERM + 128]
        M3b = cbf[:, CB_M3:CB_M3 + 128]
        onesb = cbf[:, CB_ONES:CB_ONES + 128]

        def A(i):
            return arena[:, i, :]

        def AB(i):
            return arena[:, i, :].bitcast(BF16)

        def mm(out, lhsT, rhs, start, stop, R, W, inc=True):
            fw.op(pe, lambda: nc.tensor.matmul(out, lhsT, rhs, start=start, stop=stop), R, W, inc=inc)

        def actf(out, in_, func, R, W, bias=None, scale=None):
            kw = {}
            if bias is not None:
                kw["bias"] = bias
            if scale is not None:
                kw["scale"] = scale
            fw.op(act, lambda: nc.scalar.activation(out=out, in_=in_, func=func, **kw), R, W)

        def ttop(eng, out, a, b, op, R, W):
            fw.op(eng, lambda: eng.h.tensor_tensor(out, a, b, op), R, W)

        def tsop(eng, out, a, s1, s2, op0, op1, R, W):
            if s2 is None:
                fw.op(eng, lambda: eng.h.tensor_scalar(out, a, s1, None, op0), R, W)
            else:
                fw.op(eng, lambda: eng.h.tensor_scalar(out, a, s1, s2, op0, op1), R, W)

        def sttop(eng, out, a, s, b, op0, op1, R, W):
            fw.op(eng, lambda: eng.h.scalar_tensor_tensor(out, a, s, b, op0, op1), R, W)

        def cpy(eng, out, in_, R, W):
            if eng is act:
                fw.op(act, lambda: nc.scalar.activation(out=out, in_=in_, func=AF.Identity), R, W)
            else:
                fw.op(eng, lambda: eng.h.tensor_copy(out, in_), R, W)

        def tap(name, key, src_ap, R):
            if name in taps and taps[name] == key:
                b = Buf("tap_" + name)
                dst = tap_d[name]
                fw.dma(sp, (lambda dst=dst, src_ap=src_ap: nc.sync.dma_start(out=dst, in_=src_ap)), R, [b])
                final_bufs.append(b)

        final_bufs = []

        wlist = []
        for s in range(n_seq):
            for j in range(n_blk):
                for l in layers:
                    def inblk(b, l=l):
                        return (w_in_d[l][:, 512 * b:512 * b + 512].rearrange("(kc p) c -> p kc c", p=128), 8)
                    for b in range(9):
                        wlist.append(inblk(b))
                    for n in range(3):
                        wlist.append((w_br_d[l][n].rearrange("(kc p) c -> p kc c", p=128), 4))
                        wlist.append(inblk(9 + 2 * n))
                        wlist.append(inblk(10 + 2 * n))
                    for hf in range(2):
                        wlist.append((w_out_d[l][:, 512 * hf:512 * hf + 512].rearrange("(kc p) c -> p kc c", p=128), 8))
                    def wup(e, l=l):
                        wlist.append((w1_d[l][e].rearrange("(kc p) c -> p kc c", p=128), 8))
                        wlist.append((w3_d[l][e].rearrange("(kc p) c -> p kc c", p=128), 8))
                    wup(0)
                    for e in range(n_exp):
                        if e + 1 < n_exp:
                            wup(e + 1)
                        wlist.append((w2_d[l][e].rearrange("(kc p) c -> p kc c", p=128), 4))
        wstate = {"issued": 0, "consumed": 0, "ready": []}
        wfree = Pool(range(NW))

        def w_issue():
            while wfree.free and wstate["issued"] < len(wlist):
                i = wfree.get()
                src, nk = wlist[wstate["issued"]]
                wstate["issued"] += 1
                dst = wsl[i][:].rearrange("p (k c) -> p k c", k=nk)
                fw.dma(pool, (lambda dst=dst, src=src: nc.gpsimd.dma_start(out=dst, in_=src)), [], [b_wsl[i]])
                wstate["ready"].append((i, nk))

        def w_get(nk_expect):
            w_issue()
            i, nk = wstate["ready"].pop(0)
            assert nk == nk_expect, (nk, nk_expect, wstate["consumed"])
            wstate["consumed"] += 1
            return i, wsl[i][:].rearrange("p (k c) -> p k c", k=nk)

        def w_put(i):
            wfree.put(i)
            w_issue()

        fw.dma(sp, lambda: nc.sync.dma_start(out=constA[:], in_=ca_d), [], [b_constA])
        fw.dma(sp, lambda: nc.sync.dma_start(out=small[:], in_=small_d.rearrange("l p n -> p l n")), [], [b_small])
        fw.dma(sp, lambda: nc.sync.dma_start(out=omlb_row[:], in_=lbrow_d[1:2, :].partition_broadcast(128)), [], [b_omlb])
        s0 = slots.get_run(5)
        stg = arena[:, s0:s0 + 5, :].rearrange("p a b -> p (a b)")
        stgb = [b_ar[s0 + i] for i in range(5)]
        fw.dma(sp, lambda: nc.sync.dma_start(out=stg[:, 0:NCB], in_=cb_d), [], stgb)
        cpy(dve, cbf[:], stg[:, 0:NCB], stgb, [b_cbf])
        slots.put_run(s0, 5)
        fw.op(dve, lambda: nc.vector.memset(epst[:], LN_EPS), [], [b_eps])
        s0 = slots.get()
        fw.dma(sp, lambda: nc.sync.dma_start(out=A(s0), in_=lbrow_d[0:1, :].partition_broadcast(128)), [], [b_ar[s0]])
        ttop(dve, omlb_row[:], A(s0), omlb_row[:], ALU.subtract, [b_omlb, b_ar[s0]], [b_omlb])
        actf(omlb_row[:], omlb_row[:], AF.Sigmoid, [b_omlb], [b_omlb])
        slots.put(s0)
        lbp = small[:, 0, S_LBP:S_LBP + 8].rearrange("p (l h) -> p l h", l=2)
        ttop(dve, omlb_p[:, 1, :], lbp[:, 0, :], lbp[:, 1, :], ALU.subtract, [b_small], [b_omlb])
        actf(omlb_p[:, 1, :], omlb_p[:, 1, :], AF.Sigmoid, [b_omlb], [b_omlb])
        fw.op(dve, lambda: nc.vector.memset(omlb_p[:, 0, :], 1.0), [], [b_omlb])
        sl_a = slots.get(); sl_b = slots.get(); sl_c = slots.get()
        rt = A(sl_a)[:, 0:256].rearrange("p (a b) -> p a b", a=4); b_rt = b_ar[sl_a]
        rt2 = A(sl_b)[:, 0:256].rearrange("p (a b) -> p a b", a=4); b_rt2 = b_ar[sl_b]
        for l in (range(2) if 'L' not in SKIP else []):
            lam_init = 0.8 - 0.6 * math.exp(-0.3 * l)
            lp = A(sl_c)[:, 0:256]
            lsrc = lam_d[l:l + 1, :].partition_broadcast(128)
            fw.dma(sp, (lambda lp=lp, lsrc=lsrc: nc.sync.dma_start(out=lp, in_=lsrc)), [], [b_ar[sl_c]])
            ttop(dve, rt[:, 0, :], lp[:, 0:64], lp[:, 64:128], ALU.mult, [b_ar[sl_c]], [b_rt])
            ttop(dve, rt[:, 1, :], lp[:, 128:192], lp[:, 192:256], ALU.mult, [b_ar[sl_c]], [b_rt])
            fw.op(dve, lambda: nc.vector.tensor_reduce(out=rt2[:, 0, 0:2], in_=rt[:, 0:2, :], axis=AX.X, op=ALU.add), [b_rt], [b_rt2])
            actf(rt2[:, 0, 2:4], rt2[:, 0, 0:2], AF.Exp, [b_rt2], [b_rt2])
            ttop(dve, lam_t[:, l, 0:1], rt2[:, 0, 3:4], rt2[:, 0, 2:3], ALU.subtract, [b_rt2], [b_lam])
            tsop(dve, lam_t[:, l, 0:1], lam_t[:, l, 0:1], -lam_init, None, ALU.add, None, [b_lam], [b_lam])
            tsop(dve, dagp[:, l:l + 1], small[:, l, S_DAG:S_DAG + 1], 1.0 - lam_init, None, ALU.mult, None, [b_small], [b_lam])
        slots.put(sl_a, sl_b, sl_c)

        for s in range(n_seq):
            for j in range(n_blk):
                t0 = j * T
                fw.tag = 'xload'
                for tt in (range(4) if 'X' not in SKIP else []):
                    sa = slots.get_run(2); sb_ = sa + 1
                    xt = arena[:, sa:sa + 2, :].rearrange("p a b -> p (a b)")
                    xsrc = x_d[s, t0 + tt * 128:t0 + (tt + 1) * 128, :]
                    fw.dma(sp, (lambda xt=xt, xsrc=xsrc: nc.sync.dma_start(out=xt, in_=xsrc)),
                           [], [b_ar[sa], b_ar[sb_]])
                    for half in range(2):
                        pb = banks.get()
                        for q in range(4):
                            dc = half * 4 + q
                            mm(psum[pb][:, q * 128:(q + 1) * 128], xt[:, dc * 128:(dc + 1) * 128], ident, True, True,
                               [b_ar[sa], b_ar[sb_], b_constA], [b_ps[pb]], inc=(q == 3))
                        for q in range(4):
                            dc = half * 4 + q
                            cpy(dve if (q % 2 == 0 or 'v' in SKIP) else act, xres[:, dc, tt * 128:(tt + 1) * 128], psum[pb][:, q * 128:(q + 1) * 128],
                                [b_ps[pb]], [b_xres[dc]])
                        banks.put(pb)
                    slots.put(sa, sb_)
                for dc in (range(KC) if ('X' not in SKIP and 'b' not in SKIP) else []):
                    cpy(act if dc % 2 == 0 else dve, xbf[:, dc, :], xres[:, dc, :], [b_xres[dc]], [b_xbf[dc]])
                fw.tag = 'rope'
                sa = slots.get(); sb_ = slots.get(); sc = slots.get()
                psrc = pos_d[s:s + 1, t0:t0 + T].partition_broadcast(128)
                fw.dma(sp, (lambda sa=sa, psrc=psrc: nc.sync.dma_start(out=A(sa).bitcast(I32), in_=psrc)),
                       [], [b_ar[sa]])
                cpy(dve, A(sb_), A(sa).bitcast(I32), [b_ar[sa]], [b_ar[sb_]])
                tsop(dve, A(sb_), A(sb_), constA[:, C_INVF:C_INVF + 1], None, ALU.mult, None, [b_ar[sb_], b_constA], [b_ar[sb_]])
                for which, dst in (((0, ropeS), (1, ropeC)) if 'R' not in SKIP else []):
                    if which == 1:
                        tsop(dve, A(sb_), A(sb_), float(np.pi / 2), None, ALU.add, None, [b_ar[sb_]], [b_ar[sb_]])
                    tsop(dve, A(sa).bitcast(I32), A(sb_), float(1.0 / (2 * np.pi)), None, ALU.mult, None, [b_ar[sb_]], [b_ar[sa]])
                    cpy(dve, A(sc), A(sa).bitcast(I32), [b_ar[sa]], [b_ar[sc]])
                    sttop(dve, A(sc), A(sc), float(-2 * np.pi), A(sb_), ALU.mult, ALU.add, [b_ar[sc], b_ar[sb_]], [b_ar[sc]])
                    if which == 0:
                        actf(dst[:], A(sc), AF.Sin, [b_ar[sc], b_constA], [b_rope], scale=constA[:, C_SGN:C_SGN + 1])
                    else:
                        actf(dst[:], A(sc), AF.Sin, [b_ar[sc]], [b_rope])
                slots.put(sa, sb_, sc)

                for l in layers:
                    key = (s, j, l)
                    sm = small[:, l, :]
                    emit_layer(locals())
                fw.tag = 'outstore'
                for tt in (range(4) if 'O' not in SKIP else []):
                    sa = slots.get_run(2); sb_ = sa + 1
                    ot = arena[:, sa:sa + 2, :].rearrange("p a b -> p (a b)")
                    for half in range(2):
                        pb = banks.get()
                        for q in range(4):
                            dc = half * 4 + q
                            mm(psum[pb][:, q * 128:(q + 1) * 128], xres[:, dc, tt * 128:(tt + 1) * 128], ident, True, True,
                               [b_xres[dc], b_constA], [b_ps[pb]], inc=(q == 3))
                        cpy(dve if half == 0 else act, ot[:, half * 512:(half + 1) * 512], psum[pb][:, :], [b_ps[pb]], [b_ar[sa + half]])
                        banks.put(pb)
                    bo = Buf("out")
                    odst = out_d[s, t0 + tt * 128:t0 + (tt + 1) * 128, :]
                    fw.dma(sp, (lambda ot=ot, odst=odst: nc.sync.dma_start(out=odst, in_=ot)),
                           [b_ar[sa], b_ar[sb_]], [bo])
                    final_bufs.append(bo)
                    slots.put(sa, sb_)
        assert stop < 99 or wstate["consumed"] == len(wlist), (wstate["consumed"], len(wlist))
        fw.finish(final_bufs)
    nc._pe_tags = fw.pe_tags
    nc._pe_names = fw.pe_names
    return nc


TAP_SHAPES = {
    "ya": ((128, 4, T), BF16), "yb": ((128, 4, T), BF16), "yc": ((128, 4, T), BF16),
    "x1": ((128, KC, T), F32), "x2": ((128, KC, T), F32), "mix": ((128, KC, T), F32),
    "q": ((128, 4, T), BF16), "acc": ((128, 4, D), F32), "comb": ((128, 4, 32), F32),
    "oh": ((128, 4, T), F32),
}


def emit_layer(E):
    g = E
    nc, fw = g["nc"], g["fw"]
    pe, act, dve, pool, sp = g["pe"], g["act"], g["dve"], g["pool"], g["sp"]
    mm, actf, ttop, tsop, sttop, cpy, tap = g["mm"], g["actf"], g["ttop"], g["tsop"], g["sttop"], g["cpy"], g["tap"]
    banks, slots, psum, b_ps, arena, b_ar = g["banks"], g["slots"], g["psum"], g["b_ps"], g["arena"], g["b_ar"]
    A, AB = g["A"], g["AB"]
    xres, b_xres, xbf, b_xbf = g["xres"], g["b_xres"], g["xbf"], g["b_xbf"]
    constA, b_constA, cbf, b_cbf, small, b_small = g["constA"], g["b_constA"], g["cbf"], g["b_cbf"], g["small"], g["b_small"]
    ident, M1, M2, M3, onesf, identb, permb, M3b, onesb = (g[k] for k in ("ident", "M1", "M2", "M3", "onesf", "identb", "permb", "M3b", "onesb"))
    epst, b_eps = g["epst"], g["b_eps"]
    w_get, w_put = g["w_get"], g["w_put"]
    l, j, s, key, sm = g["l"], g["j"], g["s"], g["key"], g["sm"]
    n_exp = g["n_exp"]
    t0 = j * T
    KTl, b_KTl, Vl, b_Vl = g["KT"][l], g["b_KT"][l], g["Vh"][l], g["b_V"][l]
    hstl, b_hstl = g["hst"][l], g["b_hst"][l]
    ub, b_ub = g["ubuf"][l], g["b_ubuf"][l]
    ropeC, ropeS, b_rope = g["ropeC"], g["ropeS"], g["b_rope"]
    dec_t, b_dec = g["dec_t"], g["b_dec"]
    comb, b_comb = g["comb"], g["b_comb"]
    omlb_p, omlb_row, b_omlb, lam_t, b_lam, dagp = g["omlb_p"], g["omlb_row"], g["b_omlb"], g["lam_t"], g["b_lam"], g["dagp"]
    XB = b_xbf
    stop = g["stop"]
    if stop <= 0:
        return

    def fm_proj(ws, oc, pb):
        for kc in range(KC):
            mm(psum[pb][:, :], ws[1][:, kc, oc * 128:(oc + 1) * 128], xbf[:, kc, :], kc == 0, kc == KC - 1,
               [g["b_wsl"][ws[0]], XB[kc]], [b_ps[pb]], inc=(kc == KC - 1))

    def tm_proj(ws, tt, pb):
        for kc in range(KC):
            mm(psum[pb][:, :], xbf[:, kc, tt * 128:(tt + 1) * 128], ws[1][:, kc, :], kc == 0, kc == KC - 1,
               [g["b_wsl"][ws[0]], XB[kc]], [b_ps[pb]], inc=(kc == KC - 1))

    def bc_stats(srcs, R, n_total):
        p1 = banks.get(); p2 = banks.get()
        n = len(srcs)
        sq = [slots.get(), slots.get()]
        for i, (ap, rb) in enumerate(zip(srcs, R)):
            mm(psum[p1][:, :], onesf, ap, i == 0, i == n - 1, rb + [b_constA], [b_ps[p1]], inc=(i == n - 1))
        for i, (ap, rb) in enumerate(zip(srcs, R)):
            q = sq[i % 2]
            fw.op(act, (lambda q=q, ap=ap: nc.scalar.activation(out=A(q), in_=ap, func=AF.Square)), rb, [b_ar[q]])
            mm(psum[p2][:, :], onesf, A(q), i == 0, i == n - 1, [b_ar[q], b_constA], [b_ps[p2]], inc=True)
        slots.put(*sq)
        m = slots.get(); r = slots.get()
        fw.op(act, lambda: nc.scalar.mul(A(m), psum[p1][:, :], 1.0 / n_total), [b_ps[p1]], [b_ar[m]])
        ttop(dve, A(r), A(m), A(m), ALU.mult, [b_ar[m]], [b_ar[r]])
        sttop(dve, A(r), psum[p2][:, :], 1.0 / n_total, A(r), ALU.mult, ALU.subtract, [b_ps[p2], b_ar[r]], [b_ar[r]])
        actf(A(r), A(r), AF.Sqrt, [b_ar[r], b_eps], [b_ar[r]], bias=epst[:, 0:1], scale=1.0)
        fw.op(dve, lambda: nc.vector.reciprocal(A(r), A(r)), [b_ar[r]], [b_ar[r]])
        banks.put(p1, p2)
        return m, r

    _y0 = slots.get_run(6)
    ysl = list(range(_y0, _y0 + 6))

    def ytile(n):
        return arena[:, ysl[2 * n]:ysl[2 * n] + 2, :].bitcast(BF16).rearrange("p a (h t) -> p (a h) t", h=2)

    def ybufs(n, c):
        return [b_ar[ysl[2 * n + c // 2]]]
    ya, yb, yc = ytile(0), ytile(1), ytile(2)

    fw.tag = 'conv_proj'
    wa1 = w_get(8); wa2 = w_get(8)
    if j == 0:
        for c in range(4):
            fw.op(dve, (lambda c=c: nc.vector.memset(ub[:, c, 0:HALO], 0.0)), [], [b_ub[c]])
    for c in range(4):
        pa = banks.get(); pg = banks.get()
        fm_proj(wa1, c, pa)
        fm_proj(wa2, c, pg)
        sg = slots.get()
        actf(A(sg), psum[pg][:, :], AF.Sigmoid, [b_ps[pg]], [b_ar[sg]])
        ttop(dve, ub[:, c, HALO:HALO + T], psum[pa][:, :], A(sg), ALU.mult, [b_ps[pa], b_ar[sg]], [b_ub[c]])
        slots.put(sg); banks.put(pa, pg)
    w_put(wa1[0]); w_put(wa2[0])
    fw.tag = 'conv'
    ycs = [slots.get() for _ in range(4)]
    dgs = [list(range(a_, a_ + 4)) for a_ in (slots.get_run(4), slots.get_run(4))]
    for c in range(4):
        dsl = dgs[c % 2]
        assert dsl == list(range(dsl[0], dsl[0] + 4))
        dg = arena[:, dsl[0]:dsl[0] + 4, :].bitcast(BF16).rearrange("p a b -> p (a b)")[:, 0:CONV_K * 128].rearrange("p (k m) -> p k m", k=CONV_K)
        dgb = [b_ar[i] for i in dsl]
        wv = sm[:, S_CONVW + c * CONV_K:S_CONVW + (c + 1) * CONV_K]
        fw.op(dve, (lambda dg=dg, wv=wv: nc.vector.tensor_tensor(dg, identb.unsqueeze(1).to_broadcast([128, CONV_K, 128]),
                                                                   wv.unsqueeze(2).to_broadcast([128, CONV_K, 128]), ALU.mult)),
              [b_cbf, b_small], dgb)
        pc = banks.get()
        for k in range(CONV_K):
            mm(psum[pc][:, :], dg[:, k, :], ub[:, c, k:k + T], k == 0, k == CONV_K - 1, dgb + [b_ub[c]], [b_ps[pc]], inc=(k == CONV_K - 1))
        actf(A(ycs[c]), psum[pc][:, :], AF.Identity, [b_ps[pc], b_small], [b_ar[ycs[c]]], bias=sm[:, S_CONVB + c:S_CONVB + c + 1], scale=1.0)
        banks.put(pc)
        cpy(pool, ub[:, c, 0:HALO], ub[:, c, T:T + HALO], [b_ub[c]], [b_ub[c]])
    for d_ in dgs:
        slots.put(*d_)
    fw.tag = 'conv_ln'
    m_, r_ = bc_stats([A(i) for i in ycs], [[b_ar[i]] for i in ycs], 512.0)
    for c in range(4):
        ttop(dve, A(ycs[c]), A(ycs[c]), A(m_), ALU.subtract, [b_ar[ycs[c]], b_ar[m_]], [b_ar[ycs[c]]])
        ttop(dve, A(ycs[c]), A(ycs[c]), A(r_), ALU.mult, [b_ar[ycs[c]], b_ar[r_]], [b_ar[ycs[c]]])
        actf(ya[:, c, :], A(ycs[c]), AF.Silu, [b_ar[ycs[c]], b_small], ybufs(0, c),
             bias=sm[:, S_CLNB + c:S_CLNB + c + 1], scale=sm[:, S_CLNG + c:S_CLNG + c + 1])
    slots.put(m_, r_); slots.put(*ycs)
    tap("ya", key, ya, ybufs(0, 0) + ybufs(0, 2))
    if stop <= 1:
        return

    fw.tag = 'att_qkv'
    wq = w_get(8); wk = w_get(8)
    _q0 = slots.get_run(4)
    qsl = list(range(_q0, _q0 + 4))
    qz = arena[:, _q0:_q0 + 4, :].bitcast(BF16).rearrange("p a (s t) -> p a s t", s=2)
    qTb = lambda h: [b_ar[qsl[h]]]
    for h in range(4):
        fw.op(pool, (lambda h=h: nc.gpsimd.memset(arena[:, qsl[h], :], 0.0)), [], qTb(h))
    jobs = [(wq, h, 0) for h in range(4)] + [(wk, h, 1) for h in range(4)]
    rst = {}

    def rope_s1(i):
        ws, h, kind = jobs[i]
        pq = banks.get()
        fm_proj(ws, h, pq)
        tb = slots.get()
        cpy(act, AB(tb)[:, 0:T], psum[pq][:, :], [b_ps[pq]], [b_ar[tb]])
        rst[i] = (pq, tb)
        if i == 3:
            w_put(wq[0])
        if i == 7:
            w_put(wk[0])

    def rope_s2(i):
        ws, h, kind = jobs[i]
        pq, tb = rst.pop(i)
        pp = banks.get()
        t1 = slots.get(); t2 = slots.get()
        mm(psum[pp][:, :], permb, AB(tb)[:, 0:T], True, True, [b_cbf, b_ar[tb]], [b_ps[pp]])
        ttop(dve, A(t1), psum[pq][:, :], ropeC[:], ALU.mult, [b_ps[pq], b_rope], [b_ar[t1]])
        ttop(dve, A(t2), psum[pp][:, :], ropeS[:], ALU.mult, [b_ps[pp], b_rope], [b_ar[t2]])
        if kind == 0:
            ttop(dve, qz[0:64, h, 0, :], A(t1)[0:64, :], A(t2)[0:64, :], ALU.add, [b_ar[t1], b_ar[t2]], qTb(h))
            ttop(dve, qz[64:128, h, 1, :], A(t1)[64:128, :], A(t2)[64:128, :], ALU.add, [b_ar[t1], b_ar[t2]], qTb(h))
        else:
            ttop(dve, KTl[:, h, t0:t0 + T], A(t1), A(t2), ALU.add, [b_ar[t1], b_ar[t2]], [b_KTl[h][j]])
        slots.put(tb, t1, t2); banks.put(pq, pp)
    rope_s1(0)
    for i in range(8):
        if i + 1 < 8:
            rope_s1(i + 1)
        rope_s2(i)
    wv = w_get(8)
    for tt in range(4):
        pv = banks.get()
        tm_proj(wv, tt, pv)
        cpy(act if tt % 2 == 0 else dve, Vl[:, 4 * j + tt, :], psum[pv][:, :], [b_ps[pv]], [b_Vl[4 * j + tt]])
        banks.put(pv)
    w_put(wv[0])
    fw.tag = 'att_core'
    nkb = 4 * (j + 1)
    ohs = [slots.get() for _ in range(4)]
    for h in range(4):
        po = [banks.get(), banks.get()]
        pS = [banks.get(), banks.get()]
        items = [(p, kb) for kb in range(nkb) for p in range(2)]
        ptr = [slots.get(), slots.get()]
        pbufs = [(ptr[i // 2], i % 2) for i in range(4)]
        sc_banks = {}

        def score(idx):
            p, kb = items[idx]
            pb = banks.get()
            sc_banks[idx] = pb
            diag = kb >= 4 * j
            mm(psum[pb][:, :], KTl[:, h, kb * 128:(kb + 1) * 128], qz[:, h, p, :], True, not diag,
               [b_KTl[h][kb // 4]] + qTb(h), [b_ps[pb]], inc=not diag)
            if diag:
                mk = cbf[:, CB_MASK + (kb - 4 * j) * 512:CB_MASK + (kb - 4 * j + 1) * 512]
                mm(psum[pb][:, :], identb, mk, False, True, [b_cbf], [b_ps[pb]])
        score(0)
        score(1)
        for idx in range(len(items)):
            p, kb = items[idx]
            if idx + 2 < len(items):
                score(idx + 2)
            pb = sc_banks.pop(idx)
            sl, hf = pbufs[idx % 4]
            PT = AB(sl)[:, hf * T:(hf + 1) * T]
            actf(PT, psum[pb][:, :], AF.Exp, [b_ps[pb]], [b_ar[sl]], scale=0.125)
            banks.put(pb)
            mm(psum[po[p]][:, :], Vl[:, kb, h * 128:(h + 1) * 128], PT, kb == 0, kb == nkb - 1, [b_Vl[kb], b_ar[sl]], [b_ps[po[p]]], inc=False)
            mm(psum[pS[p]][:, :], onesb, PT, kb == 0, kb == nkb - 1, [b_cbf, b_ar[sl]], [b_ps[pS[p]]], inc=True)
        slots.put(*ptr)
        r1 = slots.get(); r2 = slots.get()
        fw.op(dve, (lambda r1=r1, pS=pS: nc.vector.reciprocal(A(r1), psum[pS[0]][:, :])), [b_ps[pS[0]]], [b_ar[r1]])
        fw.op(dve, (lambda r2=r2, pS=pS: nc.vector.reciprocal(A(r2), psum[pS[1]][:, :])), [b_ps[pS[1]]], [b_ar[r2]])
        ttop(dve, A(r1), psum[po[0]][:, :], A(r1), ALU.mult, [b_ps[po[0]], b_ar[r1]], [b_ar[r1]])
        ttop(dve, A(r2), psum[po[1]][:, :], A(r2), ALU.mult, [b_ps[po[1]], b_ar[r2]], [b_ar[r2]])
        sttop(dve, A(ohs[h]), A(r2), lam_t[:, l, 0:1], A(r1), ALU.mult, ALU.add, [b_ar[r1], b_ar[r2], b_lam], [b_ar[ohs[h]]])
        slots.put(r1, r2); banks.put(*po); banks.put(*pS)
    fw.tag = 'att_norm'
    for h in range(4):
        sq = slots.get(); pm = banks.get()
        fw.op(act, (lambda sq=sq, h=h: nc.scalar.activation(out=A(sq), in_=A(ohs[h]), func=AF.Square)), [b_ar[ohs[h]]], [b_ar[sq]])
        mm(psum[pm][:, :], onesf, A(sq), True, True, [b_ar[sq], b_constA], [b_ps[pm]])
        actf(A(sq), psum[pm][:, :], AF.Sqrt, [b_ps[pm], b_eps], [b_ar[sq]], bias=epst[:, 0:1], scale=1.0 / 128)
        fw.op(dve, (lambda sq=sq: nc.vector.reciprocal(A(sq), A(sq))), [b_ar[sq]], [b_ar[sq]])
        sttop(dve, yb[:, h, :], A(ohs[h]), dagp[:, l:l + 1], A(sq), ALU.mult, ALU.mult, [b_ar[ohs[h]], b_ar[sq], b_lam], ybufs(1, h))
        slots.put(sq); banks.put(pm)
    slots.put(*ohs); slots.put(*qsl)
    tap("yb", key, yb, ybufs(1, 0) + ybufs(1, 2))
    if stop <= 2:
        return

    fw.tag = 'hg_proj'
    def bf4(n0):
        return arena[:, n0:n0 + 2, :].bitcast(BF16).rearrange("p a (h t) -> p (a h) t", h=2)

    def two():
        return slots.get_run(2)
    s_qs, s_kT, s_qb, s_kd = (two() for _ in range(4))
    qs, kTt, qb, kdec = (bf4(x) for x in (s_qs, s_kT, s_qb, s_kd))
    B2 = lambda s0: [b_ar[s0], b_ar[s0 + 1]]
    Bh = lambda s0, h: [b_ar[s0 + h // 2]]
    attm = [slots.get(), slots.get()]
    wqh = w_get(8)
    for h in range(4):
        pb = banks.get()
        fm_proj(wqh, h, pb)
        actf(qs[:, h, :], psum[pb][:, :], AF.Silu, [b_ps[pb]], Bh(s_qs, h))
        banks.put(pb)
    w_put(wqh[0])
    wfh = w_get(8)
    for h in range(4):
        pb = banks.get(); tmp = slots.get()
        fm_proj(wfh, h, pb)
        actf(A(tmp), psum[pb][:, :], AF.Sigmoid, [b_ps[pb]], [b_ar[tmp]], scale=-1.0)
        tsop(dve, kTt[:, h, :], A(tmp), omlb_p[:, l, h:h + 1], None, ALU.mult, None, [b_ar[tmp], b_omlb], Bh(s_kT, h))
        banks.put(pb); slots.put(tmp)
    if j == 0:
        fw.op(dve, lambda: nc.vector.memset(hstl[:], 0.0), [], [b_hstl])
    fw.tag = 'hg_prep'
    v3 = lambda ap: ap.rearrange("p (h t) -> p h t", h=4)
    for tt in range(4):
        tsl = slice(tt * 128, (tt + 1) * 128)
        kt_ = slots.get(); lf_ = slots.get()
        pb = banks.get()
        tm_proj(wfh, tt, pb)
        actf(A(kt_), psum[pb][:, :], AF.Sigmoid, [b_ps[pb]], [b_ar[kt_]], scale=-1.0)
        banks.put(pb)
        if l == 1:
            ttop(dve, A(kt_), A(kt_), omlb_row[:], ALU.mult, [b_ar[kt_], b_omlb], [b_ar[kt_]])
        actf(A(lf_), A(kt_), AF.Ln, [b_ar[kt_]], [b_ar[lf_]], scale=-1.0, bias=1.0)
        pb = banks.get(); e2 = slots.get()
        mm(psum[pb][:, :], M2, A(lf_), True, True, [b_constA, b_ar[lf_]], [b_ps[pb]])
        actf(A(e2), psum[pb][:, :], AF.Exp, [b_ps[pb]], [b_ar[e2]])
        ttop(dve, kdec[:, tt, :], A(kt_), A(e2), ALU.mult, [b_ar[kt_], b_ar[e2]], Bh(s_kd, tt))
        banks.put(pb); slots.put(e2); slots.put(kt_)
        p1 = banks.get(); p3 = banks.get()
        for h in range(4):
            mm(psum[p1][:, h * 128:(h + 1) * 128], A(lf_)[:, h * 128:(h + 1) * 128], M1, True, True,
               [b_ar[lf_], b_constA], [b_ps[p1]], inc=(h == 3))
        for h in range(4):
            mm(psum[p3][:, h * 128:(h + 1) * 128], A(lf_)[:, h * 128:(h + 1) * 128], M3, True, True,
               [b_ar[lf_], b_constA], [b_ps[p3]], inc=(h == 3))
        slots.put(lf_)
        c1 = slots.get(); e1 = slots.get(); e3 = slots.get(); qk = slots.get()
        tsop(dve, A(c1), psum[p1][:, :], CLAMP, -CLAMP, ALU.min, ALU.max, [b_ps[p1]], [b_ar[c1]])
        actf(A(e1), A(c1), AF.Exp, [b_ar[c1]], [b_ar[e1]])
        actf(A(c1), A(c1), AF.Exp, [b_ar[c1]], [b_ar[c1]], scale=-1.0)
        actf(A(e3), psum[p3][:, :], AF.Exp, [b_ps[p3]], [b_ar[e3]])
        banks.put(p1, p3)
        qt = v3(AB(qk)[:, 0:512]); kt = v3(AB(qk)[:, 512:1024])
        ttop(dve, qt, qs[:, :, tsl], v3(A(e1)), ALU.mult, B2(s_qs) + [b_ar[e1]], [b_ar[qk]])
        ttop(dve, kt, kTt[:, :, tsl], v3(A(c1)), ALU.mult, B2(s_kT) + [b_ar[c1]], [b_ar[qk]])
        ttop(dve, qb[:, :, tsl], qs[:, :, tsl], v3(A(e3)), ALU.mult, B2(s_qs) + [b_ar[e3]], B2(s_qb))
        e3v = A(e3).rearrange("p (h c t) -> p h c t", h=4, c=4)[:, :, :, 31]
        cpy(dve, dec_t[:, :, 4 * tt:4 * tt + 4], e3v, [b_ar[e3]], [b_dec])
        slots.put(c1, e1, e3)
        pa = banks.get()
        for h in range(4):
            mm(psum[pa][:, h * 128:(h + 1) * 128], kt[:, h, :], qt[:, h, :], True, True, [b_ar[qk]], [b_ps[pa]], inc=(h == 3))
        slots.put(qk)
        asl = attm[tt // 2]
        am = AB(asl)[:, (tt % 2) * 512:(tt % 2) * 512 + 512].rearrange("p (h t) -> p h t", h=4)
        fw.op(dve, (lambda am=am, pa=pa: nc.vector.tensor_tensor(am, psum[pa][:, :].rearrange("p (h t) -> p h t", h=4),
                                                                  M3.unsqueeze(1).to_broadcast([128, 4, 128]), ALU.mult)),
              [b_ps[pa], b_constA], [b_ar[asl]])
        banks.put(pa)
    w_put(wfh[0])
    slots.put_run(s_qs, 2); slots.put_run(s_kT, 2)
    fw.tag = 'hg_proj2'
    s_it, s_gs = two(), two()
    gsil = bf4(s_gs)
    itk = lambda tt: AB(s_it + tt % 2)[:, (tt // 2) * 512:(tt // 2) * 512 + 512]
    Bi = lambda tt: [b_ar[s_it + tt % 2]]
    wih = w_get(8); wgh = w_get(8)

    def itok_proj(tt):
        pb = banks.get()
        tm_proj(wih, tt, pb)
        cpy(act if tt % 2 else dve, itk(tt), psum[pb][:, :], [b_ps[pb]], Bi(tt))
        banks.put(pb)

    def gsil_proj(h, half):
        pb = banks.get()
        c0 = half * 256
        for kc in range(KC):
            mm(psum[pb][:, c0:c0 + 256], wgh[1][:, kc, h * 128:(h + 1) * 128], xbf[:, kc, c0:c0 + 256], kc == 0, kc == KC - 1,
               [g["b_wsl"][wgh[0]], XB[kc]], [b_ps[pb]], inc=(kc == KC - 1))
        actf(gsil[:, h, c0:c0 + 256], psum[pb][:, c0:c0 + 256], AF.Silu, [b_ps[pb]], Bh(s_gs, h))
        banks.put(pb)
    itok_proj(0)
    fw.tag = 'hg_rec'
    _o0 = slots.get_run(4)
    osl = list(range(_o0, _o0 + 4))
    sbf = [slots.get(), slots.get()]
    kdm = [slots.get() for _ in range(4)]
    cur = 0
    cpy(act, AB(sbf[cur])[:, 0:512], hstl[:].rearrange("p h v -> p (h v)"), [b_hstl], [b_ar[sbf[cur]]])
    for tt in range(4):
        po_ = banks.get()
        asl = attm[tt // 2]
        am = AB(asl)[:, (tt % 2) * 512:(tt % 2) * 512 + 512].rearrange("p (h t) -> p h t", h=4)
        pus = []
        for c in range(4):
            km = kdm[c]
            kmv = AB(km)[:, 0:512]
            actf(kmv, kdec[:, tt, :], AF.Identity, Bh(s_kd, tt) + [b_constA], [b_ar[km]], scale=constA[:, C_CM + c:C_CM + c + 1])
            pu = banks.get()
            pus.append(pu)
            for h in range(4):
                mm(psum[pu][:, h * 128:(h + 1) * 128], kmv[:, h * 128:(h + 1) * 128], itk(tt)[:, h * 128:(h + 1) * 128], True, True,
                   [b_ar[km]] + Bi(tt), [b_ps[pu]], inc=(h == 3))
        for c in range(4):
            cc = 4 * tt + c
            pu = pus[c]
            for h in range(4):
                oc_ = psum[po_][:, h * 128 + 32 * c:h * 128 + 32 * c + 32]
                mm(oc_, AB(sbf[cur])[:, h * 128:(h + 1) * 128], qb[:, h, tt * 128 + 32 * c:tt * 128 + 32 * c + 32], True, False,
                   [b_ar[sbf[cur]]] + B2(s_qb), [b_ps[po_]], inc=False)
                mm(oc_, itk(tt)[:, h * 128:(h + 1) * 128], am[:, h, 32 * c:32 * c + 32], False, True,
                   Bi(tt) + [b_ar[asl]], [b_ps[po_]], inc=(h == 3))
            for h in range(4):
                sttop(dve, hstl[:, h, :], hstl[:, h, :], dec_t[:, h, cc:cc + 1], psum[pu][:, h * 128:(h + 1) * 128], ALU.mult, ALU.add,
                      [b_hstl, b_dec, b_ps[pu]], [b_hstl])
            banks.put(pu)
            cur ^= 1
            cpy(act, AB(sbf[cur])[:, 0:512], hstl[:].rearrange("p h v -> p (h v)"), [b_hstl], [b_ar[sbf[cur]]])
            fw.tag = 'hg_fill'
            if c == 0:
                gsil_proj(tt, 0)
            elif c == 1 and tt + 1 < 4:
                itok_proj(tt + 1)
            elif c == 2:
                gsil_proj(tt, 1)
            fw.tag = 'hg_rec'
        for h in range(4):
            cpy(act if h % 2 else dve, A(osl[h])[:, tt * 128:(tt + 1) * 128], psum[po_][:, h * 128:(h + 1) * 128], [b_ps[po_]], [b_ar[osl[h]]])
        banks.put(po_)
    w_put(wih[0]); w_put(wgh[0])
    slots.put(*sbf); slots.put(*kdm); slots.put(*attm)
    for s0 in (s_it, s_qb, s_kd):
        slots.put(s0, s0 + 1)
    mixs = [slots.get() for _ in range(8)]
    ys = [ya, yb, yc]

    def merge_branch(n):
        wb = w_get(4); wg0 = w_get(8); wg1 = w_get(8)
        for dc in range(KC):
            wgx = wg0 if dc < 4 else wg1
            pp = banks.get(); pg = banks.get()
            for kc in range(4):
                mm(psum[pp][:, :], wb[1][:, kc, dc * 128:(dc + 1) * 128], ys[n][:, kc, :], kc == 0, kc == 3,
                   [g["b_wsl"][wb[0]]] + ybufs(n, kc), [b_ps[pp]], inc=(kc == 3))
            fm_proj(wgx, dc % 4, pg)
            sg = slots.get()
            actf(A(sg), psum[pg][:, :], AF.Sigmoid, [b_ps[pg], b_small], [b_ar[sg]],
                 bias=sm[:, S_BGATE + n * 8 + dc:S_BGATE + n * 8 + dc + 1], scale=1.0)
            if n == 0:
                ttop(dve, A(mixs[dc]), psum[pp][:, :], A(sg), ALU.mult, [b_ps[pp], b_ar[sg]], [b_ar[mixs[dc]]])
            else:
                ttop(dve, A(sg), psum[pp][:, :], A(sg), ALU.mult, [b_ps[pp], b_ar[sg]], [b_ar[sg]])
                ttop(dve, A(mixs[dc]), A(mixs[dc]), A(sg), ALU.add, [b_ar[mixs[dc]], b_ar[sg]], [b_ar[mixs[dc]]])
            slots.put(sg); banks.put(pp, pg)
        w_put(wb[0]); w_put(wg0[0]); w_put(wg1[0])
    fw.tag = 'merge'
    merge_branch(0)
    fw.tag = 'hg_norm'
    for h in range(4):
        sq = slots.get(); pm = banks.get()
        fw.op(act, (lambda sq=sq, h=h: nc.scalar.activation(out=A(sq), in_=A(osl[h]), func=AF.Square)), [b_ar[osl[h]]], [b_ar[sq]])
        mm(psum[pm][:, :], onesf, A(sq), True, True, [b_ar[sq], b_constA], [b_ps[pm]])
        actf(A(sq), psum[pm][:, :], AF.Sqrt, [b_ps[pm], b_eps], [b_ar[sq]], bias=epst[:, 0:1], scale=1.0 / 128)
        fw.op(dve, (lambda sq=sq: nc.vector.reciprocal(A(sq), A(sq))), [b_ar[sq]], [b_ar[sq]])
        sttop(dve, A(sq), A(osl[h]), sm[:, S_HGG:S_HGG + 1], A(sq), ALU.mult, ALU.mult, [b_ar[osl[h]], b_ar[sq], b_small], [b_ar[sq]])
        ttop(dve, yc[:, h, :], A(sq), gsil[:, h, :], ALU.mult, [b_ar[sq]] + Bh(s_gs, h), ybufs(2, h))
        slots.put(sq); banks.put(pm)
    slots.put(*osl)
    slots.put(s_gs, s_gs + 1)
    tap("yc", key, yc, ybufs(2, 0) + ybufs(2, 2))
    fw.tag = 'merge'
    merge_branch(1)
    merge_branch(2)
    slots.put(*ysl)
    _mb = slots.get_run(4)
    mb = [_mb, _mb + 2]
    mixb = arena[:, mb[0]:mb[0] + 4, :].bitcast(BF16).rearrange("p a (h t) -> p (a h) t", h=2)
    mixbB = lambda dc: [b_ar[mb[0] + dc // 2]]
    for dc in range(KC):
        cpy(act if dc % 2 else dve, mixb[:, dc, :], A(mixs[dc]), [b_ar[mixs[dc]]], mixbB(dc))
    slots.put(*mixs)
    fw.tag = 'outproj'
    for hf in range(2):
        wo = w_get(8)
        for oc in range(4):
            dc = hf * 4 + oc
            pb = banks.get()
            for kc in range(KC):
                mm(psum[pb][:, :], wo[1][:, kc, oc * 128:(oc + 1) * 128], mixb[:, kc, :], kc == 0, kc == KC - 1,
                   [g["b_wsl"][wo[0]]] + mixbB(kc), [b_ps[pb]], inc=(kc == KC - 1))
            sttop(dve, xres[:, dc, :], xres[:, dc, :], float(DN_ALPHA), psum[pb][:, :], ALU.mult, ALU.add, [b_xres[dc], b_ps[pb]], [b_xres[dc]])
            banks.put(pb)
        w_put(wo[0])
    for m0 in mb:
        slots.put(m0, m0 + 1)

    def layer_norm_res(goff, boff):
        m_, r_ = bc_stats([xres[:, dc, :] for dc in range(KC)], [[b_xres[dc]] for dc in range(KC)], float(D))
        for dc in range(KC):
            ttop(dve, xres[:, dc, :], xres[:, dc, :], A(m_), ALU.subtract, [b_xres[dc], b_ar[m_]], [b_xres[dc]])
            ttop(dve, xres[:, dc, :], xres[:, dc, :], A(r_), ALU.mult, [b_xres[dc], b_ar[r_]], [b_xres[dc]])
            actf(xbf[:, dc, :], xres[:, dc, :], AF.Identity, [b_xres[dc], b_small], [b_xbf[dc]],
                 bias=sm[:, boff + dc:boff + dc + 1], scale=sm[:, goff + dc:goff + dc + 1])
            actf(xres[:, dc, :], xres[:, dc, :], AF.Identity, [b_xres[dc], b_small], [b_xres[dc]],
                 bias=sm[:, boff + dc:boff + dc + 1], scale=sm[:, goff + dc:goff + dc + 1])
        slots.put(m_, r_)
    fw.tag = 'ln1'
    layer_norm_res(S_LN1G, S_LN1B)
    tap("x1", key, xres[:], b_xres)
    if stop <= 4:
        return

    fw.tag = 'router'
    sl_a = slots.get(); sl_b = slots.get(); sl_c = slots.get()
    rt = A(sl_a)[:, 0:256].rearrange("p (a b) -> p a b", a=4); b_rt = b_ar[sl_a]
    rt2 = A(sl_b)[:, 0:256].rearrange("p (a b) -> p a b", a=4); b_rt2 = b_ar[sl_b]
    rt3 = A(sl_c)[:, 0:256].rearrange("p (a b) -> p a b", a=4); b_rt3 = b_ar[sl_c]
    pr = banks.get()
    wr = sm[:, S_WR:S_WR + 288].rearrange("p (k c) -> p k c", k=8)
    for tt in range(4):
        for kc in range(KC):
            mm(psum[pr][:, tt * 64:tt * 64 + 36], xres[:, kc, tt * 128:(tt + 1) * 128], wr[:, kc, :], kc == 0, kc == KC - 1,
               [b_xres[kc], b_small], [b_ps[pr]], inc=(kc == KC - 1))
    L = rt[:, :, 0:36]
    prv = psum[pr][:, 0:256].rearrange("p (t c) -> p t c", c=64)[:, :, 0:36]
    rbv = sm[:, S_RB:S_RB + 36].unsqueeze(1).to_broadcast([128, 4, 36])
    ttop(dve, L, prv, rbv, ALU.add, [b_ps[pr], b_small], [b_rt])
    banks.put(pr)
    gl = rt[:, :, 0:4]
    El = rt[:, :, 4:36]
    gmax = rt2[:, :, 0:1]
    fw.op(dve, lambda: nc.vector.tensor_reduce(out=rt2[:, :, 0], in_=gl, axis=AX.X, op=ALU.max), [b_rt], [b_rt2])
    ge = rt2[:, :, 4:8]
    ttop(dve, ge, gl, gmax.to_broadcast([128, 4, 4]), ALU.subtract, [b_rt, b_rt2], [b_rt2])
    oneh = rt2[:, :, 8:12]
    tsop(dve, oneh, ge, 0.0, None, ALU.is_ge, None, [b_rt2], [b_rt2])
    actf(ge, ge, AF.Exp, [b_rt2], [b_rt2])
    fw.op(dve, lambda: nc.vector.tensor_reduce(out=rt2[:, :, 1], in_=ge, axis=AX.X, op=ALU.add), [b_rt2], [b_rt2])
    fw.op(dve, lambda: nc.vector.reciprocal(rt2[:, :, 2], rt2[:, :, 1]), [b_rt2], [b_rt2])
    pen = rt2[:, :, 12:16]
    tsop(dve, pen, oneh, 1e30, -1e30, ALU.mult, ALU.add, [b_rt2], [b_rt2])
    Em = rt3[:, :, 0:32]
    ttop(dve, Em.rearrange("p t (g e) -> p t g e", g=4), El.rearrange("p t (g e) -> p t g e", g=4),
         pen.unsqueeze(3).to_broadcast([128, 4, 4, 8]), ALU.add, [b_rt, b_rt2], [b_rt3])
    top = rt3[:, :, 32:40]
    for tt in range(4):
        fw.op(dve, (lambda tt=tt: nc.vector.max(out=rt3[:, tt, 32:40], in_=rt3[:, tt, 0:32])), [b_rt3], [b_rt3])
    wts = rt2[:, :, 16:18]
    ttop(dve, rt2[:, :, 18:19], rt3[:, :, 32:33], rt3[:, :, 33:34], ALU.subtract, [b_rt3], [b_rt2])
    actf(rt2[:, :, 18:19], rt2[:, :, 18:19], AF.Sigmoid, [b_rt2], [b_rt2])
    ttop(dve, rt2[:, :, 16:17], rt2[:, :, 18:19], rt2[:, :, 2:3], ALU.mult, [b_rt2], [b_rt2])
    ttop(dve, rt2[:, :, 17:18], rt2[:, :, 2:3], rt2[:, :, 16:17], ALU.subtract, [b_rt2], [b_rt2])
    eq = rt[:, :, 0:32]
    ttop(dve, eq, Em, rt3[:, :, 32:33].to_broadcast([128, 4, 32]), ALU.is_equal, [b_rt3], [b_rt])
    ttop(dve, comb[:], eq, rt2[:, :, 16:17].to_broadcast([128, 4, 32]), ALU.mult, [b_rt, b_rt2], [b_comb])
    ttop(dve, eq, Em, rt3[:, :, 33:34].to_broadcast([128, 4, 32]), ALU.is_equal, [b_rt3], [b_rt])
    ttop(dve, eq, eq, rt2[:, :, 17:18].to_broadcast([128, 4, 32]), ALU.mult, [b_rt, b_rt2], [b_rt])
    ttop(dve, comb[:], comb[:], eq, ALU.add, [b_comb, b_rt], [b_comb])
    tap("comb", key, comb[:], [b_comb])
    slots.put(sl_a, sl_b, sl_c)

    fw.tag = 'moe'
    _a0 = slots.get_run(8)
    accs = list(range(_a0, _a0 + 8))
    hts = [two(), two()]
    def moe_up(e):
        w1 = w_get(8); w3 = w_get(8)
        h0 = hts[e % 2]
        hT = bf4(h0)
        for oc in range(4):
            p1 = banks.get(); p3 = banks.get()
            fm_proj(w1, oc, p1)
            fm_proj(w3, oc, p3)
            sl = slots.get()
            actf(A(sl), psum[p1][:, :], AF.Silu, [b_ps[p1]], [b_ar[sl]])
            ttop(dve, hT[:, oc, :], A(sl), psum[p3][:, :], ALU.mult, [b_ar[sl], b_ps[p3]], Bh(h0, oc))
            slots.put(sl); banks.put(p1, p3)
        w_put(w1[0]); w_put(w3[0])

    def moe_down(e):
        w2 = w_get(4)
        h0 = hts[e % 2]
        hT = bf4(h0)
        for tt in range(4):
            for hf in range(2):
                pb = banks.get()
                for oc in range(4):
                    mm(psum[pb][:, :], hT[:, oc, tt * 128:(tt + 1) * 128], w2[1][:, oc, hf * 512:(hf + 1) * 512], oc == 0, oc == 3,
                       Bh(h0, oc) + [g["b_wsl"][w2[0]]], [b_ps[pb]], inc=(oc == 3))
                a_ = accs[tt * 2 + hf]
                if e == 0:
                    tsop(dve, A(a_), psum[pb][:, :], comb[:, tt, e:e + 1], None, ALU.mult, None, [b_ps[pb], b_comb], [b_ar[a_]])
                else:
                    sttop(dve, A(a_), psum[pb][:, :], comb[:, tt, e:e + 1], A(a_), ALU.mult, ALU.add, [b_ps[pb], b_comb, b_ar[a_]], [b_ar[a_]])
                banks.put(pb)
        w_put(w2[0])
    moe_up(0)
    for e in range(n_exp):
        if e + 1 < n_exp:
            moe_up(e + 1)
        moe_down(e)
    for h0 in hts:
        slots.put(h0, h0 + 1)
    tap("acc", key, arena[:, accs[0]:accs[0] + 8, :].rearrange("p (t h) c -> p t (h c)", h=2), [b_ar[i] for i in accs])
    fw.tag = 'moe_T'
    for dc in range(KC):
        pb = banks.get()
        hf = dc // 4
        cs = (dc % 4) * 128
        for tt in range(4):
            a_ = accs[tt * 2 + hf]
            mm(psum[pb][:, tt * 128:(tt + 1) * 128], A(a_)[:, cs:cs + 128], ident, True, True, [b_ar[a_], b_constA], [b_ps[pb]], inc=(tt == 3))
        sttop(dve, xres[:, dc, :], xres[:, dc, :], float(DN_ALPHA), psum[pb][:, :], ALU.mult, ALU.add, [b_xres[dc], b_ps[pb]], [b_xres[dc]])
        banks.put(pb)
    slots.put(*accs)
    fw.tag = 'ln2'
    layer_norm_res(S_LN2G, S_LN2B)
    tap("x2", key, xres[:], b_xres)


def _const_packs():
    ca = np.zeros((128, NCA), np.float32)
    ca[:, C_ID:C_ID + 128] = np.eye(128)
    idx = np.arange(128)
    ch = idx // 32
    same = ch[:, None] == ch[None, :]
    s_le_t = idx[:, None] <= idx[None, :]
    mid = ch * 32 + 15
    s_le_mid = idx[:, None] <= mid[None, :]
    ca[:, C_M1:C_M1 + 128] = same * (s_le_t.astype(np.float32) - s_le_mid.astype(np.float32))
    ca[:, C_M2:C_M2 + 128] = same * (idx[:, None] > idx[None, :])
    ca[:, C_M3:C_M3 + 128] = same * s_le_t
    for c in range(4):
        ca[:, C_CM + c] = (ch == c)
    d = idx % 64
    jj = d % 8
    inv = ROPE_THETA ** (-(2.0 * jj) / 16.0)
    ca[:, C_INVF] = np.where(d < 16, inv, 0.0).astype(np.float32)
    ca[:, C_SGN] = np.where(d < 8, -1.0, np.where(d < 16, 1.0, 0.0))
    ca[:, C_ONES:C_ONES + 128] = 1.0
    cb = np.zeros((128, NCB), np.float32)
    cb[:, CB_ID:CB_ID + 128] = np.eye(128)
    perm = np.zeros((128, 128), np.float32)
    for m in range(128):
        dm = m % 64
        if dm < 8:
            perm[m + 8, m] = 1.0
        elif dm < 16:
            perm[m - 8, m] = 1.0
    cb[:, CB_PERM:CB_PERM + 128] = perm
    cb[:, CB_M3:CB_M3 + 128] = same * s_le_t
    cb[:, CB_ONES:CB_ONES + 128] = 1.0
    q = np.arange(512)
    for jm in range(4):
        cb[:, CB_MASK + jm * 512:CB_MASK + (jm + 1) * 512] = np.where(q[None, :] >= (jm * 128 + idx[:, None]), 0.0, -30000.0)
    return ca, cb


def _small_pack(inp):
    sp_ = np.zeros((2, 128, NS), np.float32)
    f = lambda a: np.asarray(a, np.float32)
    for l in range(2):
        sp_[l, :, S_CONVW:S_CONVW + 124] = f(inp["conv_w"][l]).reshape(CONV_K, 4, 128).transpose(2, 1, 0).reshape(128, 124)
        sp_[l, :, S_CONVB:S_CONVB + 4] = f(inp["conv_b"][l]).reshape(4, 128).T
        sp_[l, :, S_CLNG:S_CLNG + 4] = f(inp["conv_ln_g"][l]).reshape(4, 128).T
        sp_[l, :, S_CLNB:S_CLNB + 4] = f(inp["conv_ln_b"][l]).reshape(4, 128).T
        sp_[l, :, S_DAG] = f(inp["da_norm_g"][l])
        sp_[l, :, S_HGG] = f(inp["hg_norm_g"][l])
        sp_[l, :, S_BGATE:S_BGATE + 24] = f(inp["b_gate"][l]).reshape(3, 8, 128).transpose(2, 0, 1).reshape(128, 24)
        for nm, off in (("ln1_g", S_LN1G), ("ln1_b", S_LN1B), ("ln2_g", S_LN2G), ("ln2_b", S_LN2B)):
            sp_[l, :, off:off + 8] = f(inp[nm][l]).reshape(8, 128).T
        wr = np.concatenate([f(inp["router_group"][l]), f(inp["router_expert"][l]).transpose(1, 0, 2).reshape(D, 32)], axis=1)
        sp_[l, :, S_WR:S_WR + 288] = wr.reshape(8, 128, 36).transpose(1, 0, 2).reshape(128, 288)
        rb = np.concatenate([f(inp["router_group_b"][l]), f(inp["router_expert_b"][l]).reshape(32)])
        sp_[l, :, S_RB:S_RB + 36] = rb[None, :]
        sp_[l, :, S_LBP:S_LBP + 8] = f(inp["hg_lb"]).reshape(2, 4, 128).transpose(2, 0, 1).reshape(128, 8)
    return sp_


_PROG_CACHE = {}


def _run(inp, layers, x_in):
    keyp = tuple(layers)
    if keyp not in _PROG_CACHE:
        _PROG_CACHE[keyp] = build_program(layers=layers)
    nc = _PROG_CACHE[keyp]
    ca, cb = _const_packs()
    smallp = _small_pack(inp)
    f = lambda a: np.ascontiguousarray(np.asarray(a, np.float32))
    shared = {
        "w_in": f(inp["w_in"]), "w_branch": f(inp["w_branch"]), "w_out": f(inp["w_out"]),
        "exp_w1": f(inp["exp_w1"]), "exp_w3": f(inp["exp_w3"]), "exp_w2": f(inp["exp_w2"]),
        "small": smallp, "lbrow": f(inp["hg_lb"]), "lamrow": f(inp["da_lambda"]).reshape(2, 256), "constA": ca, "constB": cb,
    }
    pos = np.ascontiguousarray(np.asarray(inp["positions"], np.int32))
    in_maps = []
    for c in range(NCORES):
        m = dict(shared)
        m["x"] = np.ascontiguousarray(x_in[c * SPC:(c + 1) * SPC])
        m["positions"] = np.ascontiguousarray(pos[c * SPC:(c + 1) * SPC])
        in_maps.append(m)
    res = run_bass_kernel_spmd(nc, in_maps, core_ids=list(range(NCORES)))
    return np.concatenate([np.asarray(r["out"], np.float32) for r in res.results], axis=0)


def kernel(**inputs):
    x = np.asarray(inputs["x"], np.float32)
    return _run(inputs, (0, 1), x)
```
